# Optimizing a Trainium2 kernel written in Bass

```python
import jax
import jax.numpy as jnp
from jax import lax
import numpy as np

D_MODEL = 4096
BATCH = 4
SEQ = 4096
DEPTH = 1

RET_HEADS = 8
RET_DK = 256
RET_DV = 512
RET_CHUNK = 128
ROPE_BASE = 10000.0
GLA_HEADS = 8
GLA_DK = 256
GLA_DV = 512
GLA_CHUNK = 64
GLA_RANK = 16
GLA_TAU = 16.0
N_BRANCH = 2
N_EXPERTS = 16
EXPERT_FF = 2048
EC_CAPACITY_FACTOR = 2
PLE_DIM = 256
EPS = 1e-6

RET_QK = RET_HEADS * RET_DK
RET_V = RET_HEADS * RET_DV
GLA_QK = GLA_HEADS * GLA_DK
GLA_V = GLA_HEADS * GLA_DV
BRANCH_W = RET_V
IN_SPLITS = (RET_QK, RET_QK, RET_V, RET_V, GLA_QK, GLA_QK, GLA_V, GLA_V, 2 * GLA_RANK, N_BRANCH * D_MODEL)
IN_WIDTH = sum(IN_SPLITS)
IN_OFFSETS = tuple(sum(IN_SPLITS[:i + 1]) for i in range(len(IN_SPLITS) - 1))

kernel_name = 'hybrid_retention_gla_ec_moe_encoder_block'


def rmsnorm(x, g):
    xf = x.astype(jnp.float32)
    y = xf * lax.rsqrt(jnp.mean(xf * xf, axis=-1, keepdims=True) + EPS)
    return (y * g.astype(jnp.float32)).astype(x.dtype)


def head_groupnorm(o, g):
    mu = jnp.mean(o, axis=-1, keepdims=True)
    var = jnp.mean(jnp.square(o - mu), axis=-1, keepdims=True)
    return (o - mu) * lax.rsqrt(var + EPS) * g.astype(jnp.float32).reshape(o.shape[2], o.shape[3])


def head_rmsnorm(o, g):
    return o * lax.rsqrt(jnp.mean(o * o, axis=-1, keepdims=True) + EPS) * g.astype(jnp.float32).reshape(o.shape[2], o.shape[3])


def rotary(t, positions):
    half = t.shape[-1] // 2
    inv_freq = ROPE_BASE ** (-jnp.arange(half, dtype=jnp.float32) / half)
    ang = positions.astype(jnp.float32)[:, :, None, None] * inv_freq
    cos, sin = jnp.cos(ang), jnp.sin(ang)
    t1, t2 = t[..., :half], t[..., half:]
    return jnp.concatenate([t1 * cos - t2 * sin, t1 * sin + t2 * cos], axis=-1)


def to_chunks(t, c):
    b, s, h, d = t.shape
    return t.reshape(b, s // c, c, h, d).transpose(1, 0, 3, 2, 4)


def from_chunks(t):
    n, b, h, c, d = t.shape
    return t.transpose(1, 0, 3, 2, 4).reshape(b, n * c, h, d)


def flip_seq(t):
    return jnp.flip(t, axis=1)


def retention_scan(q, k, v, log_gamma, strict):
    bsz, _, nh, dk = q.shape
    dv = v.shape[-1]
    c = RET_CHUNK
    pos = jnp.arange(c, dtype=jnp.float32)
    diff = pos[:, None] - pos[None, :]
    mask = (diff > 0) if strict else (diff >= 0)
    decay_in = jnp.where(mask, jnp.exp(jnp.maximum(diff, 0.0)[None] * log_gamma[:, None, None]), 0.0)
    q_dec = jnp.exp((pos + 1.0)[None, :] * log_gamma[:, None])
    k_dec = jnp.exp((c - 1.0 - pos)[None, :] * log_gamma[:, None])
    chunk_dec = jnp.exp(c * log_gamma)

    def step(state, qkv):
        qc, kc, vc = qkv
        scores = jnp.einsum('bhid,bhjd->bhij', qc, kc) * decay_in
        out = jnp.einsum('bhij,bhjv->bhiv', scores, vc) + jnp.einsum('bhid,bhdv->bhiv', qc * q_dec[..., None], state)
        state = chunk_dec[:, None, None] * state + jnp.einsum('bhjd,bhjv->bhdv', kc * k_dec[..., None], vc)
        return state, out

    state0 = jnp.zeros((bsz, nh, dk, dv), jnp.float32)
    _, out = lax.scan(step, state0, (to_chunks(q, c), to_chunks(k, c), to_chunks(v, c)))
    return from_chunks(out)


def gla_scan(q, k, v, log_alpha, strict):
    bsz, _, nh, dk = q.shape
    dv = v.shape[-1]
    c = GLA_CHUNK
    pos = jnp.arange(c)
    mask = (pos[:, None] > pos[None, :]) if strict else (pos[:, None] >= pos[None, :])

    def step(state, inp):
        qc, kc, vc, ac = inp
        cum = jnp.cumsum(ac, axis=2)
        rel = cum[:, :, :, None, :] - cum[:, :, None, :, :]
        rel = jnp.where(mask[:, :, None], rel, -jnp.inf)
        scores = jnp.einsum('bhid,bhjd,bhijd->bhij', qc, kc, jnp.exp(rel))
        out = jnp.einsum('bhij,bhjv->bhiv', scores, vc) + jnp.einsum('bhid,bhdv->bhiv', qc * jnp.exp(cum), state)
        last = cum[:, :, -1:, :]
        state = jnp.exp(last[:, :, 0, :])[..., None] * state + jnp.einsum('bhjd,bhjv->bhdv', kc * jnp.exp(last - cum), vc)
        return state, out

    state0 = jnp.zeros((bsz, nh, dk, dv), jnp.float32)
    _, out = lax.scan(step, state0, (to_chunks(q, c), to_chunks(k, c), to_chunks(v, c), to_chunks(log_alpha, c)))
    return from_chunks(out)


def expert_choice_ffn(xn, w_router, w_gate, w_up, w_down):
    bsz, seq, d = xn.shape
    capacity = EC_CAPACITY_FACTOR * seq // N_EXPERTS
    affinity = jax.nn.softmax(jnp.einsum('bsd,de->bse', xn, w_router).astype(jnp.float32), axis=-1)
    gate, idx = lax.top_k(jnp.swapaxes(affinity, 1, 2), capacity)
    x_sel = jax.vmap(lambda xb, ib: xb[ib])(xn, idx)
    hid = jax.nn.silu(jnp.einsum('becd,edf->becf', x_sel, w_gate)) * jnp.einsum('becd,edf->becf', x_sel, w_up)
    y_sel = jnp.einsum('becf,efd->becd', hid, w_down) * gate[..., None].astype(xn.dtype)
    return jax.vmap(lambda yb, ib: jnp.zeros((seq, d), yb.dtype).at[ib.reshape(-1)].add(yb.reshape(-1, d)))(y_sel, idx)


def setup_inputs(seed: int = 0) -> dict:
    key = jax.random.key(seed)
    ks = jax.random.split(key, 24)
    f32 = jnp.float32

    def normal(k, shape, scale):
        return jax.random.normal(k, shape, f32) * scale

    def gain(k, shape):
        return 1.0 + 0.02 * jax.random.normal(k, shape, f32)

    x = normal(ks[0], (BATCH, SEQ, D_MODEL), 1.0)
    p = normal(ks[1], (DEPTH, BATCH, SEQ, PLE_DIM), 1.0)
    positions = jnp.arange(SEQ, dtype=jnp.int32)[None, :] + jax.random.randint(ks[2], (BATCH, 1), 0, 1024, dtype=jnp.int32)
    norm_mix = gain(ks[3], (DEPTH, D_MODEL))
    w_in = normal(ks[4], (DEPTH, D_MODEL, IN_WIDTH), D_MODEL ** -0.5)
    gam = 1.0 - jnp.exp2(-5.0 - jnp.arange(RET_HEADS, dtype=f32))
    ret_decay_logit = (jnp.log(gam) - jnp.log1p(-gam)) + 0.05 * jax.random.normal(ks[5], (DEPTH, 2, RET_HEADS), f32)
    ret_norm = gain(ks[6], (DEPTH, RET_V))
    gla_gate_w = normal(ks[7], (DEPTH, 2, GLA_RANK, GLA_QK), GLA_RANK ** -0.5)
    gla_gate_b = normal(ks[8], (DEPTH, 2, GLA_QK), 0.1)
    gla_norm = gain(ks[9], (DEPTH, GLA_V))
    w_branch = normal(ks[10], (DEPTH, N_BRANCH, BRANCH_W, D_MODEL), BRANCH_W ** -0.5)
    w_out = normal(ks[11], (DEPTH, D_MODEL, D_MODEL), D_MODEL ** -0.5)
    norm_ffn = gain(ks[12], (DEPTH, D_MODEL))
    w_router = normal(ks[13], (DEPTH, D_MODEL, N_EXPERTS), D_MODEL ** -0.5)
    w_expert_gate = normal(ks[14], (DEPTH, N_EXPERTS, D_MODEL, EXPERT_FF), D_MODEL ** -0.5)
    w_expert_up = normal(ks[15], (DEPTH, N_EXPERTS, D_MODEL, EXPERT_FF), D_MODEL ** -0.5)
    w_expert_down = normal(ks[16], (DEPTH, N_EXPERTS, EXPERT_FF, D_MODEL), EXPERT_FF ** -0.5)
    norm_ple = gain(ks[17], (DEPTH, D_MODEL))
    w_ple_gate = normal(ks[18], (DEPTH, D_MODEL, D_MODEL), D_MODEL ** -0.5)
    w_ple_proj = normal(ks[19], (DEPTH, PLE_DIM, D_MODEL), PLE_DIM ** -0.5)
    norm_final = gain(ks[20], (D_MODEL,))
    return {'x': x, 'p': p, 'positions': positions, 'norm_mix': norm_mix, 'w_in': w_in,
            'ret_decay_logit': ret_decay_logit, 'ret_norm': ret_norm, 'gla_gate_w': gla_gate_w,
            'gla_gate_b': gla_gate_b, 'gla_norm': gla_norm, 'w_branch': w_branch, 'w_out': w_out,
            'norm_ffn': norm_ffn, 'w_router': w_router, 'w_expert_gate': w_expert_gate,
            'w_expert_up': w_expert_up, 'w_expert_down': w_expert_down, 'norm_ple': norm_ple,
            'w_ple_gate': w_ple_gate, 'w_ple_proj': w_ple_proj, 'norm_final': norm_final}


def reference(x, p, positions, norm_mix, w_in, ret_decay_logit, ret_norm, gla_gate_w, gla_gate_b, gla_norm,
              w_branch, w_out, norm_ffn, w_router, w_expert_gate, w_expert_up, w_expert_down, norm_ple,
              w_ple_gate, w_ple_proj, norm_final):
    f32 = jnp.float32
    bsz, seq, _ = x.shape
    h = x
    for i in range(DEPTH):
        xn = rmsnorm(h, norm_mix[i])
        proj = jnp.einsum('bsd,dn->bsn', xn, w_in[i])
        rq, rk, rv, rg, gq, gk, gv, gg, glr, bg = jnp.split(proj, IN_OFFSETS, axis=-1)

        rq = rotary(rq.reshape(bsz, seq, RET_HEADS, RET_DK).astype(f32), positions)
        rk = rotary(rk.reshape(bsz, seq, RET_HEADS, RET_DK).astype(f32), positions) * (RET_DK ** -0.5)
        rv = rv.reshape(bsz, seq, RET_HEADS, RET_DV).astype(f32)
        log_gamma = jax.nn.log_sigmoid(ret_decay_logit[i].astype(f32))
        ret = retention_scan(rq, rk, rv, log_gamma[0], False) + flip_seq(
            retention_scan(flip_seq(rq), flip_seq(rk), flip_seq(rv), log_gamma[1], True))
        ret = head_groupnorm(ret, ret_norm[i]).reshape(bsz, seq, RET_V) * jax.nn.silu(rg.astype(f32))

        gq = gq.reshape(bsz, seq, GLA_HEADS, GLA_DK).astype(f32) * (GLA_DK ** -0.5)
        gk = gk.reshape(bsz, seq, GLA_HEADS, GLA_DK).astype(f32)
        gv = gv.reshape(bsz, seq, GLA_HEADS, GLA_DV).astype(f32)
        gate_logits = jnp.einsum('bszr,zrk->zbsk', glr.reshape(bsz, seq, 2, GLA_RANK).astype(f32),
                                 gla_gate_w[i].astype(f32)) + gla_gate_b[i].astype(f32)[:, None, None, :]
        log_alpha = (jax.nn.log_sigmoid(gate_logits) / GLA_TAU).reshape(2, bsz, seq, GLA_HEADS, GLA_DK)
        gla = gla_scan(gq, gk, gv, log_alpha[0], False) + flip_seq(
            gla_scan(flip_seq(gq), flip_seq(gk), flip_seq(gv), flip_seq(log_alpha[1]), True))
        gla = head_rmsnorm(gla, gla_norm[i]).reshape(bsz, seq, GLA_V) * jax.nn.silu(gg.astype(f32))

        branches = jnp.stack([ret, gla], axis=2).astype(x.dtype)
        y_b = jnp.einsum('bszc,zcd->bszd', branches, w_branch[i])
        gates = jax.nn.sigmoid(bg.reshape(bsz, seq, N_BRANCH, D_MODEL))
        merged = jnp.sum(gates * y_b, axis=2)
        h = h + jnp.einsum('bsd,de->bse', merged, w_out[i])

        h = h + expert_choice_ffn(rmsnorm(h, norm_ffn[i]), w_router[i], w_expert_gate[i], w_expert_up[i], w_expert_down[i])

        ple_gate = jax.nn.sigmoid(jnp.einsum('bsd,de->bse', rmsnorm(h, norm_ple[i]), w_ple_gate[i]))
        h = h + ple_gate * jnp.einsum('bsq,qd->bsd', p[i], w_ple_proj[i])
    return rmsnorm(h, norm_final)
```

```python
import numpy as np
import ml_dtypes
from contextlib import ExitStack
import concourse.bass as bass
import concourse.mybir as mybir
from concourse.bass_utils import run_bass_kernel_spmd

F32 = mybir.dt.float32
BF16 = mybir.dt.bfloat16
I32 = mybir.dt.int32
AF = mybir.ActivationFunctionType
ALU = mybir.AluOpType
AX = mybir.AxisListType

T = 2048
NT = 16
D = 4096
KC = 32
INW = 32800
EPS = 1e-6
NH = 8
NCORES = 8


class Buf:
    __slots__ = ("w", "r", "name")

    def __init__(self, name=""):
        self.w = {}
        self.r = {}
        self.name = name


class SemCtr:
    def __init__(self, sem):
        self.sem = sem
        self.n = 0


class Ctx:
    def __init__(self, nc, es):
        self.nc = nc
        self.es = es
        self.eng = {"pe": nc.tensor, "act": nc.scalar, "dve": nc.vector, "pool": nc.gpsimd, "sp": nc.sync}
        self.esem = {k: self.sem("E_" + k) for k in ("pe", "act", "dve", "pool")}
        self.ecnt = {k: 0 for k in self.esem}
        self.waited = {k: {} for k in self.eng}
        self.nsem = 0
        self.all_sc = []

    def sem(self, name):
        return self.es.enter_context(self.nc.semaphore(name))

    def semctr(self, name):
        sc = SemCtr(self.sem(name))
        self.all_sc.append(sc)
        return sc

    def barrier(self):
        for e in self.eng:
            for k, sem in self.esem.items():
                if self.ecnt[k] > 0 and k != e:
                    self._wait(e, sem, self.ecnt[k])
            for sc in self.all_sc:
                if sc.n > 0:
                    self._wait(e, sc.sem, sc.n)

    def _wait(self, e, sem, val):
        if e == "pe" and sem is self.esem["pe"]:
            return
        w = self.waited[e]
        if w.get(sem, 0) >= val:
            return
        w[sem] = val
        self.eng[e].wait_ge(sem, val)

    def deps(self, e, reads, writes):
        for b in reads:
            for sem, v in b.w.items():
                self._wait(e, sem, v)
        for b in writes:
            for sem, v in b.w.items():
                self._wait(e, sem, v)
            for sem, v in b.r.items():
                self._wait(e, sem, v)

    def _record(self, sem, val, reads, writes, partial=False):
        for b in reads:
            b.r[sem] = val
        for b in writes:
            if partial:
                b.w[sem] = val
            else:
                b.w = {sem: val}
                b.r = {}

    def op(self, e, fn, reads=(), writes=()):
        self.deps(e, reads, writes)
        ins = fn()
        self.ecnt[e] += 1
        ins.then_inc(self.esem[e], 1)
        self._record(self.esem[e], self.ecnt[e], reads, writes)
        return ins

    def mm(self, out_ap, pairs, reads, out_buf, transpose=False):
        self.deps("pe", reads, [out_buf])
        n = len(pairs)
        ins = None
        for i, (l, r) in enumerate(pairs):
            ins = self.nc.tensor.matmul(out_ap, l, r, start=(i == 0), stop=(i == n - 1))
        self.ecnt["pe"] += 1
        ins.then_inc(self.esem["pe"], 1)
        self._record(self.esem["pe"], self.ecnt["pe"], reads, [out_buf])

    def mm_multi(self, fns, reads, out_buf):
        self.deps("pe", reads, [out_buf])
        ins = None
        for f in fns:
            ins = f()
        self.ecnt["pe"] += 1
        ins.then_inc(self.esem["pe"], 1)
        self._record(self.esem["pe"], self.ecnt["pe"], reads, [out_buf])

    def dma(self, q, out_ap, in_ap, reads, writes, sc, partial=False, **kw):
        self.deps(q, reads, [] if partial else writes)
        ins = self.eng[q].dma_start(out=out_ap, in_=in_ap, **kw)
        sc.n += 16
        ins.then_inc(sc.sem, 16)
        self._record(sc.sem, sc.n, reads, writes, partial)
        return ins

    def wait_all(self, e, bufs):
        self.deps(e, bufs, [])


class Ring:
    def __init__(self, cx, name, n, shape, dt, es=None):
        es = es or cx.es
        self.t = [es.enter_context(cx.nc.sbuf_tensor(f"{name}{i}", shape, dt)) for i in range(n)]
        self.b = [Buf(f"{name}{i}") for i in range(n)]
        self.s = [cx.semctr(f"s_{name}{i}") for i in range(n)]
        self.n = n
        self.i = 0

    def next(self):
        i = self.i % self.n
        self.i += 1
        return self.t[i], self.b[i], self.s[i]


def build(stage=99, debug=None):
    nc = bass.Bass("TRN2", target_bir_lowering=False)
    dt = nc.dram_tensor

    def din(name, shape, d=F32):
        return dt(name, list(shape), d, kind="ExternalInput").ap()

    x = din("x", [T, D])
    norm_mix = din("norm_mix", [1, D])
    ident_in = din("ident", [128, 128])
    invf_in = din("invf", [128, 1])
    if stage >= 1:
        pos = din("pos", [1, T], I32)
        w_in = din("w_in", [D, INW])
    out = dt("out", [T, D], F32, kind="ExternalOutput").ap()

    def scr(name, shape, d=BF16):
        kind = "ExternalOutput" if (debug and name in debug) else "Internal"
        return dt(name, list(shape), d, kind=kind).ap()

    qkT = [scr(f"qkT{b}", [NH, 2, 2, 128, T]) for b in range(2)]
    vtm = [scr(f"v{b}", [T, D]) for b in range(2)]
    sgtm = [scr(f"sg{b}", [T, D]) for b in range(2)]
    sbgT = scr("sbgT", [2 * KC, 128, T])
    glrT_d = scr("glrT", [2, 16, T], F32)
    b_qkT = [Buf() for _ in range(2)]
    b_v = [Buf() for _ in range(2)]
    b_sg = [Buf() for _ in range(2)]
    b_sbgT = Buf()
    b_glr = Buf()

    with ExitStack() as es:
        cx = Ctx(nc, es)
        sb = lambda name, shape, d: es.enter_context(nc.sbuf_tensor(name, list(shape), d))
        setup = cx.semctr("setup")
        b_const = Buf("const")
        ident = sb("identb", [128, 128], BF16)
        cx.dma("pool", ident[:], ident_in, [], [b_const], setup, partial=True)
        invf = sb("invf_s", [128, 1], F32)
        cx.dma("sp", invf[:], invf_in, [], [b_const], setup, partial=True)
        psb = [es.enter_context(nc.psum_tensor(f"ps{i}", [128, 512], F32)) for i in range(8)]
        b_ps = [Buf(f"ps{i}") for i in range(8)]
        psi = [0]

        def next_ps():
            i = psi[0] % 8
            psi[0] += 1
            return psb[i], b_ps[i]

        esX = es.enter_context(ExitStack())
        XT = esX.enter_context(nc.sbuf_tensor("AT", [128, KC, T], BF16))
        b_XT = Buf("AT")

        def norm_T(pfx, src, src_bufs, gain_row, XT_, b_XT_, tm_dst=None, b_tm=None):
            with ExitStack() as es0:
                sb0 = lambda name, shape, d: es0.enter_context(nc.sbuf_tensor(pfx + name, list(shape), d))
                gain = sb0("gain", [128, D], F32)
                b_g = Buf()
                cx.dma("sp", gain[:], gain_row.partition_broadcast(128), [], [b_g], setup)
                xr_t = [sb0(f"xr{i}", [128, D], F32) for i in range(2)]
                xr_b = [Buf() for _ in range(2)]
                xr_s = [cx.semctr(f"s_{pfx}xr{i}") for i in range(2)]
                junk = sb0("junk", [128, D], BF16)
                b_junk = Buf()
                xnb_t = [sb0(f"xnb{i}", [128, D], BF16) for i in range(2)]
                xnb_b = [Buf() for _ in range(2)]
                xnb_s = [cx.semctr(f"s_{pfx}xnb{i}") for i in range(2)]
                st = sb0("st", [128, 4 * NT], F32)
                b_st = Buf()
                for t in range(NT):
                    xt, xb, xs = xr_t[t % 2], xr_b[t % 2], xr_s[t % 2]
                    xnb, b_xnb, s_xnb = xnb_t[t % 2], xnb_b[t % 2], xnb_s[t % 2]
                    cx.dma("sp", xt[:], src[t * 128:(t + 1) * 128, :], src_bufs, [xb], xs)
                    c0 = st[:, 4 * t:4 * t + 1]
                    c1 = st[:, 4 * t + 1:4 * t + 2]
                    c2 = st[:, 4 * t + 2:4 * t + 3]
                    cx.op("act", lambda: nc.scalar.activation(out=junk[:], in_=xt[:], func=AF.Square, accum_out=c0), [xb], [b_junk, b_st])
                    cx.op("dve", lambda: nc.vector.tensor_scalar(c1, c0, 1.0 / D, EPS, ALU.mult, ALU.add), [b_st], [b_st])
                    cx.op("act", lambda: nc.scalar.activation(out=c2, in_=c1, func=AF.Sqrt), [b_st], [b_st])
                    cx.op("dve", lambda: nc.vector.reciprocal(c1, c2), [b_st], [b_st])
                    cx.op("dve", lambda: nc.vector.scalar_tensor_tensor(out=xnb[:], in0=xt[:], scalar=c1, in1=gain[:], op0=ALU.mult, op1=ALU.mult),
                          [xb, b_st, b_g], [b_xnb])
                    if tm_dst is not None:
                        cx.dma("sp", tm_dst[t * 128:(t + 1) * 128, :], xnb[:], [b_xnb], [b_tm], s_xnb, partial=True)
                    for g in range(4):
                        ps, pb = next_ps()
                        psv = ps[:].bitcast(BF16)
                        pst = psv[:, 0:1024].rearrange("p (a b) -> p a b", a=8)
                        cx.mm_multi([(lambda j=j: nc.tensor.transpose(pst[:, j, :], xnb[:, (g * 8 + j) * 128:(g * 8 + j + 1) * 128], ident[:])) for j in range(8)],
                                    [b_xnb, b_const], pb)
                        dst = XT_[:, g * 8:(g + 1) * 8, t * 128:(t + 1) * 128]
                        if g % 2 == 0:
                            cx.op("act", lambda: nc.scalar.copy(out=dst, in_=pst), [pb], [b_XT_])
                        else:
                            cx.op("dve", lambda: nc.vector.tensor_copy(out=dst, in_=pst), [pb], [b_XT_])
                cx.barrier()

        norm_T("n0", x, [], norm_mix[0], XT, b_XT)
        if debug == "XT":
            dbg = dt("dbg", [128, KC, T], BF16, kind="ExternalOutput").ap()
            fin = cx.semctr("fin")
            cx.dma("sp", dbg, XT[:], [b_XT], [], fin)
            nc.sync.wait_ge(fin.sem, fin.n)
            return nc
        cx.barrier()
        if stage < 1:
            return nc
        TWO_PI = float(2 * np.pi)
        PI = float(np.pi)
        with ExitStack() as es1:
            sb1 = lambda name, shape, d: es1.enter_context(nc.sbuf_tensor(name, list(shape), d))
            cosb = sb1("cosb", [128, T], BF16)
            sinb = sb1("sinb", [128, T], BF16)
            b_tab = Buf("tab")
            with ExitStack() as est:
                sbt = lambda name, shape, d: est.enter_context(nc.sbuf_tensor(name, list(shape), d))
                posi = sbt("posi", [128, T], I32)
                ang = sbt("ang", [128, T], F32)
                a2 = sbt("a2", [128, T], F32)
                ki = sbt("ki", [128, T], I32)
                kf = sbt("kf", [128, T], F32)
                msk = sbt("msk", [128, T], F32)
                b_t = Buf("tmp_tab")
                cx.dma("sp", posi[:], pos[0].partition_broadcast(128), [], [b_t], setup)
                V = nc.vector
                cx.op("dve", lambda: V.tensor_copy(out=ang[:], in_=posi[:]), [b_t], [b_t])
                cx.op("dve", lambda: V.tensor_scalar(ang[:], ang[:], invf[:, 0:1], None, ALU.mult), [b_t, b_const], [b_t])
                for which, dst in ((0, sinb), (1, cosb)):
                    cx.op("dve", lambda: V.tensor_scalar(a2[:], ang[:], (PI / 2 if which else 0.0), None, ALU.add), [b_t], [b_t])
                    cx.op("dve", lambda: V.tensor_scalar(kf[:], a2[:], 1.0 / TWO_PI, None, ALU.mult), [b_t], [b_t])
                    cx.op("dve", lambda: V.tensor_copy(out=ki[:], in_=kf[:]), [b_t], [b_t])
                    cx.op("dve", lambda: V.tensor_copy(out=kf[:], in_=ki[:]), [b_t], [b_t])
                    cx.op("dve", lambda: V.scalar_tensor_tensor(out=a2[:], in0=kf[:], scalar=-TWO_PI, in1=a2[:], op0=ALU.mult, op1=ALU.add), [b_t], [b_t])
                    cx.op("dve", lambda: V.tensor_single_scalar(msk[:], a2[:], PI, ALU.is_gt), [b_t], [b_t])
                    cx.op("dve", lambda: V.scalar_tensor_tensor(out=a2[:], in0=msk[:], scalar=-TWO_PI, in1=a2[:], op0=ALU.mult, op1=ALU.add), [b_t], [b_t])
                    cx.op("dve", lambda: V.tensor_single_scalar(msk[:], a2[:], -PI, ALU.is_lt), [b_t], [b_t])
                    cx.op("dve", lambda: V.scalar_tensor_tensor(out=a2[:], in0=msk[:], scalar=TWO_PI, in1=a2[:], op0=ALU.mult, op1=ALU.add), [b_t], [b_t])
                    cx.op("dve", lambda: V.tensor_scalar(a2[:], a2[:], PI, -PI, ALU.min, ALU.max), [b_t], [b_t])
                    cx.op("act", lambda: nc.scalar.activation(out=dst[:], in_=a2[:], func=AF.Sin), [b_t], [b_tab])
            cx.barrier()
            wring = Ring(cx, "w", 2, [128, KC, 256], BF16, es1)
            sring = Ring(cx, "stg", 2, [128, 4096], BF16, es1)
            ta = sb1("rot_a", [128, 512], F32)
            tb_ = sb1("rot_b", [128, 512], F32)
            b_rt = Buf("rot_tmp")
            glr_s = sb1("glr_s", [16, 2, T], F32)
            b_glrs = Buf()

            def load_w(c0, ncols):
                wt, wb, ws = wring.next()
                src = w_in[:, c0:c0 + ncols].rearrange("(kc p) c -> p kc c", p=128)
                cx.dma("pool", wt[:, :, 0:ncols], src, [], [wb], ws)
                return wt, wb

            def fm_block(c0, kind, scale, dst_ap, dst_buf):
                wt, wb = load_w(c0, 256)
                stt, stb, sts = sring.next()
                stv = stt[:].rearrange("p (a b) -> p a b", a=2)
                for tb in range(4):
                    tsl = slice(tb * 512, (tb + 1) * 512)
                    pss = []
                    for dch in range(2):
                        ps, pb = next_ps()
                        cx.mm(ps[:, 0:512], [(wt[:, k, dch * 128:(dch + 1) * 128], XT[:, k, tsl]) for k in range(KC)], [wb, b_XT], pb)
                        pss.append((ps, pb))
                    if kind == "rot":
                        (p1, b1), (p2, b2) = pss
                        V = nc.vector
                        cx.op("dve", lambda: V.tensor_tensor(out=ta[:], in0=p1[:, 0:512], in1=cosb[:, tsl], op=ALU.mult), [b1, b_tab], [b_rt])
                        cx.op("dve", lambda: V.tensor_tensor(out=tb_[:], in0=p2[:, 0:512], in1=sinb[:, tsl], op=ALU.mult), [b2, b_tab], [b_rt])
                        cx.op("dve", lambda: V.scalar_tensor_tensor(out=stv[:, 0, tsl], in0=ta[:], scalar=scale, in1=tb_[:], op0=ALU.mult, op1=ALU.subtract) if False else
                              V.tensor_tensor(out=ta[:], in0=ta[:], in1=tb_[:], op=ALU.subtract), [b_rt], [b_rt])
                        cx.op("act", lambda: nc.scalar.activation(out=stv[:, 0, tsl], in_=ta[:], func=AF.Copy, scale=scale), [b_rt], [stb])
                        cx.op("dve", lambda: V.tensor_tensor(out=tb_[:], in0=p1[:, 0:512], in1=sinb[:, tsl], op=ALU.mult), [b1, b_tab], [b_rt])
                        cx.op("dve", lambda: V.tensor_tensor(out=ta[:], in0=p2[:, 0:512], in1=cosb[:, tsl], op=ALU.mult), [b2, b_tab, stb], [b_rt])
                        cx.op("dve", lambda: V.tensor_tensor(out=ta[:], in0=ta[:], in1=tb_[:], op=ALU.add), [b_rt], [b_rt])
                        cx.op("act", lambda: nc.scalar.activation(out=stv[:, 1, tsl], in_=ta[:], func=AF.Copy, scale=scale), [b_rt], [stb])
                    else:
                        fn = AF.Sigmoid if kind == "sig" else AF.Copy
                        for dch, (ps, pb) in enumerate(pss):
                            cx.op("act", lambda: nc.scalar.activation(out=stv[:, dch, tsl], in_=ps[:, 0:512], func=fn, scale=scale), [pb], [stb])
                cx.dma("sp", dst_ap.rearrange("a p t -> p a t"), stv, [stb], [dst_buf], sts, partial=True)

            def tm_block(c0, kind, dst_ap, dst_buf):
                wt, wb = load_w(c0, 256)
                stt, stb, sts = sring.next()
                stv = stt[:].rearrange("p (a b) -> p a b", a=NT)
                for t in range(NT):
                    ps, pb = next_ps()
                    cx.mm(ps[:, 0:256], [(XT[:, k, t * 128:(t + 1) * 128], wt[:, k, :]) for k in range(KC)], [wb, b_XT], pb)
                    if kind == "silu":
                        cx.op("act", lambda: nc.scalar.activation(out=stv[:, t, :], in_=ps[:, 0:256], func=AF.Silu), [pb], [stb])
                    elif t % 2 == 0:
                        cx.op("act", lambda: nc.scalar.copy(out=stv[:, t, :], in_=ps[:, 0:256]), [pb], [stb])
                    else:
                        cx.op("dve", lambda: nc.vector.tensor_copy(out=stv[:, t, :], in_=ps[:, 0:256]), [pb], [stb])
                cx.dma("sp", dst_ap.rearrange("(t p) c -> p t c", p=128), stv, [stb], [dst_buf], sts, partial=True)

            OFF = {"rq": 0, "rk": 2048, "rv": 4096, "rg": 8192, "gq": 12288, "gk": 14336, "gv": 16384, "gg": 20480, "glr": 24576, "bg": 24608}
            nblk = NH if stage >= 2 else 1
            for h in range(nblk):
                fm_block(OFF["rq"] + 256 * h, "rot", 1.0, qkT[0][h, 0], b_qkT[0])
            for h in range(nblk):
                fm_block(OFF["rk"] + 256 * h, "rot", 1.0 / 16, qkT[0][h, 1], b_qkT[0])
            for h in range(nblk):
                fm_block(OFF["gq"] + 256 * h, "copy", 1.0 / 16, qkT[1][h, 0], b_qkT[1])
            for h in range(nblk):
                fm_block(OFF["gk"] + 256 * h, "copy", 1.0, qkT[1][h, 1], b_qkT[1])
            for j in range(2 * nblk):
                tm_block(OFF["rv"] + 256 * j, "copy", vtm[0][:, 256 * j:256 * (j + 1)], b_v[0])
            for j in range(2 * nblk):
                tm_block(OFF["gv"] + 256 * j, "copy", vtm[1][:, 256 * j:256 * (j + 1)], b_v[1])
            for j in range(2 * nblk):
                tm_block(OFF["rg"] + 256 * j, "silu", sgtm[0][:, 256 * j:256 * (j + 1)], b_sg[0])
            for j in range(2 * nblk):
                tm_block(OFF["gg"] + 256 * j, "silu", sgtm[1][:, 256 * j:256 * (j + 1)], b_sg[1])
            for j in range(4 * nblk):
                fm_block(OFF["bg"] + 256 * j, "sig", 1.0, sbgT[2 * j:2 * j + 2], b_sbgT)
            wt, wb = load_w(OFF["glr"], 32)
            glr_sc = cx.semctr("s_glr")
            for z in range(2):
                for tb in range(4):
                    tsl = slice(tb * 512, (tb + 1) * 512)
                    ps, pb = next_ps()
                    cx.mm(ps[0:16, 0:512], [(wt[:, k, z * 16:(z + 1) * 16], XT[:, k, tsl]) for k in range(KC)], [wb, b_XT], pb)
                    cx.op("act", lambda: nc.scalar.copy(out=glr_s[:, z, tsl], in_=ps[0:16, 0:512]), [pb], [b_glrs])
            cx.dma("sp", glrT_d.rearrange("z r t -> r z t"), glr_s[:], [b_glrs], [b_glr], glr_sc, partial=True)
            allb = b_qkT + b_v + b_sg + [b_sbgT, b_glr]
        cx.barrier()
        esX.close()
        if stage < 3:
            fin = cx.semctr("fin")
            cx.dma("sp", out[0:128, 0:128], ident_in, allb, [], fin)
            nc.sync.wait_ge(fin.sem, fin.n)
            return nc
        nhead = NH if stage >= 4 or debug is None else 1
        XROWS = 2 * NH * 2 * 2 * 128
        XCH = 1024
        NXC = XROWS // XCH
        xs_src = [dt(f"xs_src{i}", [XCH, 512], BF16).ap() for i in range(NXC)]
        xs_dst = [dt(f"xs_dst{i}", [2 * XCH, 512], BF16).ap() for i in range(NXC)]
        b_xsrc = Buf("xs_src")
        b_xdst = Buf("xs_dst")
        branchT = [scr(f"branchT{b}", [KC, 128, T]) for b in range(2)]
        b_brT = [Buf() for _ in range(2)]
        with ExitStack() as es2:
            sb2 = lambda name, shape, d: es2.enter_context(nc.sbuf_tensor(name, list(shape), d))
            V = nc.vector
            A = nc.scalar
            G = nc.gpsimd
            identf = sb2("identf", [128, 128], F32)
            maskF = sb2("maskF_s", [128, 128], F32)
            maskB = sb2("maskB_s", [128, 128], F32)
            rmask = sb2("rmask_s", [128, T], F32)
            flags = sb2("flags_s", [128, 2], F32)
            dl = sb2("dl", [128, 16], F32)
            negb = sb2("negb", [128, 32], F32)
            gbr = sb2("gbr", [32, 128], F32)
            b_c2 = Buf("c2")
            cx.dma("sp", identf[:], ident_in, [], [b_c2], setup, partial=True)
            cx.dma("sp", maskF[:], din("maskF", [128, 128]), [], [b_c2], setup, partial=True)
            cx.dma("sp", maskB[:], din("maskB", [128, 128]), [], [b_c2], setup, partial=True)
            cx.dma("sp", rmask[:], din("rmask", [1, T])[0].partition_broadcast(128), [], [b_c2], setup, partial=True)
            cx.dma("sp", flags[:], din("flags", [1, 2])[0].partition_broadcast(128), [], [b_c2], setup, partial=True)
            cx.dma("sp", dl[:], din("ret_decay_logit", [1, 16])[0].partition_broadcast(128), [], [b_c2], setup, partial=True)
            gate_b = din("gla_gate_b", [2, 2048])
            gate_w = din("gla_gate_w", [2, 16, 2048])
            ret_norm = din("ret_norm", [1, 4096])
            gla_norm = din("gla_norm", [1, 4096])
            cx.dma("sp", gbr[:], gate_b.rearrange("z (c p) -> (z c) p", p=128), [], [b_c2], setup, partial=True)
            cx.op("act", lambda: A.activation(out=dl[:], in_=dl[:], func=AF.Exp, scale=-1.0), [b_c2], [b_c2])
            cx.op("act", lambda: A.activation(out=dl[:], in_=dl[:], func=AF.Ln, bias=1.0), [b_c2], [b_c2])
            ps, pb = next_ps()
            cx.mm_multi([lambda: nc.tensor.transpose(ps[:, 0:32], gbr[:], identf[0:32, 0:32])], [b_c2], pb)
            cx.op("act", lambda: A.activation(out=negb[:], in_=ps[:, 0:32], func=AF.Copy, scale=-1.0), [pb], [b_c2])

            glr = sb2("glr2", [16, 2, T], F32)
            b_glr2 = Buf()
            ld = cx.semctr("s_ld2")
            cx.dma("sp", glr[:], glrT_d.rearrange("z r t -> r z t"), [b_glr], [b_glr2], ld)
            gw = sb2("gw", [16, 2, 256], F32)
            b_gw = Buf()
            qk = sb2("qk", [128, 2, 2, T], BF16)
            b_qk = Buf()
            vv = sb2("vv", [128, NT, 512], BF16)
            b_vv = Buf()
            spt = sb2("spt", [128, T], F32)
            cum = sb2("cum", [128, T], F32)
            Et = sb2("Et", [128, T], F32)
            b_dec = Buf("dec")
            qh = [sb2(f"qh{z}", [128, 2, T], BF16) for z in range(2)]
            kh = [sb2(f"kh{z}", [128, 2, T], BF16) for z in range(2)]
            b_qh = [Buf() for _ in range(2)]
            b_kh = [Buf() for _ in range(2)]
            ktm = [sb2(f"ktm{z}", [128, NT, 256], BF16) for z in range(2)]
            b_ktm = [Buf() for _ in range(2)]
            sdec = [sb2(f"sdec{z}", [128, 2, NT], F32) for z in range(2)]
            b_sdec = [Buf() for _ in range(2)]
            R = [sb2(f"R{z}", [128, 2, 512], F32) for z in range(2)]
            Rb = [sb2(f"Rb{z}", [128, 2, 512], BF16) for z in range(2)]
            b_R = [Buf() for _ in range(2)]
            b_Rb = [Buf() for _ in range(2)]
            Rtmp = sb2("Rtmp", [128, 512], F32)
            b_Rtmp = Buf()
            cum3 = cum[:].rearrange("p (n c) -> p n c", c=128)
            spt3 = spt[:].rearrange("p (n c) -> p n c", c=128)
            SC = (1.0, 1.0 / 16)

            def prep(b, h, need_q):
                cx.dma("sp", qk[:], qkT[b][h].rearrange("a c p t -> p a c t"), [b_qkT[b]], [b_qk], ld)
                cx.dma("sp", vv[:], vtm[b][:, h * 512:(h + 1) * 512].rearrange("(n p) c -> p n c", p=128), [b_v[b]], [b_vv], ld)
                if b == 1:
                    cx.dma("sp", gw[:], gate_w[:, :, h * 256:(h + 1) * 256].rearrange("z r c -> r z c"), [], [b_gw], ld)
                s = SC[b]
                for z in range(2):
                    for dch in range(2):
                        if b == 0:
                            cx.op("act", lambda: A.activation(out=spt[:], in_=rmask[:], func=AF.Identity, scale=0.0, bias=dl[:, z * 8 + h:z * 8 + h + 1]),
                                  [b_c2], [b_dec])
                        else:
                            col = z * 16 + h * 2 + dch
                            for tb in range(4):
                                tsl = slice(tb * 512, (tb + 1) * 512)
                                ps, pb = next_ps()
                                cx.mm(ps[:, 0:512], [(gw[:, z, dch * 128:(dch + 1) * 128], glr[:, z, tsl])], [b_gw, b_glr2], pb)
                                cx.op("act", lambda: A.activation(out=spt[:, tsl], in_=ps[:, 0:512], func=AF.Exp, scale=-1.0, bias=negb[:, col:col + 1]),
                                      [pb, b_c2], [b_dec])
                            cx.op("act", lambda: A.activation(out=spt[:], in_=spt[:], func=AF.Ln, bias=1.0), [b_dec], [b_dec])
                        cx.op("dve", lambda: V.tensor_tensor_scan(out=cum[:], data0=rmask[:], data1=spt[:], initial=0.0, op0=ALU.mult, op1=ALU.add),
                              [b_dec, b_c2], [b_dec])
                        cx.op("act", lambda: A.activation(out=sdec[z][:, dch, :], in_=cum3[:, :, 127], func=AF.Exp, scale=-s), [b_dec], [b_sdec[z]])
                        if z == 1:
                            cx.op("dve", lambda: V.tensor_tensor(out=cum[:], in0=cum[:], in1=spt[:], op=ALU.subtract), [b_dec], [b_dec])
                        sq = -s if z == 0 else s
                        if need_q:
                            cx.op("act", lambda: A.activation(out=Et[:], in_=cum[:], func=AF.Exp, scale=sq), [b_dec], [b_dec])
                            cx.op("dve", lambda: V.tensor_tensor(out=qh[z][:, dch, :], in0=qk[:, 0, dch, :], in1=Et[:], op=ALU.mult), [b_dec, b_qk], [b_qh[z]])
                        cx.op("act", lambda: A.activation(out=Et[:], in_=cum[:], func=AF.Exp, scale=-sq), [b_dec, b_qh[z]], [b_dec])
                        cx.op("dve", lambda: V.tensor_tensor(out=kh[z][:, dch, :], in0=qk[:, 1, dch, :], in1=Et[:], op=ALU.mult), [b_dec, b_qk], [b_kh[z]])
                    for n in range(NT):
                        ps, pb = next_ps()
                        pv = ps[:].bitcast(BF16)
                        cx.mm_multi([(lambda d_=d_: nc.tensor.transpose(pv[:, d_ * 128:(d_ + 1) * 128], kh[z][:, d_, n * 128:(n + 1) * 128], ident[:])) for d_ in range(2)],
                                    [b_kh[z], b_const], pb)
                        if n % 2 == 0:
                            cx.op("act", lambda: A.copy(out=ktm[z][:, n, :], in_=pv[:, 0:256]), [pb], [b_ktm[z]])
                        else:
                            cx.op("dve", lambda: V.tensor_copy(out=ktm[z][:, n, :], in_=pv[:, 0:256]), [pb], [b_ktm[z]])

            def kv_update(z, n, form_f):
                for dch in range(2):
                    ps, pb = next_ps()
                    cx.mm(ps[:, 0:512], [(ktm[z][:, n, dch * 128:(dch + 1) * 128], vv[:, n, :])], [b_ktm[z], b_vv], pb)
                    if form_f:
                        cx.op("dve", lambda: V.tensor_tensor(out=Rtmp[:], in0=ps[:, 0:512], in1=R[z][:, dch, :], op=ALU.add), [pb, b_R[z]], [b_Rtmp])
                        cx.op("act", lambda: A.activation(out=R[z][:, dch, :], in_=Rtmp[:], func=AF.Copy, scale=sdec[z][:, dch, n:n + 1]), [b_Rtmp, b_sdec[z]], [b_R[z]])
                    else:
                        cx.op("dve", lambda: V.tensor_tensor(out=R[z][:, dch, :], in0=ps[:, 0:512], in1=R[z][:, dch, :], op=ALU.add), [pb, b_R[z]], [b_R[z]])

            def scale_state(z, n):
                for dch in range(2):
                    cx.op("act", lambda: A.activation(out=R[z][:, dch, :], in_=R[z][:, dch, :], func=AF.Copy, scale=sdec[z][:, dch, n:n + 1]), [b_R[z], b_sdec[z]], [b_R[z]])

            def xloc(b, h, z):
                base = ((b * NH + h) * 2 + z) * 256
                return base // XCH, base % XCH

            stA = cx.semctr("s_stA")
            for b in range(2):
                for h in range(nhead):
                    prep(b, h, False)
                    for z in range(2):
                        cx.op("pool", lambda: G.memset(R[z][:], 0.0), [], [b_R[z]])
                    for n in range(NT):
                        kv_update(0, n, True)
                    for n in range(NT - 1, -1, -1):
                        scale_state(1, n)
                        kv_update(1, n, False)
                    for z in range(2):
                        cx.op("dve", lambda: V.tensor_copy(out=Rb[z][:], in_=R[z][:]), [b_R[z]], [b_Rb[z]])
                        ci, r0 = xloc(b, h, z)
                        cx.dma("sp", xs_src[ci][r0:r0 + 256, :].rearrange("(c p) f -> p c f", p=128), Rb[z][:], [b_Rb[z]], [b_xsrc], stA, partial=True)
            cx.deps("pool", [b_xsrc], [])
            ccs = cx.sem("ccsem")
            for i in range(NXC):
                nc.gpsimd.collective_compute("AllGather", ALU.bypass, replica_groups=[[2 * r, 2 * r + 1] for r in range(NCORES // 2)],
                                             ins=[xs_src[i].opt()], outs=[xs_dst[i].opt()]).then_inc(ccs)
                nc.gpsimd.wait_ge(ccs, i + 1)
            for e in ("pool", "sp"):
                cx.eng[e].wait_ge(ccs, NXC)
            o_acc = sb2("o_acc", [128, NT, 512], F32)
            b_o = Buf()
            brs = sb2("brs", [128, 4, T], BF16)
            b_brs = Buf()
            sgc = [sb2(f"sgc{i}", [128, 512], BF16) for i in range(2)]
            b_sgc = [Buf() for _ in range(2)]
            s_sgc = [cx.semctr(f"s_sgc{i}") for i in range(2)]
            gn = sb2("gn", [128, 512], F32)
            b_gn = Buf()
            PT = sb2("PT", [128, 128], BF16)
            b_PT = Buf()
            pt1 = sb2("pt1", [128, 128], F32)
            pt2 = sb2("pt2", [128, 128], F32)
            b_pt = Buf()
            stt = sb2("stt", [128, 8], F32)
            b_stt = Buf()
            yn = sb2("yn", [128, 512], F32)
            ynb = sb2("ynb", [128, 512], BF16)
            b_yn = Buf()
            junk2 = sb2("junk2", [128, 512], BF16)
            b_j2 = Buf()
            stB = cx.semctr("s_stB")
            sgi = [0]
            for b in range(2):
                for h in range(nhead):
                    prep(b, h, True)
                    nrm = ret_norm if b == 0 else gla_norm
                    cx.dma("sp", gn[:], nrm[0, h * 512:(h + 1) * 512].partition_broadcast(128), [], [b_gn], ld)
                    for z in range(2):
                        ci, r0 = xloc(b, h, z)
                        r0 += z * XCH
                        cx.dma("sp", Rb[z][:], xs_dst[ci][r0:r0 + 256, :].rearrange("(c p) f -> p c f", p=128), [], [b_Rb[z]], ld)
                        cx.op("dve", lambda: V.tensor_scalar(R[z][:], Rb[z][:], flags[:, z:z + 1], None, ALU.mult), [b_Rb[z], b_c2], [b_R[z]])
                    for n in range(NT):
                        csl = slice(n * 128, (n + 1) * 128)
                        cx.op("dve", lambda: V.tensor_copy(out=Rb[0][:], in_=R[0][:]), [b_R[0]], [b_Rb[0]])
                        ps, pb = next_ps()
                        cx.mm(ps[:, 0:128], [(kh[0][:, d_, csl], qh[0][:, d_, csl]) for d_ in range(2)], [b_kh[0], b_qh[0]], pb)
                        cx.mm(ps[:, 128:256], [(kh[1][:, d_, csl], qh[1][:, d_, csl]) for d_ in range(2)], [b_kh[1], b_qh[1]], pb)
                        cx.op("dve", lambda: V.tensor_tensor(out=pt1[:], in0=ps[:, 0:128], in1=maskF[:], op=ALU.mult), [pb, b_c2], [b_pt])
                        cx.op("dve", lambda: V.tensor_tensor(out=pt2[:], in0=ps[:, 128:256], in1=maskB[:], op=ALU.mult), [pb, b_c2], [b_pt])
                        cx.op("dve", lambda: V.tensor_tensor(out=PT[:], in0=pt1[:], in1=pt2[:], op=ALU.add), [b_pt], [b_PT])
                        ps2, pb2 = next_ps()
                        cx.mm(ps2[:, 0:512], [(PT[:], vv[:, n, :])] + [(qh[0][:, d_, csl], Rb[0][:, d_, :]) for d_ in range(2)],
                              [b_PT, b_vv, b_qh[0], b_Rb[0]], pb2)
                        cx.op("act", lambda: A.copy(out=o_acc[:, n, :], in_=ps2[:, 0:512]), [pb2], [b_o])
                        kv_update(0, n, True)
                    for n in range(NT - 1, -1, -1):
                        csl = slice(n * 128, (n + 1) * 128)
                        scale_state(1, n)
                        cx.op("dve", lambda: V.tensor_copy(out=Rb[1][:], in_=R[1][:]), [b_R[1]], [b_Rb[1]])
                        ps2, pb2 = next_ps()
                        cx.mm(ps2[:, 0:512], [(qh[1][:, d_, csl], Rb[1][:, d_, :]) for d_ in range(2)], [b_qh[1], b_Rb[1]], pb2)
                        cx.op("dve", lambda: V.tensor_tensor(out=yn[:], in0=ps2[:, 0:512], in1=o_acc[:, n, :], op=ALU.add), [pb2, b_o], [b_yn])
                        kv_update(1, n, False)
                        c = lambda i: stt[:, i:i + 1]
                        cx.op("act", lambda: A.activation(out=junk2[:], in_=yn[:], func=AF.Identity, accum_out=c(0)), [b_yn], [b_j2, b_stt])
                        cx.op("act", lambda: A.activation(out=junk2[:], in_=yn[:], func=AF.Square, accum_out=c(1)), [b_yn], [b_j2, b_stt])
                        cx.op("dve", lambda: V.tensor_scalar(c(2), c(0), 1.0 / 512, None, ALU.mult), [b_stt], [b_stt])
                        cx.op("dve", lambda: V.tensor_scalar(c(3), c(1), 1.0 / 512, EPS, ALU.mult, ALU.add), [b_stt], [b_stt])
                        if b == 0:
                            cx.op("dve", lambda: V.tensor_tensor(out=c(4), in0=c(2), in1=c(2), op=ALU.mult), [b_stt], [b_stt])
                            cx.op("dve", lambda: V.tensor_tensor(out=c(3), in0=c(3), in1=c(4), op=ALU.subtract), [b_stt], [b_stt])
                        cx.op("act", lambda: A.activation(out=c(5), in_=c(3), func=AF.Sqrt), [b_stt], [b_stt])
                        cx.op("dve", lambda: V.reciprocal(c(6), c(5)), [b_stt], [b_stt])
                        if b == 0:
                            cx.op("dve", lambda: V.tensor_scalar(yn[:], yn[:], c(2), c(6), ALU.subtract, ALU.mult), [b_stt, b_yn], [b_yn])
                        else:
                            cx.op("dve", lambda: V.tensor_scalar(yn[:], yn[:], c(6), None, ALU.mult), [b_stt, b_yn], [b_yn])
                        i = sgi[0] % 2
                        sgi[0] += 1
                        cx.dma("sp", sgc[i][:], sgtm[b][n * 128:(n + 1) * 128, h * 512:(h + 1) * 512], [b_sg[b]], [b_sgc[i]], s_sgc[i])
                        cx.op("dve", lambda: V.tensor_tensor(out=yn[:], in0=yn[:], in1=gn[:], op=ALU.mult), [b_yn, b_gn], [b_yn])
                        cx.op("dve", lambda: V.tensor_tensor(out=ynb[:], in0=yn[:], in1=sgc[i][:], op=ALU.mult), [b_yn, b_sgc[i]], [b_yn])
                        ps3, pb3 = next_ps()
                        pv = ps3[:].bitcast(BF16)
                        cx.mm_multi([(lambda cc=cc: nc.tensor.transpose(pv[:, cc * 128:(cc + 1) * 128], ynb[:, cc * 128:(cc + 1) * 128], ident[:])) for cc in range(4)],
                                    [b_yn, b_const], pb3)
                        cx.op("act", lambda: A.copy(out=brs[:, :, csl], in_=pv[:, 0:512].rearrange("p (a b) -> p a b", a=4)), [pb3], [b_brs])
                    cx.dma("sp", branchT[b][h * 4:(h + 1) * 4].rearrange("a p t -> p a t"), brs[:], [b_brs], [b_brT[b]], stB, partial=True)
        cx.barrier()
        if stage < 4:
            fin = cx.semctr("fin")
            cx.dma("sp", out[0:128, 0:128], ident_in, b_brT, [], fin)
            nc.sync.wait_ge(fin.sem, fin.n)
            return nc
        w_branch = din("w_branch", [2, D, D])
        w_out = din("w_out", [D, D])
        norm_ffn = din("norm_ffn", [1, D])
        m0T = scr("m0T", [KC, 128, T])
        mergedT = scr("mergedT", [KC, 128, T])
        h1 = scr("h1", [T, D], F32)
        xn2tm = scr("xn2tm", [T, D])
        b_m0 = Buf()
        b_mT = Buf()
        b_h1 = Buf()
        b_xn2tm = Buf()
        V = nc.vector
        A = nc.scalar
        G = nc.gpsimd
        esX = es.enter_context(ExitStack())
        XT = esX.enter_context(nc.sbuf_tensor("AT3", [128, KC, T], BF16))
        b_XT = Buf("AT3")
        ldx = cx.semctr("s_ldx")

        def load_XT(src, src_buf):
            for g in range(4):
                cx.dma("sp", XT[:, g * 8:(g + 1) * 8, :], src[g * 8:(g + 1) * 8].rearrange("k p t -> p k t"), [src_buf], [b_XT], ldx, partial=(g > 0))

        def w_loader(ring):
            def load_w(W, c0):
                wt, wb, ws = ring.next()
                cx.dma("pool", wt[:], W[:, c0:c0 + 256].rearrange("(kc p) c -> p kc c", p=128), [], [wb], ws)
                return wt, wb
            return load_w

        with ExitStack() as es3:
            sb3 = lambda name, shape, d: es3.enter_context(nc.sbuf_tensor(name, list(shape), d))
            load_w = w_loader(Ring(cx, "w3", 2, [128, KC, 256], BF16, es3))
            with ExitStack() as es3a:
                sb3a = lambda name, shape, d: es3a.enter_context(nc.sbuf_tensor(name, list(shape), d))
                sring = Ring(cx, "stg3", 2, [128, 2, T], BF16, es3a)
                sbg1 = sb3a("sbg1", [128, 2, T], BF16)
                b_sbg1 = Buf()
                s_sbg1 = cx.semctr("s_sbg1")
                m0b = sb3a("m0b", [128, 2, T], BF16)
                b_m0b = Buf()
                s_m0b = cx.semctr("s_m0b")
                gtmp = sb3a("gtmp", [128, 512], F32)
                b_gtmp = Buf()
                for b in range(2):
                    load_XT(branchT[b], b_brT[b])
                    for blk in range(16):
                        wt, wb = load_w(w_branch[b], blk * 256)
                        cx.dma("sp", sbg1[:], sbgT[b * KC + 2 * blk:b * KC + 2 * blk + 2].rearrange("a p t -> p a t"), [b_sbgT], [b_sbg1], s_sbg1)
                        if b == 1:
                            cx.dma("sp", m0b[:], m0T[2 * blk:2 * blk + 2].rearrange("a p t -> p a t"), [b_m0], [b_m0b], s_m0b)
                        stt_, stb, sts = sring.next()
                        for tb in range(4):
                            tsl = slice(tb * 512, (tb + 1) * 512)
                            for dch in range(2):
                                ps, pb = next_ps()
                                cx.mm(ps[:, 0:512], [(wt[:, k, dch * 128:(dch + 1) * 128], XT[:, k, tsl]) for k in range(KC)], [wb, b_XT], pb)
                                if b == 0:
                                    cx.op("dve", lambda: V.tensor_tensor(out=stt_[:, dch, tsl], in0=ps[:, 0:512], in1=sbg1[:, dch, tsl], op=ALU.mult), [pb, b_sbg1], [stb])
                                else:
                                    cx.op("dve", lambda: V.tensor_tensor(out=gtmp[:], in0=ps[:, 0:512], in1=sbg1[:, dch, tsl], op=ALU.mult), [pb, b_sbg1], [b_gtmp])
                                    cx.op("pool", lambda: G.tensor_tensor(out=stt_[:, dch, tsl], in0=gtmp[:], in1=m0b[:, dch, tsl], op=ALU.add), [b_gtmp, b_m0b], [stb])
                        dstT, dstB = (m0T, b_m0) if b == 0 else (mergedT, b_mT)
                        cx.dma("sp", dstT[2 * blk:2 * blk + 2].rearrange("a p t -> p a t"), stt_[:], [stb], [dstB], sts, partial=True)
                cx.barrier()
            with ExitStack() as es3b:
                sb3b = lambda name, shape, d: es3b.enter_context(nc.sbuf_tensor(name, list(shape), d))
                xblk = sb3b("xblk", [128, NT, 256], F32)
                b_xblk = Buf()
                s_xblk = cx.semctr("s_xblk")
                h1s = sb3b("h1s", [128, NT, 256], F32)
                b_h1s = Buf()
                s_h1s = cx.semctr("s_h1s")
                load_XT(mergedT, b_mT)
                for blk in range(16):
                    csl = slice(blk * 256, (blk + 1) * 256)
                    wt, wb = load_w(w_out, blk * 256)
                    cx.dma("sp", xblk[:], x[:, csl].rearrange("(t p) c -> p t c", p=128), [], [b_xblk], s_xblk)
                    for t in range(NT):
                        ps, pb = next_ps()
                        cx.mm(ps[:, 0:256], [(XT[:, k, t * 128:(t + 1) * 128], wt[:, k, :]) for k in range(KC)], [wb, b_XT], pb)
                        cx.op("dve", lambda: V.tensor_tensor(out=h1s[:, t, :], in0=ps[:, 0:256], in1=xblk[:, t, :], op=ALU.add), [pb, b_xblk], [b_h1s])
                    cx.dma("sp", h1[:, csl].rearrange("(t p) c -> p t c", p=128), h1s[:], [b_h1s], [b_h1], s_h1s, partial=True)
                cx.barrier()
        norm_T("n2", h1, [b_h1], norm_ffn[0], XT, b_XT, tm_dst=xn2tm, b_tm=b_xn2tm)
        if stage < 5:
            fin = cx.semctr("fin")
            cx.dma("sp", out[0:128, 0:128], ident_in, [b_xn2tm, b_h1], [], fin)
            nc.sync.wait_ge(fin.sem, fin.n)
            return nc
        w_router = din("w_router", [D, 16])
        CAP = 512
        xa_src = dt("xa_src", [16, T], F32).ap()
        xa_dst = dt("xa_dst", [32, T], F32).ap()
        b_xa = Buf()
        es4 = es.enter_context(ExitStack())
        sb4 = lambda name, shape, d: es4.enter_context(nc.sbuf_tensor(name, list(shape), d))
        rkT_d = scr("rkT_d", [16, T], F32)
        rktm_d = scr("rktm_d", [128, NT * 16], F32)
        gtm_d = scr("gtm_d", [128, NT * 16], BF16)
        b_rt = Buf()
        with ExitStack() as es4a:
            sb4a = lambda name, shape, d: es4a.enter_context(nc.sbuf_tensor(name, list(shape), d))
            identf = sb4a("identf4", [128, 128], F32)
            b_c4 = Buf()
            cx.dma("sp", identf[:], ident_in, [], [b_c4], setup)
            wr = sb4a("wr", [128, KC, 16], BF16)
            cx.dma("pool", wr[:], w_router.rearrange("(kc p) e -> p kc e", p=128), [], [b_c4], setup, partial=True)
            aff = sb4a("aff", [128, NT, 16], F32)
            b_aff = Buf()
            sm4 = sb4a("sm4", [128, 4 * NT], F32)
            b_sm4 = Buf()
            affT = sb4a("affT", [16, T], F32)
            b_affT = Buf()
            for t in range(NT):
                ps, pb = next_ps()
                cx.mm(ps[:, 0:16], [(XT[:, k, t * 128:(t + 1) * 128], wr[:, k, :]) for k in range(KC)], [b_XT, b_c4], pb)
                c = lambda i: sm4[:, 4 * t + i:4 * t + i + 1]
                cx.op("dve", lambda: V.reduce_max(out=c(0), in_=ps[:, 0:16], axis=AX.X), [pb], [b_sm4])
                cx.op("dve", lambda: V.tensor_scalar(c(1), c(0), -1.0, None, ALU.mult), [b_sm4], [b_sm4])
                cx.op("act", lambda: A.activation(out=aff[:, t, :], in_=ps[:, 0:16], func=AF.Exp, bias=c(1), accum_out=c(2)), [pb, b_sm4], [b_aff, b_sm4])
                cx.op("dve", lambda: V.reciprocal(c(3), c(2)), [b_sm4], [b_sm4])
                cx.op("dve", lambda: V.tensor_scalar(aff[:, t, :], aff[:, t, :], c(3), None, ALU.mult), [b_sm4, b_aff], [b_aff])
            for g in range(4):
                ps, pb = next_ps()
                cx.mm_multi([(lambda j=j: nc.tensor.transpose(ps[0:16, j * 128:(j + 1) * 128], aff[:, g * 4 + j, :], identf[:])) for j in range(4)], [b_aff, b_c4], pb)
                cx.op("act", lambda: A.copy(out=affT[:, g * 512:(g + 1) * 512], in_=ps[0:16, 0:512]), [pb], [b_affT])
            s_xa = cx.semctr("s_xa")
            cx.dma("sp", xa_src, affT[:], [b_affT], [b_xa], s_xa)
            cx.deps("pool", [b_xa], [])
            ccs2 = cx.sem("ccsem2")
            nc.gpsimd.collective_compute("AllGather", ALU.bypass, replica_groups=[[2 * r, 2 * r + 1] for r in range(NCORES // 2)],
                                         ins=[xa_src.opt()], outs=[xa_dst.opt()]).then_inc(ccs2)
            for e_ in ("pool", "sp"):
                cx.eng[e_].wait_ge(ccs2, 1)
            work = sb4a("work", [16, 2, T], F32)
            b_work = Buf()
            cx.dma("sp", work[:], xa_dst.rearrange("(r e) t -> e r t", e=16), [], [b_work], s_xa)
            m8 = sb4a("m8", [16, 8], F32)
            b_m8 = Buf()
            workf = work[:].rearrange("e r t -> e (r t)")
            for it in range(CAP // 8):
                cx.op("dve", lambda: V.max(out=m8[:], in_=workf), [b_work], [b_m8])
                if it < CAP // 8 - 1:
                    cx.op("dve", lambda: V.match_replace(out=workf, in_to_replace=m8[:], in_values=workf, imm_value=-1.0), [b_m8, b_work], [b_work])
            maskT = sb4a("maskT", [16, T], F32)
            cntT = sb4a("cntT", [16, T], F32)
            onesT = sb4a("onesT", [16, T], F32)
            rkT = sb4a("rkT", [16, T], F32)
            b_mk = Buf()
            cx.op("pool", lambda: G.memset(onesT[:], 1.0), [], [b_mk])
            cx.op("dve", lambda: V.tensor_scalar(maskT[:], affT[:], m8[:, 7:8], None, ALU.is_ge), [b_affT, b_m8], [b_mk])
            cx.op("dve", lambda: V.tensor_tensor_scan(out=cntT[:], data0=onesT[:], data1=maskT[:], initial=0.0, op0=ALU.mult, op1=ALU.add), [b_mk], [b_mk])
            cx.op("dve", lambda: V.tensor_tensor(out=cntT[:], in0=cntT[:], in1=maskT[:], op=ALU.mult), [b_mk], [b_mk])
            cx.op("dve", lambda: V.tensor_scalar(rkT[:], cntT[:], -1.0, None, ALU.add), [b_mk], [b_mk])
            rktm = sb4a("rktm", [128, NT, 16], F32)
            mtm = sb4a("mtm", [128, NT, 16], F32)
            gtm = sb4a("gtm", [128, NT, 16], BF16)
            b_rk = Buf()
            ps, pb = next_ps()
            cx.mm_multi([(lambda t=t: nc.tensor.transpose(ps[:, t * 16:(t + 1) * 16], rkT[:, t * 128:(t + 1) * 128], identf[0:16, 0:16])) for t in range(NT)], [b_mk, b_c4], pb)
            cx.op("act", lambda: A.copy(out=rktm[:].rearrange("p t e -> p (t e)"), in_=ps[:, 0:256]), [pb], [b_rk])
            cx.op("dve", lambda: V.tensor_single_scalar(mtm[:], rktm[:], 0.0, ALU.is_ge), [b_rk], [b_rk])
            cx.op("dve", lambda: V.tensor_tensor(out=gtm[:], in0=aff[:], in1=mtm[:], op=ALU.mult), [b_rk, b_aff], [b_rk])
            cx.dma("sp", rkT_d, rkT[:], [b_mk], [b_rt], s_xa, partial=True)
            cx.dma("sp", rktm_d, rktm[:].rearrange("p t e -> p (t e)"), [b_rk], [b_rt], s_xa, partial=True)
            cx.dma("sp", gtm_d, gtm[:].rearrange("p t e -> p (t e)"), [b_rk], [b_rt], s_xa, partial=True)
            cx.barrier()
        es4.close()
        esX.close()
        if stage < 6:
            fin = cx.semctr("fin")
            cx.dma("sp", out[0:128, 0:128], ident_in, [b_rt], [], fin)
            nc.sync.wait_ge(fin.sem, fin.n)
            return nc
        weg = din("w_expert_gate", [16, D, 2048])
        weu = din("w_expert_up", [16, D, 2048])
        wed = din("w_expert_down", [16, 2048, D])
        b_h2 = [[Buf() for _ in range(8)] for _ in range(NT)]
        with ExitStack() as es5:
            sb5 = lambda name, shape, d: es5.enter_context(nc.sbuf_tensor(name, list(shape), d))
            b_c5 = Buf()
            rkT = sb5("rkT5", [16, T], F32)
            rktm = sb5("rktm5", [128, NT, 16], F32)
            gtm = sb5("gtm5", [128, NT, 16], BF16)
            cx.dma("sp", rkT[:], rkT_d, [b_rt], [b_c5], setup)
            cx.dma("sp", rktm[:].rearrange("p t e -> p (t e)"), rktm_d, [b_rt], [b_c5], setup, partial=True)
            cx.dma("sp", gtm[:].rearrange("p t e -> p (t e)"), gtm_d, [b_rt], [b_c5], setup, partial=True)
            iota_r = sb5("iota_r", [128, CAP], F32)
            cx.dma("sp", iota_r[:], din("iota_row", [1, CAP])[0].partition_broadcast(128), [], [b_c5], setup, partial=True)
            jv = sb5("jv", [128, 4], F32)
            cx.dma("sp", jv[:], din("jvals", [128, 4]), [], [b_c5], setup, partial=True)
            selc = sb5("selc_s", [16, 16, 128], F32)
            cx.dma("sp", selc[:], din("selc", [16, 16, 128]), [], [b_c5], setup, partial=True)
            xn2h = sb5("xn2h", [128, NT, 2048], BF16)
            b_xh = Buf()
            s_xh = cx.semctr("s_xh")
            wring = Ring(cx, "w5", 2, [128, KC * 256], BF16, es5)
            Pm = sb5("Pm", [128, NT * CAP], BF16)
            b_P = Buf()
            Pv = Pm[:].rearrange("p (t j) -> p t j", t=NT)
            PTv = Pm[:].rearrange("p (c t) -> p c t", c=4)
            xsT = sb5("xsT", [128, KC, CAP], BF16)
            b_xs = Buf()
            hidT = sb5("hidT", [128, 16, CAP], BF16)
            b_hid = Buf()
            ysel = sb5("ysel", [128, 4, 512], BF16)
            b_ys = Buf()
            gsel = sb5("gsel", [128, 4], F32)
            b_gs = Buf()
            stmp = sb5("stmp", [128, 512], F32)
            b_stmp = Buf()
            dtmp = sb5("dtmp", [128, 512], F32)
            b_dtmp = Buf()
            yring = Ring(cx, "yst", 2, [128, 512], F32, es5)
            nexp = 16 if (debug is None or stage >= 7) else 2
            for e in range(nexp):
                for t in range(NT):
                    cx.op("dve", lambda: V.tensor_scalar(Pv[:, t, :], iota_r[:], rktm[:, t, e:e + 1], None, ALU.is_equal), [b_c5], [b_P])
                ps, pb = next_ps()
                for jc in range(4):
                    cx.mm(ps[:, jc:jc + 1], [(Pv[:, t, jc * 128:(jc + 1) * 128], gtm[:, t, e:e + 1]) for t in range(NT)], [b_P, b_c5], pb)
                cx.op("act", lambda: A.copy(out=gsel[:], in_=ps[:, 0:4]), [pb], [b_gs])
                for half in range(2):
                    cx.dma("sp", xn2h[:], xn2tm[:, half * 2048:(half + 1) * 2048].rearrange("(t p) c -> p t c", p=128), [b_xn2tm], [b_xh], s_xh)
                    for dc in range(16):
                        ps, pb = next_ps()
                        cx.mm(ps[:, 0:CAP], [(xn2h[:, t, dc * 128:(dc + 1) * 128], Pv[:, t, :]) for t in range(NT)], [b_xh, b_P], pb)
                        if dc % 2 == 0:
                            cx.op("act", lambda: A.copy(out=xsT[:, half * 16 + dc, :], in_=ps[:, 0:CAP]), [pb], [b_xs])
                        else:
                            cx.op("dve", lambda: V.tensor_copy(out=xsT[:, half * 16 + dc, :], in_=ps[:, 0:CAP]), [pb], [b_xs])
                for blk in range(8):
                    ws_ = []
                    for Wm in (weg, weu):
                        wt, wb, wsm = wring.next()
                        cx.dma("pool", wt[:].rearrange("p (k c) -> p k c", k=KC), Wm[e][:, blk * 256:(blk + 1) * 256].rearrange("(kc p) c -> p kc c", p=128), [], [wb], wsm)
                        ws_.append((wt[:].rearrange("p (k c) -> p k c", k=KC), wb))
                    for fs in range(2):
                        pss = []
                        for (wv, wb) in ws_:
                            ps, pb = next_ps()
                            cx.mm(ps[:, 0:CAP], [(wv[:, k, fs * 128:(fs + 1) * 128], xsT[:, k, :]) for k in range(KC)], [wb, b_xs], pb)
                            pss.append((ps, pb))
                        cx.op("act", lambda: A.activation(out=stmp[:], in_=pss[0][0][:, 0:CAP], func=AF.Silu), [pss[0][1]], [b_stmp])
                        cx.op("dve", lambda: V.tensor_tensor(out=hidT[:, blk * 2 + fs, :], in0=stmp[:], in1=pss[1][0][:, 0:CAP], op=ALU.mult), [b_stmp, pss[1][1]], [b_hid])
                for tb in range(4):
                    tsl = slice(tb * 512, (tb + 1) * 512)
                    ps, pb = next_ps()
                    cx.mm(ps[:, 0:512], [(selc[:, e, :], rkT[:, tsl])], [b_c5], pb)
                    for jc in range(4):
                        cx.op("dve", lambda: V.tensor_scalar(dtmp[:], ps[:, 0:512], jv[:, jc:jc + 1], None, ALU.subtract), [pb, b_c5], [b_dtmp])
                        cx.op("act", lambda: A.activation(out=dtmp[:], in_=dtmp[:], func=AF.Square), [b_dtmp], [b_dtmp])
                        cx.op("dve", lambda: V.tensor_single_scalar(PTv[:, jc, tsl], dtmp[:], 0.25, ALU.is_lt), [b_dtmp], [b_P])
                for cb in range(8):
                    csl = slice(cb * 512, (cb + 1) * 512)
                    wt, wb, wsm = wring.next()
                    wv = wt[:].rearrange("p (f c) -> p f c", f=16)
                    cx.dma("pool", wv, wed[e][:, csl].rearrange("(f p) c -> p f c", p=128), [], [wb], wsm)
                    for jc in range(4):
                        ps, pb = next_ps()
                        cx.mm(ps[:, 0:512], [(hidT[:, f, jc * 128:(jc + 1) * 128], wv[:, f, :]) for f in range(16)], [b_hid, wb], pb)
                        cx.op("act", lambda: A.activation(out=ysel[:, jc, :], in_=ps[:, 0:512], func=AF.Copy, scale=gsel[:, jc:jc + 1]), [pb, b_gs], [b_ys])
                    for t in range(NT):
                        ps, pb = next_ps()
                        cx.mm(ps[:, 0:512], [(PTv[:, jc, t * 128:(t + 1) * 128], ysel[:, jc, :]) for jc in range(4)], [b_P, b_ys], pb)
                        yt, yb, ysm = yring.next()
                        if t % 2 == 0:
                            cx.op("act", lambda: A.copy(out=yt[:], in_=ps[:, 0:512]), [pb], [yb])
                        else:
                            cx.op("dve", lambda: V.tensor_copy(out=yt[:], in_=ps[:, 0:512]), [pb], [yb])
                        cx.dma("pool", h1[t * 128:(t + 1) * 128, csl], yt[:], [yb, b_h1], [b_h2[t][cb]], ysm, accum_op=ALU.add)
            cx.barrier()
        if stage < 7:
            fin = cx.semctr("fin")
            cx.dma("sp", out[0:128, 0:128], ident_in, [], [], fin)
            nc.sync.wait_ge(fin.sem, fin.n)
            return nc
        norm_ple = din("norm_ple", [1, D])
        w_pg = din("w_ple_gate", [D, D])
        w_pp = din("w_ple_proj", [256, D])
        p_in = din("p", [T, 256])
        norm_final = din("norm_final", [1, D])
        h3 = scr("h3", [T, D], F32)
        b_h3 = Buf()
        esX = es.enter_context(ExitStack())
        XT = esX.enter_context(nc.sbuf_tensor("AT6", [128, KC, T], BF16))
        b_XT = Buf("AT6")
        norm_T("n3", h1, [], norm_ple[0], XT, b_XT)
        with ExitStack() as es6:
            sb6 = lambda name, shape, d: es6.enter_context(nc.sbuf_tensor(name, list(shape), d))
            b_c6 = Buf()
            wpp = sb6("wpp", [128, 2, D], BF16)
            cx.dma("pool", wpp[:], w_pp.rearrange("(k p) c -> p k c", p=128), [], [b_c6], setup)
            pT = sb6("pT", [128, 2, T], BF16)
            b_pT = Buf()
            h2b = sb6("h2b", [128, NT, 256], F32)
            b_h2b = Buf()
            s_h2b = cx.semctr("s_h2b")
            with ExitStack() as es6p:
                ptm = es6p.enter_context(nc.sbuf_tensor("ptm", [128, NT, 256], F32))
                pbf = es6p.enter_context(nc.sbuf_tensor("pbf", [128, NT, 256], BF16))
                b_pp = Buf()
                cx.dma("sp", ptm[:], p_in.rearrange("(t p) c -> p t c", p=128), [], [b_pp], setup)
                cx.op("dve", lambda: V.tensor_copy(out=pbf[:], in_=ptm[:]), [b_pp], [b_pp])
                for t in range(NT):
                    ps, pb = next_ps()
                    pv = ps[:].bitcast(BF16)
                    cx.mm_multi([(lambda k2=k2: nc.tensor.transpose(pv[:, k2 * 128:(k2 + 1) * 128], pbf[:, t, k2 * 128:(k2 + 1) * 128], ident[:])) for k2 in range(2)], [b_pp, b_const], pb)
                    cx.op("act", lambda: A.copy(out=pT[:, :, t * 128:(t + 1) * 128], in_=pv[:, 0:256].rearrange("p (a b) -> p a b", a=2)), [pb], [b_pT])
                cx.barrier()
            h3s = h2b
            b_h3s = b_h2b
            s_h3s = s_h2b
            load_w = w_loader(Ring(cx, "w6", 2, [128, KC, 256], BF16, es6))
            sgt = sb6("sgt", [128, 256], F32)
            b_sgt = Buf()
            t1t = sb6("t1t", [128, 256], F32)
            b_t1 = Buf()
            for blk in range(16):
                csl = slice(blk * 256, (blk + 1) * 256)
                wt, wb = load_w(w_pg, blk * 256)
                cx.dma("sp", h2b[:], h1[:, csl].rearrange("(t p) c -> p t c", p=128), [], [b_h2b], s_h2b)
                for t in range(NT):
                    tsl = slice(t * 128, (t + 1) * 128)
                    ps, pb = next_ps()
                    cx.mm(ps[:, 0:256], [(XT[:, k, tsl], wt[:, k, :]) for k in range(KC)], [wb, b_XT], pb)
                    ps2, pb2 = next_ps()
                    cx.mm(ps2[:, 0:256], [(pT[:, k2, tsl], wpp[:, k2, csl]) for k2 in range(2)], [b_pT, b_c6], pb2)
                    cx.op("act", lambda: A.activation(out=sgt[:], in_=ps[:, 0:256], func=AF.Sigmoid), [pb], [b_sgt])
                    cx.op("dve", lambda: V.tensor_tensor(out=t1t[:], in0=sgt[:], in1=ps2[:, 0:256], op=ALU.mult), [b_sgt, pb2], [b_t1])
                    cx.op("pool", lambda: G.tensor_tensor(out=h3s[:, t, :], in0=t1t[:], in1=h2b[:, t, :], op=ALU.add), [b_t1, b_h2b], [b_h3s])
                cx.dma("sp", h3[:, csl].rearrange("(t p) c -> p t c", p=128), h3s[:], [b_h3s], [b_h3], s_h3s, partial=True)
            cx.barrier()
        esX.close()
        fin = cx.semctr("fin")
        with ExitStack() as es7:
            sb7 = lambda name, shape, d: es7.enter_context(nc.sbuf_tensor(name, list(shape), d))
            gain = sb7("gainF", [128, D], F32)
            b_g = Buf()
            cx.dma("sp", gain[:], norm_final[0].partition_broadcast(128), [], [b_g], setup)
            xr_t = [sb7(f"fr{i}", [128, D], F32) for i in range(2)]
            xr_b = [Buf() for _ in range(2)]
            xr_s = [cx.semctr(f"s_fr{i}") for i in range(2)]
            yo_t = [sb7(f"fo{i}", [128, D], F32) for i in range(2)]
            yo_b = [Buf() for _ in range(2)]
            junk = sb7("junkF", [128, D], BF16)
            b_junk = Buf()
            st = sb7("stF", [128, 4 * NT], F32)
            b_st = Buf()
            for t in range(NT):
                xt, xb, xs = xr_t[t % 2], xr_b[t % 2], xr_s[t % 2]
                yo, yb = yo_t[t % 2], yo_b[t % 2]
                cx.dma("sp", xt[:], h3[t * 128:(t + 1) * 128, :], [b_h3], [xb], xs)
                c0 = st[:, 4 * t:4 * t + 1]
                c1 = st[:, 4 * t + 1:4 * t + 2]
                c2 = st[:, 4 * t + 2:4 * t + 3]
                cx.op("act", lambda: A.activation(out=junk[:], in_=xt[:], func=AF.Square, accum_out=c0), [xb], [b_junk, b_st])
                cx.op("dve", lambda: V.tensor_scalar(c1, c0, 1.0 / D, EPS, ALU.mult, ALU.add), [b_st], [b_st])
                cx.op("act", lambda: A.activation(out=c2, in_=c1, func=AF.Sqrt), [b_st], [b_st])
                cx.op("dve", lambda: V.reciprocal(c1, c2), [b_st], [b_st])
                cx.op("dve", lambda: V.scalar_tensor_tensor(out=yo[:], in0=xt[:], scalar=c1, in1=gain[:], op0=ALU.mult, op1=ALU.mult), [xb, b_st, b_g], [yb])
                cx.dma("sp", out[t * 128:(t + 1) * 128, :], yo[:], [yb], [], fin)
            nc.sync.wait_ge(fin.sem, fin.n)
            cx.barrier()
    return nc


def make_consts():
    ident = np.eye(128, dtype=np.float32)
    invf = (10000.0 ** (-np.arange(128, dtype=np.float32) / np.float32(128))).astype(np.float32).reshape(128, 1)
    jj = np.arange(128)
    maskF = (jj[None, :] >= jj[:, None]).astype(np.float32)
    maskB = (jj[:, None] > jj[None, :]).astype(np.float32)
    rmask = (np.arange(T) % 128 != 0).astype(np.float32).reshape(1, T)
    iota_row = np.arange(512, dtype=np.float32).reshape(1, 512)
    jvals = (np.arange(128)[:, None] + 128 * np.arange(4)[None, :]).astype(np.float32)
    selc = np.zeros((16, 16, 128), np.float32)
    for e in range(16):
        selc[e, e, :] = 1.0
    return {"ident": ident, "invf": invf, "maskF": maskF, "maskB": maskB, "rmask": rmask, "iota_row": iota_row, "jvals": jvals, "selc": selc}


def make_in_maps(inputs, cores):
    f = lambda k: np.asarray(inputs[k], dtype=np.float32)
    x = f("x")
    positions = np.asarray(inputs["positions"], dtype=np.int32)
    p = f("p")[0]
    consts = make_consts()
    shared = {
        "norm_mix": f("norm_mix").reshape(1, D),
        "w_in": np.ascontiguousarray(f("w_in")[0]),
        "ret_decay_logit": f("ret_decay_logit").reshape(1, 16),
        "gla_gate_w": np.ascontiguousarray(f("gla_gate_w")[0]),
        "gla_gate_b": np.ascontiguousarray(f("gla_gate_b")[0]),
        "ret_norm": f("ret_norm").reshape(1, D),
        "gla_norm": f("gla_norm").reshape(1, D),
        "w_branch": np.ascontiguousarray(f("w_branch")[0]),
        "w_out": np.ascontiguousarray(f("w_out")[0]),
        "norm_ffn": f("norm_ffn").reshape(1, D),
        "w_router": np.ascontiguousarray(f("w_router")[0]),
        "w_expert_gate": np.ascontiguousarray(f("w_expert_gate")[0]),
        "w_expert_up": np.ascontiguousarray(f("w_expert_up")[0]),
        "w_expert_down": np.ascontiguousarray(f("w_expert_down")[0]),
        "norm_ple": f("norm_ple").reshape(1, D),
        "w_ple_gate": np.ascontiguousarray(f("w_ple_gate")[0]),
        "w_ple_proj": np.ascontiguousarray(f("w_ple_proj")[0]),
        "norm_final": f("norm_final").reshape(1, D),
    }
    in_maps = []
    for c in cores:
        b, hf = c // 2, c % 2
        sl = slice(hf * T, (hf + 1) * T)
        m = dict(consts)
        m.update(shared)
        m["x"] = np.ascontiguousarray(x[b, sl])
        m["pos"] = np.ascontiguousarray(positions[b, sl]).reshape(1, T)
        m["p"] = np.ascontiguousarray(p[b, sl])
        m["flags"] = np.array([[float(hf), float(1 - hf)]], np.float32)
        in_maps.append(m)
    return in_maps


def kernel(**inputs):
    n = NCORES
    in_maps = make_in_maps(inputs, list(range(n)))
    nc = build(stage=99)
    res = run_bass_kernel_spmd(nc, in_maps, core_ids=list(range(n)))
    outs = [np.asarray(r["out"], dtype=np.float32) for r in res.results]
    return np.stack(outs, 0).reshape(n // 2, 2 * T, D)
```

```python
import numpy as np
import ml_dtypes
from contextlib import ExitStack
import concourse.bass as bass
import concourse.mybir as mybir
from concourse.bass_utils import run_bass_kernel_spmd

F32 = mybir.dt.float32
BF16 = mybir.dt.bfloat16
I32 = mybir.dt.int32
AF = mybir.ActivationFunctionType
ALU = mybir.AluOpType
AX = mybir.AxisListType

T = 2048
NT = 16
D = 4096
KC = 32
INW = 32800
EPS = 1e-6
NH = 8
NCORES = 8


class Buf:
    __slots__ = ("w", "r", "name")

    def __init__(self, name=""):
        self.w = {}
        self.r = {}
        self.name = name


class SemCtr:
    def __init__(self, sem):
        self.sem = sem
        self.n = 0


class Ctx:
    def __init__(self, nc, es):
        self.nc = nc
        self.es = es
        self.eng = {"pe": nc.tensor, "act": nc.scalar, "dve": nc.vector, "pool": nc.gpsimd, "sp": nc.sync}
        self.esem = {k: self.sem("E_" + k) for k in ("pe", "act", "dve", "pool")}
        self.ecnt = {k: 0 for k in self.esem}
        self.waited = {k: {} for k in self.eng}
        self.nsem = 0
        self.all_sc = []

    def sem(self, name):
        return self.es.enter_context(self.nc.semaphore(name))

    def semctr(self, name):
        sc = SemCtr(self.sem(name))
        self.all_sc.append(sc)
        return sc

    def barrier(self):
        for e in self.eng:
            for k, sem in self.esem.items():
                if self.ecnt[k] > 0 and k != e:
                    self._wait(e, sem, self.ecnt[k])
            for sc in self.all_sc:
                if sc.n > 0:
                    self._wait(e, sc.sem, sc.n)

    def _wait(self, e, sem, val):
        if e == "pe" and sem is self.esem["pe"]:
            return
        w = self.waited[e]
        if w.get(sem, 0) >= val:
            return
        w[sem] = val
        self.eng[e].wait_ge(sem, val)

    def deps(self, e, reads, writes):
        for b in reads:
            for sem, v in b.w.items():
                self._wait(e, sem, v)
        for b in writes:
            for sem, v in b.w.items():
                self._wait(e, sem, v)
            for sem, v in b.r.items():
                self._wait(e, sem, v)

    def _record(self, sem, val, reads, writes, partial=False):
        for b in reads:
            b.r[sem] = val
        for b in writes:
            if partial:
                b.w[sem] = val
            else:
                b.w = {sem: val}
                b.r = {}

    def op(self, e, fn, reads=(), writes=()):
        self.deps(e, reads, writes)
        ins = fn()
        self.ecnt[e] += 1
        ins.then_inc(self.esem[e], 1)
        self._record(self.esem[e], self.ecnt[e], reads, writes)
        return ins

    def mm(self, out_ap, pairs, reads, out_buf, transpose=False):
        self.deps("pe", reads, [out_buf])
        n = len(pairs)
        ins = None
        for i, (l, r) in enumerate(pairs):
            ins = self.nc.tensor.matmul(out_ap, l, r, start=(i == 0), stop=(i == n - 1))
        self.ecnt["pe"] += 1
        ins.then_inc(self.esem["pe"], 1)
        self._record(self.esem["pe"], self.ecnt["pe"], reads, [out_buf])

    def mm_multi(self, fns, reads, out_buf):
        self.deps("pe", reads, [out_buf])
        ins = None
        for f in fns:
            ins = f()
        self.ecnt["pe"] += 1
        ins.then_inc(self.esem["pe"], 1)
        self._record(self.esem["pe"], self.ecnt["pe"], reads, [out_buf])

    def dma(self, q, out_ap, in_ap, reads, writes, sc, partial=False, **kw):
        self.deps(q, reads, [] if partial else writes)
        ins = self.eng[q].dma_start(out=out_ap, in_=in_ap, **kw)
        sc.n += 16
        ins.then_inc(sc.sem, 16)
        self._record(sc.sem, sc.n, reads, writes, partial)
        return ins

    def wait_all(self, e, bufs):
        self.deps(e, bufs, [])


class Ring:
    def __init__(self, cx, name, n, shape, dt, es=None):
        es = es or cx.es
        self.t = [es.enter_context(cx.nc.sbuf_tensor(f"{name}{i}", shape, dt)) for i in range(n)]
        self.b = [Buf(f"{name}{i}") for i in range(n)]
        self.s = [cx.semctr(f"s_{name}{i}") for i in range(n)]
        self.n = n
        self.i = 0

    def next(self):
        i = self.i % self.n
        self.i += 1
        return self.t[i], self.b[i], self.s[i]


def build(stage=99, debug=None):
    nc = bass.Bass("TRN2", target_bir_lowering=False)
    dt = nc.dram_tensor

    def din(name, shape, d=F32):
        return dt(name, list(shape), d, kind="ExternalInput").ap()

    x = din("x", [T, D])
    norm_mix = din("norm_mix", [1, D])
    ident_in = din("ident", [128, 128])
    invf_in = din("invf", [128, 1])
    if stage >= 1:
        pos = din("pos", [1, T], I32)
        w_in = din("w_in", [D, INW])
    out = dt("out", [T, D], F32, kind="ExternalOutput").ap()

    def scr(name, shape, d=BF16):
        kind = "ExternalOutput" if (debug and name in debug) else "Internal"
        return dt(name, list(shape), d, kind=kind).ap()

    qkT = [scr(f"qkT{b}", [NH, 2, 2, 128, T]) for b in range(2)]
    vtm = [scr(f"v{b}", [T, D]) for b in range(2)]
    sgtm = [scr(f"sg{b}", [T, D]) for b in range(2)]
    sbgT = scr("sbgT", [2 * KC, 128, T])
    glrT_d = scr("glrT", [2, 16, T], F32)
    b_qkT = [Buf() for _ in range(2)]
    b_v = [Buf() for _ in range(2)]
    b_sg = [Buf() for _ in range(2)]
    b_sbgT = Buf()
    b_glr = Buf()

    with ExitStack() as es:
        cx = Ctx(nc, es)
        sb = lambda name, shape, d: es.enter_context(nc.sbuf_tensor(name, list(shape), d))
        setup = cx.semctr("setup")
        b_const = Buf("const")
        ident = sb("identb", [128, 128], BF16)
        cx.dma("pool", ident[:], ident_in, [], [b_const], setup, partial=True)
        invf = sb("invf_s", [128, 1], F32)
        cx.dma("sp", invf[:], invf_in, [], [b_const], setup, partial=True)
        psb = [es.enter_context(nc.psum_tensor(f"ps{i}", [128, 512], F32)) for i in range(8)]
        b_ps = [Buf(f"ps{i}") for i in range(8)]
        psi = [0]

        def next_ps():
            i = psi[0] % 8
            psi[0] += 1
            return psb[i], b_ps[i]

        esX = es.enter_context(ExitStack())
        XT = esX.enter_context(nc.sbuf_tensor("AT", [128, KC, T], BF16))
        b_XT = Buf("AT")

        def norm_T(pfx, src, src_bufs, gain_row, XT_, b_XT_, tm_dst=None, b_tm=None):
            with ExitStack() as es0:
                sb0 = lambda name, shape, d: es0.enter_context(nc.sbuf_tensor(pfx + name, list(shape), d))
                gain = sb0("gain", [128, D], F32)
                b_g = Buf()
                cx.dma("sp", gain[:], gain_row.partition_broadcast(128), [], [b_g], setup)
                xr_t = [sb0(f"xr{i}", [128, D], F32) for i in range(2)]
                xr_b = [Buf() for _ in range(2)]
                xr_s = [cx.semctr(f"s_{pfx}xr{i}") for i in range(2)]
                junk = sb0("junk", [128, D], BF16)
                b_junk = Buf()
                xnb_t = [sb0(f"xnb{i}", [128, D], BF16) for i in range(2)]
                xnb_b = [Buf() for _ in range(2)]
                xnb_s = [cx.semctr(f"s_{pfx}xnb{i}") for i in range(2)]
                st = sb0("st", [128, 4 * NT], F32)
                b_st = Buf()
                for t in range(NT):
                    xt, xb, xs = xr_t[t % 2], xr_b[t % 2], xr_s[t % 2]
                    xnb, b_xnb, s_xnb = xnb_t[t % 2], xnb_b[t % 2], xnb_s[t % 2]
                    cx.dma("sp", xt[:], src[t * 128:(t + 1) * 128, :], src_bufs, [xb], xs)
                    c0 = st[:, 4 * t:4 * t + 1]
                    c1 = st[:, 4 * t + 1:4 * t + 2]
                    c2 = st[:, 4 * t + 2:4 * t + 3]
                    cx.op("act", lambda: nc.scalar.activation(out=junk[:], in_=xt[:], func=AF.Square, accum_out=c0), [xb], [b_junk, b_st])
                    cx.op("dve", lambda: nc.vector.tensor_scalar(c1, c0, 1.0 / D, EPS, ALU.mult, ALU.add), [b_st], [b_st])
                    cx.op("act", lambda: nc.scalar.activation(out=c2, in_=c1, func=AF.Sqrt), [b_st], [b_st])
                    cx.op("dve", lambda: nc.vector.reciprocal(c1, c2), [b_st], [b_st])
                    cx.op("dve", lambda: nc.vector.scalar_tensor_tensor(out=xnb[:], in0=xt[:], scalar=c1, in1=gain[:], op0=ALU.mult, op1=ALU.mult),
                          [xb, b_st, b_g], [b_xnb])
                    if tm_dst is not None:
                        cx.dma("sp", tm_dst[t * 128:(t + 1) * 128, :], xnb[:], [b_xnb], [b_tm], s_xnb, partial=True)
                    for g in range(4):
                        ps, pb = next_ps()
                        psv = ps[:].bitcast(BF16)
                        pst = psv[:, 0:1024].rearrange("p (a b) -> p a b", a=8)
                        cx.mm_multi([(lambda j=j: nc.tensor.transpose(pst[:, j, :], xnb[:, (g * 8 + j) * 128:(g * 8 + j + 1) * 128], ident[:])) for j in range(8)],
                                    [b_xnb, b_const], pb)
                        dst = XT_[:, g * 8:(g + 1) * 8, t * 128:(t + 1) * 128]
                        if g % 2 == 0:
                            cx.op("act", lambda: nc.scalar.copy(out=dst, in_=pst), [pb], [b_XT_])
                        else:
                            cx.op("dve", lambda: nc.vector.tensor_copy(out=dst, in_=pst), [pb], [b_XT_])
                cx.barrier()

        norm_T("n0", x, [], norm_mix[0], XT, b_XT)
        if debug == "XT":
            dbg = dt("dbg", [128, KC, T], BF16, kind="ExternalOutput").ap()
            fin = cx.semctr("fin")
            cx.dma("sp", dbg, XT[:], [b_XT], [], fin)
            nc.sync.wait_ge(fin.sem, fin.n)
            return nc
        cx.barrier()
        if stage < 1:
            return nc
        TWO_PI = float(2 * np.pi)
        PI = float(np.pi)
        with ExitStack() as es1:
            sb1 = lambda name, shape, d: es1.enter_context(nc.sbuf_tensor(name, list(shape), d))
            cosb = sb1("cosb", [128, T], BF16)
            sinb = sb1("sinb", [128, T], BF16)
            b_tab = Buf("tab")
            with ExitStack() as est:
                sbt = lambda name, shape, d: est.enter_context(nc.sbuf_tensor(name, list(shape), d))
                posi = sbt("posi", [128, T], I32)
                ang = sbt("ang", [128, T], F32)
                a2 = sbt("a2", [128, T], F32)
                ki = sbt("ki", [128, T], I32)
                kf = sbt("kf", [128, T], F32)
                msk = sbt("msk", [128, T], F32)
                b_t = Buf("tmp_tab")
                cx.dma("sp", posi[:], pos[0].partition_broadcast(128), [], [b_t], setup)
                V = nc.vector
                cx.op("dve", lambda: V.tensor_copy(out=ang[:], in_=posi[:]), [b_t], [b_t])
                cx.op("dve", lambda: V.tensor_scalar(ang[:], ang[:], invf[:, 0:1], None, ALU.mult), [b_t, b_const], [b_t])
                for which, dst in ((0, sinb), (1, cosb)):
                    cx.op("dve", lambda: V.tensor_scalar(a2[:], ang[:], (PI / 2 if which else 0.0), None, ALU.add), [b_t], [b_t])
                    cx.op("dve", lambda: V.tensor_scalar(kf[:], a2[:], 1.0 / TWO_PI, None, ALU.mult), [b_t], [b_t])
                    cx.op("dve", lambda: V.tensor_copy(out=ki[:], in_=kf[:]), [b_t], [b_t])
                    cx.op("dve", lambda: V.tensor_copy(out=kf[:], in_=ki[:]), [b_t], [b_t])
                    cx.op("dve", lambda: V.scalar_tensor_tensor(out=a2[:], in0=kf[:], scalar=-TWO_PI, in1=a2[:], op0=ALU.mult, op1=ALU.add), [b_t], [b_t])
                    cx.op("dve", lambda: V.tensor_single_scalar(msk[:], a2[:], PI, ALU.is_gt), [b_t], [b_t])
                    cx.op("dve", lambda: V.scalar_tensor_tensor(out=a2[:], in0=msk[:], scalar=-TWO_PI, in1=a2[:], op0=ALU.mult, op1=ALU.add), [b_t], [b_t])
                    cx.op("dve", lambda: V.tensor_single_scalar(msk[:], a2[:], -PI, ALU.is_lt), [b_t], [b_t])
                    cx.op("dve", lambda: V.scalar_tensor_tensor(out=a2[:], in0=msk[:], scalar=TWO_PI, in1=a2[:], op0=ALU.mult, op1=ALU.add), [b_t], [b_t])
                    cx.op("dve", lambda: V.tensor_scalar(a2[:], a2[:], PI, -PI, ALU.min, ALU.max), [b_t], [b_t])
                    cx.op("act", lambda: nc.scalar.activation(out=dst[:], in_=a2[:], func=AF.Sin), [b_t], [b_tab])
            cx.barrier()
            wring = Ring(cx, "w", 2, [128, KC, 256], BF16, es1)
            sring = Ring(cx, "stg", 2, [128, 4096], BF16, es1)
            ta = sb1("rot_a", [128, 512], F32)
            tb_ = sb1("rot_b", [128, 512], F32)
            b_rt = Buf("rot_tmp")
            glr_s = sb1("glr_s", [16, 2, T], F32)
            b_glrs = Buf()

            def load_w(c0, ncols):
                wt, wb, ws = wring.next()
                src = w_in[:, c0:c0 + ncols].rearrange("(kc p) c -> p kc c", p=128)
                cx.dma("pool", wt[:, :, 0:ncols], src, [], [wb], ws)
                return wt, wb

            def fm_block(c0, kind, scale, dst_ap, dst_buf):
                wt, wb = load_w(c0, 256)
                stt, stb, sts = sring.next()
                stv = stt[:].rearrange("p (a b) -> p a b", a=2)
                for tb in range(4):
                    tsl = slice(tb * 512, (tb + 1) * 512)
                    pss = []
                    for dch in range(2):
                        ps, pb = next_ps()
                        cx.mm(ps[:, 0:512], [(wt[:, k, dch * 128:(dch + 1) * 128], XT[:, k, tsl]) for k in range(KC)], [wb, b_XT], pb)
                        pss.append((ps, pb))
                    if kind == "rot":
                        (p1, b1), (p2, b2) = pss
                        V = nc.vector
                        cx.op("dve", lambda: V.tensor_tensor(out=ta[:], in0=p1[:, 0:512], in1=cosb[:, tsl], op=ALU.mult), [b1, b_tab], [b_rt])
                        cx.op("dve", lambda: V.tensor_tensor(out=tb_[:], in0=p2[:, 0:512], in1=sinb[:, tsl], op=ALU.mult), [b2, b_tab], [b_rt])
                        cx.op("dve", lambda: V.scalar_tensor_tensor(out=stv[:, 0, tsl], in0=ta[:], scalar=scale, in1=tb_[:], op0=ALU.mult, op1=ALU.subtract) if False else
                              V.tensor_tensor(out=ta[:], in0=ta[:], in1=tb_[:], op=ALU.subtract), [b_rt], [b_rt])
                        cx.op("act", lambda: nc.scalar.activation(out=stv[:, 0, tsl], in_=ta[:], func=AF.Copy, scale=scale), [b_rt], [stb])
                        cx.op("dve", lambda: V.tensor_tensor(out=tb_[:], in0=p1[:, 0:512], in1=sinb[:, tsl], op=ALU.mult), [b1, b_tab], [b_rt])
                        cx.op("dve", lambda: V.tensor_tensor(out=ta[:], in0=p2[:, 0:512], in1=cosb[:, tsl], op=ALU.mult), [b2, b_tab, stb], [b_rt])
                        cx.op("dve", lambda: V.tensor_tensor(out=ta[:], in0=ta[:], in1=tb_[:], op=ALU.add), [b_rt], [b_rt])
                        cx.op("act", lambda: nc.scalar.activation(out=stv[:, 1, tsl], in_=ta[:], func=AF.Copy, scale=scale), [b_rt], [stb])
                    else:
                        fn = AF.Sigmoid if kind == "sig" else AF.Copy
                        for dch, (ps, pb) in enumerate(pss):
                            cx.op("act", lambda: nc.scalar.activation(out=stv[:, dch, tsl], in_=ps[:, 0:512], func=fn, scale=scale), [pb], [stb])
                cx.dma("sp", dst_ap.rearrange("a p t -> p a t"), stv, [stb], [dst_buf], sts, partial=True)

            def tm_block(c0, kind, dst_ap, dst_buf):
                wt, wb = load_w(c0, 256)
                stt, stb, sts = sring.next()
                stv = stt[:].rearrange("p (a b) -> p a b", a=NT)
                for t in range(NT):
                    ps, pb = next_ps()
                    cx.mm(ps[:, 0:256], [(XT[:, k, t * 128:(t + 1) * 128], wt[:, k, :]) for k in range(KC)], [wb, b_XT], pb)
                    if kind == "silu":
                        cx.op("act", lambda: nc.scalar.activation(out=stv[:, t, :], in_=ps[:, 0:256], func=AF.Silu), [pb], [stb])
                    elif t % 2 == 0:
                        cx.op("act", lambda: nc.scalar.copy(out=stv[:, t, :], in_=ps[:, 0:256]), [pb], [stb])
                    else:
                        cx.op("dve", lambda: nc.vector.tensor_copy(out=stv[:, t, :], in_=ps[:, 0:256]), [pb], [stb])
                cx.dma("sp", dst_ap.rearrange("(t p) c -> p t c", p=128), stv, [stb], [dst_buf], sts, partial=True)

            OFF = {"rq": 0, "rk": 2048, "rv": 4096, "rg": 8192, "gq": 12288, "gk": 14336, "gv": 16384, "gg": 20480, "glr": 24576, "bg": 24608}
            nblk = NH if stage >= 2 else 1
            for h in range(nblk):
                fm_block(OFF["rq"] + 256 * h, "rot", 1.0, qkT[0][h, 0], b_qkT[0])
            for h in range(nblk):
                fm_block(OFF["rk"] + 256 * h, "rot", 1.0 / 16, qkT[0][h, 1], b_qkT[0])
            for h in range(nblk):
                fm_block(OFF["gq"] + 256 * h, "copy", 1.0 / 16, qkT[1][h, 0], b_qkT[1])
            for h in range(nblk):
                fm_block(OFF["gk"] + 256 * h, "copy", 1.0, qkT[1][h, 1], b_qkT[1])
            for j in range(2 * nblk):
                tm_block(OFF["rv"] + 256 * j, "copy", vtm[0][:, 256 * j:256 * (j + 1)], b_v[0])
            for j in range(2 * nblk):
                tm_block(OFF["gv"] + 256 * j, "copy", vtm[1][:, 256 * j:256 * (j + 1)], b_v[1])
            for j in range(2 * nblk):
                tm_block(OFF["rg"] + 256 * j, "silu", sgtm[0][:, 256 * j:256 * (j + 1)], b_sg[0])
            for j in range(2 * nblk):
                tm_block(OFF["gg"] + 256 * j, "silu", sgtm[1][:, 256 * j:256 * (j + 1)], b_sg[1])
            for j in range(4 * nblk):
                fm_block(OFF["bg"] + 256 * j, "sig", 1.0, sbgT[2 * j:2 * j + 2], b_sbgT)
            wt, wb = load_w(OFF["glr"], 32)
            glr_sc = cx.semctr("s_glr")
            for z in range(2):
                for tb in range(4):
                    tsl = slice(tb * 512, (tb + 1) * 512)
                    ps, pb = next_ps()
                    cx.mm(ps[0:16, 0:512], [(wt[:, k, z * 16:(z + 1) * 16], XT[:, k, tsl]) for k in range(KC)], [wb, b_XT], pb)
                    cx.op("act", lambda: nc.scalar.copy(out=glr_s[:, z, tsl], in_=ps[0:16, 0:512]), [pb], [b_glrs])
            cx.dma("sp", glrT_d.rearrange("z r t -> r z t"), glr_s[:], [b_glrs], [b_glr], glr_sc, partial=True)
            allb = b_qkT + b_v + b_sg + [b_sbgT, b_glr]
        cx.barrier()
        esX.close()
        if stage < 3:
            fin = cx.semctr("fin")
            cx.dma("sp", out[0:128, 0:128], ident_in, allb, [], fin)
            nc.sync.wait_ge(fin.sem, fin.n)
            return nc
        nhead = NH if stage >= 4 or debug is None else 1
        XROWS = 2 * NH * 2 * 2 * 128
        XCH = 1024
        NXC = XROWS // XCH
        xs_src = [dt(f"xs_src{i}", [XCH, 512], BF16).ap() for i in range(NXC)]
        xs_dst = [dt(f"xs_dst{i}", [2 * XCH, 512], BF16).ap() for i in range(NXC)]
        b_xsrc = Buf("xs_src")
        b_xdst = Buf("xs_dst")
        branchT = [scr(f"branchT{b}", [KC, 128, T]) for b in range(2)]
        b_brT = [Buf() for _ in range(2)]
        with ExitStack() as es2:
            sb2 = lambda name, shape, d: es2.enter_context(nc.sbuf_tensor(name, list(shape), d))
            V = nc.vector
            A = nc.scalar
            G = nc.gpsimd
            identf = sb2("identf", [128, 128], F32)
            maskF = sb2("maskF_s", [128, 128], F32)
            maskB = sb2("maskB_s", [128, 128], F32)
            rmask = sb2("rmask_s", [128, T], F32)
            flags = sb2("flags_s", [128, 2], F32)
            dl = sb2("dl", [128, 16], F32)
            negb = sb2("negb", [128, 32], F32)
            gbr = sb2("gbr", [32, 128], F32)
            b_c2 = Buf("c2")
            cx.dma("sp", identf[:], ident_in, [], [b_c2], setup, partial=True)
            cx.dma("sp", maskF[:], din("maskF", [128, 128]), [], [b_c2], setup, partial=True)
            cx.dma("sp", maskB[:], din("maskB", [128, 128]), [], [b_c2], setup, partial=True)
            cx.dma("sp", rmask[:], din("rmask", [1, T])[0].partition_broadcast(128), [], [b_c2], setup, partial=True)
            cx.dma("sp", flags[:], din("flags", [1, 2])[0].partition_broadcast(128), [], [b_c2], setup, partial=True)
            cx.dma("sp", dl[:], din("ret_decay_logit", [1, 16])[0].partition_broadcast(128), [], [b_c2], setup, partial=True)
            gate_b = din("gla_gate_b", [2, 2048])
            gate_w = din("gla_gate_w", [2, 16, 2048])
            ret_norm = din("ret_norm", [1, 4096])
            gla_norm = din("gla_norm", [1, 4096])
            cx.dma("sp", gbr[:], gate_b.rearrange("z (c p) -> (z c) p", p=128), [], [b_c2], setup, partial=True)
            cx.op("act", lambda: A.activation(out=dl[:], in_=dl[:], func=AF.Exp, scale=-1.0), [b_c2], [b_c2])
            cx.op("act", lambda: A.activation(out=dl[:], in_=dl[:], func=AF.Ln, bias=1.0), [b_c2], [b_c2])
            ps, pb = next_ps()
            cx.mm_multi([lambda: nc.tensor.transpose(ps[:, 0:32], gbr[:], identf[0:32, 0:32])], [b_c2], pb)
            cx.op("act", lambda: A.activation(out=negb[:], in_=ps[:, 0:32], func=AF.Copy, scale=-1.0), [pb], [b_c2])

            glr = sb2("glr2", [16, 2, T], F32)
            b_glr2 = Buf()
            ld = cx.semctr("s_ld2")
            cx.dma("sp", glr[:], glrT_d.rearrange("z r t -> r z t"), [b_glr], [b_glr2], ld)
            gw = sb2("gw", [16, 2, 256], F32)
            b_gw = Buf()
            qk = sb2("qk", [128, 2, 2, T], BF16)
            b_qk = Buf()
            vv = sb2("vv", [128, NT, 512], BF16)
            b_vv = Buf()
            spt = sb2("spt", [128, T], F32)
            cum = sb2("cum", [128, T], F32)
            Et = sb2("Et", [128, T], F32)
            b_dec = Buf("dec")
            qh = [sb2(f"qh{z}", [128, 2, T], BF16) for z in range(2)]
            kh = [sb2(f"kh{z}", [128, 2, T], BF16) for z in range(2)]
            b_qh = [Buf() for _ in range(2)]
            b_kh = [Buf() for _ in range(2)]
            ktm = [sb2(f"ktm{z}", [128, NT, 256], BF16) for z in range(2)]
            b_ktm = [Buf() for _ in range(2)]
            sdec = [sb2(f"sdec{z}", [128, 2, NT], F32) for z in range(2)]
            b_sdec = [Buf() for _ in range(2)]
            R = [sb2(f"R{z}", [128, 2, 512], F32) for z in range(2)]
            Rb = [sb2(f"Rb{z}", [128, 2, 512], BF16) for z in range(2)]
            b_R = [Buf() for _ in range(2)]
            b_Rb = [Buf() for _ in range(2)]
            Rtmp = sb2("Rtmp", [128, 512], F32)
            b_Rtmp = Buf()
            cum3 = cum[:].rearrange("p (n c) -> p n c", c=128)
            spt3 = spt[:].rearrange("p (n c) -> p n c", c=128)
            SC = (1.0, 1.0 / 16)

            def prep(b, h, need_q):
                cx.dma("sp", qk[:], qkT[b][h].rearrange("a c p t -> p a c t"), [b_qkT[b]], [b_qk], ld)
                cx.dma("sp", vv[:], vtm[b][:, h * 512:(h + 1) * 512].rearrange("(n p) c -> p n c", p=128), [b_v[b]], [b_vv], ld)
                if b == 1:
                    cx.dma("sp", gw[:], gate_w[:, :, h * 256:(h + 1) * 256].rearrange("z r c -> r z c"), [], [b_gw], ld)
                s = SC[b]
                for z in range(2):
                    for dch in range(2):
                        if b == 0:
                            cx.op("act", lambda: A.activation(out=spt[:], in_=rmask[:], func=AF.Identity, scale=0.0, bias=dl[:, z * 8 + h:z * 8 + h + 1]),
                                  [b_c2], [b_dec])
                        else:
                            col = z * 16 + h * 2 + dch
                            for tb in range(4):
                                tsl = slice(tb * 512, (tb + 1) * 512)
                                ps, pb = next_ps()
                                cx.mm(ps[:, 0:512], [(gw[:, z, dch * 128:(dch + 1) * 128], glr[:, z, tsl])], [b_gw, b_glr2], pb)
                                cx.op("act", lambda: A.activation(out=spt[:, tsl], in_=ps[:, 0:512], func=AF.Exp, scale=-1.0, bias=negb[:, col:col + 1]),
                                      [pb, b_c2], [b_dec])
                            cx.op("act", lambda: A.activation(out=spt[:], in_=spt[:], func=AF.Ln, bias=1.0), [b_dec], [b_dec])
                        cx.op("dve", lambda: V.tensor_tensor_scan(out=cum[:], data0=rmask[:], data1=spt[:], initial=0.0, op0=ALU.mult, op1=ALU.add),
                              [b_dec, b_c2], [b_dec])
                        cx.op("act", lambda: A.activation(out=sdec[z][:, dch, :], in_=cum3[:, :, 127], func=AF.Exp, scale=-s), [b_dec], [b_sdec[z]])
                        if z == 1:
                            cx.op("dve", lambda: V.tensor_tensor(out=cum[:], in0=cum[:], in1=spt[:], op=ALU.subtract), [b_dec], [b_dec])
                        sq = -s if z == 0 else s
                        if need_q:
                            cx.op("act", lambda: A.activation(out=Et[:], in_=cum[:], func=AF.Exp, scale=sq), [b_dec], [b_dec])
                            cx.op("dve", lambda: V.tensor_tensor(out=qh[z][:, dch, :], in0=qk[:, 0, dch, :], in1=Et[:], op=ALU.mult), [b_dec, b_qk], [b_qh[z]])
                        cx.op("act", lambda: A.activation(out=Et[:], in_=cum[:], func=AF.Exp, scale=-sq), [b_dec, b_qh[z]], [b_dec])
                        cx.op("dve", lambda: V.tensor_tensor(out=kh[z][:, dch, :], in0=qk[:, 1, dch, :], in1=Et[:], op=ALU.mult), [b_dec, b_qk], [b_kh[z]])
                    for n in range(NT):
                        ps, pb = next_ps()
                        pv = ps[:].bitcast(BF16)
                        cx.mm_multi([(lambda d_=d_: nc.tensor.transpose(pv[:, d_ * 128:(d_ + 1) * 128], kh[z][:, d_, n * 128:(n + 1) * 128], ident[:])) for d_ in range(2)],
                                    [b_kh[z], b_const], pb)
                        if n % 2 == 0:
                            cx.op("act", lambda: A.copy(out=ktm[z][:, n, :], in_=pv[:, 0:256]), [pb], [b_ktm[z]])
                        else:
                            cx.op("dve", lambda: V.tensor_copy(out=ktm[z][:, n, :], in_=pv[:, 0:256]), [pb], [b_ktm[z]])

            def kv_update(z, n, form_f):
                for dch in range(2):
                    ps, pb = next_ps()
                    cx.mm(ps[:, 0:512], [(ktm[z][:, n, dch * 128:(dch + 1) * 128], vv[:, n, :])], [b_ktm[z], b_vv], pb)
                    if form_f:
                        cx.op("dve", lambda: V.tensor_tensor(out=Rtmp[:], in0=ps[:, 0:512], in1=R[z][:, dch, :], op=ALU.add), [pb, b_R[z]], [b_Rtmp])
                        cx.op("act", lambda: A.activation(out=R[z][:, dch, :], in_=Rtmp[:], func=AF.Copy, scale=sdec[z][:, dch, n:n + 1]), [b_Rtmp, b_sdec[z]], [b_R[z]])
                    else:
                        cx.op("dve", lambda: V.tensor_tensor(out=R[z][:, dch, :], in0=ps[:, 0:512], in1=R[z][:, dch, :], op=ALU.add), [pb, b_R[z]], [b_R[z]])

            def scale_state(z, n):
                for dch in range(2):
                    cx.op("act", lambda: A.activation(out=R[z][:, dch, :], in_=R[z][:, dch, :], func=AF.Copy, scale=sdec[z][:, dch, n:n + 1]), [b_R[z], b_sdec[z]], [b_R[z]])

            def xloc(b, h, z):
                base = ((b * NH + h) * 2 + z) * 256
                return base // XCH, base % XCH

            stA = cx.semctr("s_stA")
            for b in range(2):
                for h in range(nhead):
                    prep(b, h, False)
                    for z in range(2):
                        cx.op("pool", lambda: G.memset(R[z][:], 0.0), [], [b_R[z]])
                    for n in range(NT):
                        kv_update(0, n, True)
                        scale_state(1, NT - 1 - n)
                        kv_update(1, NT - 1 - n, False)
                    for z in range(2):
                        cx.op("dve", lambda: V.tensor_copy(out=Rb[z][:], in_=R[z][:]), [b_R[z]], [b_Rb[z]])
                        ci, r0 = xloc(b, h, z)
                        cx.dma("sp", xs_src[ci][r0:r0 + 256, :].rearrange("(c p) f -> p c f", p=128), Rb[z][:], [b_Rb[z]], [b_xsrc], stA, partial=True)
            cx.deps("pool", [b_xsrc], [])
            ccs = cx.sem("ccsem")
            for i in range(NXC):
                nc.gpsimd.collective_compute("AllGather", ALU.bypass, replica_groups=[[2 * r, 2 * r + 1] for r in range(NCORES // 2)],
                                             ins=[xs_src[i].opt()], outs=[xs_dst[i].opt()]).then_inc(ccs)
                nc.gpsimd.wait_ge(ccs, i + 1)
            for e in ("pool", "sp"):
                cx.eng[e].wait_ge(ccs, NXC)
            o_acc = sb2("o_acc", [128, NT, 512], F32)
            b_o = Buf()
            brs = sb2("brs", [128, 4, T], BF16)
            b_brs = Buf()
            sgc = [sb2(f"sgc{i}", [128, 512], BF16) for i in range(2)]
            b_sgc = [Buf() for _ in range(2)]
            s_sgc = [cx.semctr(f"s_sgc{i}") for i in range(2)]
            gn = sb2("gn", [128, 512], F32)
            b_gn = Buf()
            PT = sb2("PT", [128, 128], BF16)
            b_PT = Buf()
            pt1 = sb2("pt1", [128, 128], F32)
            pt2 = sb2("pt2", [128, 128], F32)
            b_pt = Buf()
            stt = sb2("stt", [128, 8], F32)
            b_stt = Buf()
            yn = sb2("yn", [128, 512], F32)
            ynb = sb2("ynb", [128, 512], BF16)
            b_yn = Buf()
            junk2 = sb2("junk2", [128, 512], BF16)
            b_j2 = Buf()
            stB = cx.semctr("s_stB")
            sgi = [0]
            for b in range(2):
                for h in range(nhead):
                    prep(b, h, True)
                    nrm = ret_norm if b == 0 else gla_norm
                    cx.dma("sp", gn[:], nrm[0, h * 512:(h + 1) * 512].partition_broadcast(128), [], [b_gn], ld)
                    for z in range(2):
                        ci, r0 = xloc(b, h, z)
                        r0 += z * XCH
                        cx.dma("sp", Rb[z][:], xs_dst[ci][r0:r0 + 256, :].rearrange("(c p) f -> p c f", p=128), [], [b_Rb[z]], ld)
                        cx.op("dve", lambda: V.tensor_scalar(R[z][:], Rb[z][:], flags[:, z:z + 1], None, ALU.mult), [b_Rb[z], b_c2], [b_R[z]])
                    for n in range(NT):
                        csl = slice(n * 128, (n + 1) * 128)
                        cx.op("dve", lambda: V.tensor_copy(out=Rb[0][:], in_=R[0][:]), [b_R[0]], [b_Rb[0]])
                        ps, pb = next_ps()
                        cx.mm(ps[:, 0:128], [(kh[0][:, d_, csl], qh[0][:, d_, csl]) for d_ in range(2)], [b_kh[0], b_qh[0]], pb)
                        cx.mm(ps[:, 128:256], [(kh[1][:, d_, csl], qh[1][:, d_, csl]) for d_ in range(2)], [b_kh[1], b_qh[1]], pb)
                        cx.op("dve", lambda: V.tensor_tensor(out=pt1[:], in0=ps[:, 0:128], in1=maskF[:], op=ALU.mult), [pb, b_c2], [b_pt])
                        cx.op("dve", lambda: V.tensor_tensor(out=pt2[:], in0=ps[:, 128:256], in1=maskB[:], op=ALU.mult), [pb, b_c2], [b_pt])
                        cx.op("dve", lambda: V.tensor_tensor(out=PT[:], in0=pt1[:], in1=pt2[:], op=ALU.add), [b_pt], [b_PT])
                        ps2, pb2 = next_ps()
                        cx.mm(ps2[:, 0:512], [(PT[:], vv[:, n, :])] + [(qh[0][:, d_, csl], Rb[0][:, d_, :]) for d_ in range(2)],
                              [b_PT, b_vv, b_qh[0], b_Rb[0]], pb2)
                        cx.op("act", lambda: A.copy(out=o_acc[:, n, :], in_=ps2[:, 0:512]), [pb2], [b_o])
                        kv_update(0, n, True)
                    for n in range(NT - 1, -1, -1):
                        csl = slice(n * 128, (n + 1) * 128)
                        scale_state(1, n)
                        cx.op("dve", lambda: V.tensor_copy(out=Rb[1][:], in_=R[1][:]), [b_R[1]], [b_Rb[1]])
                        ps2, pb2 = next_ps()
                        cx.mm(ps2[:, 0:512], [(qh[1][:, d_, csl], Rb[1][:, d_, :]) for d_ in range(2)], [b_qh[1], b_Rb[1]], pb2)
                        cx.op("dve", lambda: V.tensor_tensor(out=yn[:], in0=ps2[:, 0:512], in1=o_acc[:, n, :], op=ALU.add), [pb2, b_o], [b_yn])
                        kv_update(1, n, False)
                        c = lambda i: stt[:, i:i + 1]
                        cx.op("act", lambda: A.activation(out=junk2[:], in_=yn[:], func=AF.Identity, accum_out=c(0)), [b_yn], [b_j2, b_stt])
                        cx.op("act", lambda: A.activation(out=junk2[:], in_=yn[:], func=AF.Square, accum_out=c(1)), [b_yn], [b_j2, b_stt])
                        cx.op("dve", lambda: V.tensor_scalar(c(2), c(0), 1.0 / 512, None, ALU.mult), [b_stt], [b_stt])
                        cx.op("dve", lambda: V.tensor_scalar(c(3), c(1), 1.0 / 512, EPS, ALU.mult, ALU.add), [b_stt], [b_stt])
                        if b == 0:
                            cx.op("dve", lambda: V.tensor_tensor(out=c(4), in0=c(2), in1=c(2), op=ALU.mult), [b_stt], [b_stt])
                            cx.op("dve", lambda: V.tensor_tensor(out=c(3), in0=c(3), in1=c(4), op=ALU.subtract), [b_stt], [b_stt])
                        cx.op("act", lambda: A.activation(out=c(5), in_=c(3), func=AF.Sqrt), [b_stt], [b_stt])
                        cx.op("dve", lambda: V.reciprocal(c(6), c(5)), [b_stt], [b_stt])
                        if b == 0:
                            cx.op("dve", lambda: V.tensor_scalar(yn[:], yn[:], c(2), c(6), ALU.subtract, ALU.mult), [b_stt, b_yn], [b_yn])
                        else:
                            cx.op("dve", lambda: V.tensor_scalar(yn[:], yn[:], c(6), None, ALU.mult), [b_stt, b_yn], [b_yn])
                        i = sgi[0] % 2
                        sgi[0] += 1
                        cx.dma("sp", sgc[i][:], sgtm[b][n * 128:(n + 1) * 128, h * 512:(h + 1) * 512], [b_sg[b]], [b_sgc[i]], s_sgc[i])
                        cx.op("dve", lambda: V.tensor_tensor(out=yn[:], in0=yn[:], in1=gn[:], op=ALU.mult), [b_yn, b_gn], [b_yn])
                        cx.op("dve", lambda: V.tensor_tensor(out=ynb[:], in0=yn[:], in1=sgc[i][:], op=ALU.mult), [b_yn, b_sgc[i]], [b_yn])
                        ps3, pb3 = next_ps()
                        pv = ps3[:].bitcast(BF16)
                        cx.mm_multi([(lambda cc=cc: nc.tensor.transpose(pv[:, cc * 128:(cc + 1) * 128], ynb[:, cc * 128:(cc + 1) * 128], ident[:])) for cc in range(4)],
                                    [b_yn, b_const], pb3)
                        cx.op("act", lambda: A.copy(out=brs[:, :, csl], in_=pv[:, 0:512].rearrange("p (a b) -> p a b", a=4)), [pb3], [b_brs])
                    cx.dma("sp", branchT[b][h * 4:(h + 1) * 4].rearrange("a p t -> p a t"), brs[:], [b_brs], [b_brT[b]], stB, partial=True)
        cx.barrier()
        if stage < 4:
            fin = cx.semctr("fin")
            cx.dma("sp", out[0:128, 0:128], ident_in, b_brT, [], fin)
            nc.sync.wait_ge(fin.sem, fin.n)
            return nc
        w_branch = din("w_branch", [2, D, D])
        w_out = din("w_out", [D, D])
        norm_ffn = din("norm_ffn", [1, D])
        m0T = scr("m0T", [KC, 128, T])
        mergedT = scr("mergedT", [KC, 128, T])
        h1 = scr("h1", [T, D], F32)
        xn2tm = scr("xn2tm", [T, D])
        b_m0 = Buf()
        b_mT = Buf()
        b_h1 = Buf()
        b_xn2tm = Buf()
        V = nc.vector
        A = nc.scalar
        G = nc.gpsimd
        esX = es.enter_context(ExitStack())
        XT = esX.enter_context(nc.sbuf_tensor("AT3", [128, KC, T], BF16))
        b_XT = Buf("AT3")
        ldx = cx.semctr("s_ldx")

        def load_XT(src, src_buf):
            for g in range(4):
                cx.dma("sp", XT[:, g * 8:(g + 1) * 8, :], src[g * 8:(g + 1) * 8].rearrange("k p t -> p k t"), [src_buf], [b_XT], ldx, partial=(g > 0))

        def w_loader(ring):
            def load_w(W, c0):
                wt, wb, ws = ring.next()
                cx.dma("pool", wt[:], W[:, c0:c0 + 256].rearrange("(kc p) c -> p kc c", p=128), [], [wb], ws)
                return wt, wb
            return load_w

        with ExitStack() as es3:
            sb3 = lambda name, shape, d: es3.enter_context(nc.sbuf_tensor(name, list(shape), d))
            load_w = w_loader(Ring(cx, "w3", 2, [128, KC, 256], BF16, es3))
            with ExitStack() as es3a:
                sb3a = lambda name, shape, d: es3a.enter_context(nc.sbuf_tensor(name, list(shape), d))
                sring = Ring(cx, "stg3", 2, [128, 2, T], BF16, es3a)
                sbg1 = sb3a("sbg1", [128, 2, T], BF16)
                b_sbg1 = Buf()
                s_sbg1 = cx.semctr("s_sbg1")
                m0b = sb3a("m0b", [128, 2, T], BF16)
                b_m0b = Buf()
                s_m0b = cx.semctr("s_m0b")
                gtmp = sb3a("gtmp", [128, 512], F32)
                b_gtmp = Buf()
                for b in range(2):
                    load_XT(branchT[b], b_brT[b])
                    for blk in range(16):
                        wt, wb = load_w(w_branch[b], blk * 256)
                        cx.dma("sp", sbg1[:], sbgT[b * KC + 2 * blk:b * KC + 2 * blk + 2].rearrange("a p t -> p a t"), [b_sbgT], [b_sbg1], s_sbg1)
                        if b == 1:
                            cx.dma("sp", m0b[:], m0T[2 * blk:2 * blk + 2].rearrange("a p t -> p a t"), [b_m0], [b_m0b], s_m0b)
                        stt_, stb, sts = sring.next()
                        for tb in range(4):
                            tsl = slice(tb * 512, (tb + 1) * 512)
                            for dch in range(2):
                                ps, pb = next_ps()
                                cx.mm(ps[:, 0:512], [(wt[:, k, dch * 128:(dch + 1) * 128], XT[:, k, tsl]) for k in range(KC)], [wb, b_XT], pb)
                                if b == 0:
                                    cx.op("dve", lambda: V.tensor_tensor(out=stt_[:, dch, tsl], in0=ps[:, 0:512], in1=sbg1[:, dch, tsl], op=ALU.mult), [pb, b_sbg1], [stb])
                                else:
                                    cx.op("dve", lambda: V.tensor_tensor(out=gtmp[:], in0=ps[:, 0:512], in1=sbg1[:, dch, tsl], op=ALU.mult), [pb, b_sbg1], [b_gtmp])
                                    cx.op("pool", lambda: G.tensor_tensor(out=stt_[:, dch, tsl], in0=gtmp[:], in1=m0b[:, dch, tsl], op=ALU.add), [b_gtmp, b_m0b], [stb])
                        dstT, dstB = (m0T, b_m0) if b == 0 else (mergedT, b_mT)
                        cx.dma("sp", dstT[2 * blk:2 * blk + 2].rearrange("a p t -> p a t"), stt_[:], [stb], [dstB], sts, partial=True)
                cx.barrier()
            with ExitStack() as es3b:
                sb3b = lambda name, shape, d: es3b.enter_context(nc.sbuf_tensor(name, list(shape), d))
                xblk = sb3b("xblk", [128, NT, 256], F32)
                b_xblk = Buf()
                s_xblk = cx.semctr("s_xblk")
                h1s = sb3b("h1s", [128, NT, 256], F32)
                b_h1s = Buf()
                s_h1s = cx.semctr("s_h1s")
                load_XT(mergedT, b_mT)
                for blk in range(16):
                    csl = slice(blk * 256, (blk + 1) * 256)
                    wt, wb = load_w(w_out, blk * 256)
                    cx.dma("sp", xblk[:], x[:, csl].rearrange("(t p) c -> p t c", p=128), [], [b_xblk], s_xblk)
                    for t in range(NT):
                        ps, pb = next_ps()
                        cx.mm(ps[:, 0:256], [(XT[:, k, t * 128:(t + 1) * 128], wt[:, k, :]) for k in range(KC)], [wb, b_XT], pb)
                        cx.op("dve", lambda: V.tensor_tensor(out=h1s[:, t, :], in0=ps[:, 0:256], in1=xblk[:, t, :], op=ALU.add), [pb, b_xblk], [b_h1s])
                    cx.dma("sp", h1[:, csl].rearrange("(t p) c -> p t c", p=128), h1s[:], [b_h1s], [b_h1], s_h1s, partial=True)
                cx.barrier()
        norm_T("n2", h1, [b_h1], norm_ffn[0], XT, b_XT, tm_dst=xn2tm, b_tm=b_xn2tm)
        if stage < 5:
            fin = cx.semctr("fin")
            cx.dma("sp", out[0:128, 0:128], ident_in, [b_xn2tm, b_h1], [], fin)
            nc.sync.wait_ge(fin.sem, fin.n)
            return nc
        w_router = din("w_router", [D, 16])
        CAP = 512
        xa_src = dt("xa_src", [16, T], F32).ap()
        xa_dst = dt("xa_dst", [32, T], F32).ap()
        b_xa = Buf()
        es4 = es.enter_context(ExitStack())
        sb4 = lambda name, shape, d: es4.enter_context(nc.sbuf_tensor(name, list(shape), d))
        rkT_d = scr("rkT_d", [16, T], F32)
        rktm_d = scr("rktm_d", [128, NT * 16], F32)
        gtm_d = scr("gtm_d", [128, NT * 16], BF16)
        b_rt = Buf()
        with ExitStack() as es4a:
            sb4a = lambda name, shape, d: es4a.enter_context(nc.sbuf_tensor(name, list(shape), d))
            identf = sb4a("identf4", [128, 128], F32)
            b_c4 = Buf()
            cx.dma("sp", identf[:], ident_in, [], [b_c4], setup)
            wr = sb4a("wr", [128, KC, 16], BF16)
            cx.dma("pool", wr[:], w_router.rearrange("(kc p) e -> p kc e", p=128), [], [b_c4], setup, partial=True)
            aff = sb4a("aff", [128, NT, 16], F32)
            b_aff = Buf()
            sm4 = sb4a("sm4", [128, 4 * NT], F32)
            b_sm4 = Buf()
            affT = sb4a("affT", [16, T], F32)
            b_affT = Buf()
            for t in range(NT):
                ps, pb = next_ps()
                cx.mm(ps[:, 0:16], [(XT[:, k, t * 128:(t + 1) * 128], wr[:, k, :]) for k in range(KC)], [b_XT, b_c4], pb)
                c = lambda i: sm4[:, 4 * t + i:4 * t + i + 1]
                cx.op("dve", lambda: V.reduce_max(out=c(0), in_=ps[:, 0:16], axis=AX.X), [pb], [b_sm4])
                cx.op("dve", lambda: V.tensor_scalar(c(1), c(0), -1.0, None, ALU.mult), [b_sm4], [b_sm4])
                cx.op("act", lambda: A.activation(out=aff[:, t, :], in_=ps[:, 0:16], func=AF.Exp, bias=c(1), accum_out=c(2)), [pb, b_sm4], [b_aff, b_sm4])
                cx.op("dve", lambda: V.reciprocal(c(3), c(2)), [b_sm4], [b_sm4])
                cx.op("dve", lambda: V.tensor_scalar(aff[:, t, :], aff[:, t, :], c(3), None, ALU.mult), [b_sm4, b_aff], [b_aff])
            for g in range(4):
                ps, pb = next_ps()
                cx.mm_multi([(lambda j=j: nc.tensor.transpose(ps[0:16, j * 128:(j + 1) * 128], aff[:, g * 4 + j, :], identf[:])) for j in range(4)], [b_aff, b_c4], pb)
                cx.op("act", lambda: A.copy(out=affT[:, g * 512:(g + 1) * 512], in_=ps[0:16, 0:512]), [pb], [b_affT])
            s_xa = cx.semctr("s_xa")
            cx.dma("sp", xa_src, affT[:], [b_affT], [b_xa], s_xa)
            cx.deps("pool", [b_xa], [])
            ccs2 = cx.sem("ccsem2")
            nc.gpsimd.collective_compute("AllGather", ALU.bypass, replica_groups=[[2 * r, 2 * r + 1] for r in range(NCORES // 2)],
                                         ins=[xa_src.opt()], outs=[xa_dst.opt()]).then_inc(ccs2)
            for e_ in ("pool", "sp"):
                cx.eng[e_].wait_ge(ccs2, 1)
            work = sb4a("work", [16, 2, T], F32)
            b_work = Buf()
            cx.dma("sp", work[:], xa_dst.rearrange("(r e) t -> e r t", e=16), [], [b_work], s_xa)
            m8 = sb4a("m8", [16, 8], F32)
            b_m8 = Buf()
            workf = work[:].rearrange("e r t -> e (r t)")
            for it in range(CAP // 8):
                cx.op("dve", lambda: V.max(out=m8[:], in_=workf), [b_work], [b_m8])
                if it < CAP // 8 - 1:
                    cx.op("dve", lambda: V.match_replace(out=workf, in_to_replace=m8[:], in_values=workf, imm_value=-1.0), [b_m8, b_work], [b_work])
            maskT = sb4a("maskT", [16, T], F32)
            cntT = sb4a("cntT", [16, T], F32)
            onesT = sb4a("onesT", [16, T], F32)
            rkT = sb4a("rkT", [16, T], F32)
            b_mk = Buf()
            cx.op("pool", lambda: G.memset(onesT[:], 1.0), [], [b_mk])
            cx.op("dve", lambda: V.tensor_scalar(maskT[:], affT[:], m8[:, 7:8], None, ALU.is_ge), [b_affT, b_m8], [b_mk])
            cx.op("dve", lambda: V.tensor_tensor_scan(out=cntT[:], data0=onesT[:], data1=maskT[:], initial=0.0, op0=ALU.mult, op1=ALU.add), [b_mk], [b_mk])
            cx.op("dve", lambda: V.tensor_tensor(out=cntT[:], in0=cntT[:], in1=maskT[:], op=ALU.mult), [b_mk], [b_mk])
            cx.op("dve", lambda: V.tensor_scalar(rkT[:], cntT[:], -1.0, None, ALU.add), [b_mk], [b_mk])
            rktm = sb4a("rktm", [128, NT, 16], F32)
            mtm = sb4a("mtm", [128, NT, 16], F32)
            gtm = sb4a("gtm", [128, NT, 16], BF16)
            b_rk = Buf()
            ps, pb = next_ps()
            cx.mm_multi([(lambda t=t: nc.tensor.transpose(ps[:, t * 16:(t + 1) * 16], rkT[:, t * 128:(t + 1) * 128], identf[0:16, 0:16])) for t in range(NT)], [b_mk, b_c4], pb)
            cx.op("act", lambda: A.copy(out=rktm[:].rearrange("p t e -> p (t e)"), in_=ps[:, 0:256]), [pb], [b_rk])
            cx.op("dve", lambda: V.tensor_single_scalar(mtm[:], rktm[:], 0.0, ALU.is_ge), [b_rk], [b_rk])
            cx.op("dve", lambda: V.tensor_tensor(out=gtm[:], in0=aff[:], in1=mtm[:], op=ALU.mult), [b_rk, b_aff], [b_rk])
            cx.dma("sp", rkT_d, rkT[:], [b_mk], [b_rt], s_xa, partial=True)
            cx.dma("sp", rktm_d, rktm[:].rearrange("p t e -> p (t e)"), [b_rk], [b_rt], s_xa, partial=True)
            cx.dma("sp", gtm_d, gtm[:].rearrange("p t e -> p (t e)"), [b_rk], [b_rt], s_xa, partial=True)
            cx.barrier()
        es4.close()
        esX.close()
        if stage < 6:
            fin = cx.semctr("fin")
            cx.dma("sp", out[0:128, 0:128], ident_in, [b_rt], [], fin)
            nc.sync.wait_ge(fin.sem, fin.n)
            return nc
        weg = din("w_expert_gate", [16, D, 2048])
        weu = din("w_expert_up", [16, D, 2048])
        wed = din("w_expert_down", [16, 2048, D])
        b_h2 = [[Buf() for _ in range(8)] for _ in range(NT)]
        with ExitStack() as es5:
            sb5 = lambda name, shape, d: es5.enter_context(nc.sbuf_tensor(name, list(shape), d))
            b_c5 = Buf()
            rkT = sb5("rkT5", [16, T], F32)
            rktm = sb5("rktm5", [128, NT, 16], F32)
            gtm = sb5("gtm5", [128, NT, 16], BF16)
            cx.dma("sp", rkT[:], rkT_d, [b_rt], [b_c5], setup)
            cx.dma("sp", rktm[:].rearrange("p t e -> p (t e)"), rktm_d, [b_rt], [b_c5], setup, partial=True)
            cx.dma("sp", gtm[:].rearrange("p t e -> p (t e)"), gtm_d, [b_rt], [b_c5], setup, partial=True)
            iota_r = sb5("iota_r", [128, CAP], F32)
            cx.dma("sp", iota_r[:], din("iota_row", [1, CAP])[0].partition_broadcast(128), [], [b_c5], setup, partial=True)
            jv = sb5("jv", [128, 4], F32)
            cx.dma("sp", jv[:], din("jvals", [128, 4]), [], [b_c5], setup, partial=True)
            selc = sb5("selc_s", [16, 16, 128], F32)
            cx.dma("sp", selc[:], din("selc", [16, 16, 128]), [], [b_c5], setup, partial=True)
            xring = Ring(cx, "xn2h", 2, [128, NT, 512], BF16, es5)
            wring = Ring(cx, "w5", 4, [128, KC * 256], BF16, es5)
            Pm = sb5("Pm", [128, NT * CAP], BF16)
            b_P = Buf()
            Pv = Pm[:].rearrange("p (t j) -> p t j", t=NT)
            PTv = Pm[:].rearrange("p (c t) -> p c t", c=4)
            xsT = sb5("xsT", [128, KC, CAP], BF16)
            b_xs = Buf()
            hidT = sb5("hidT", [128, 16, CAP], BF16)
            b_hid = Buf()
            ysel = sb5("ysel", [128, 4, 512], BF16)
            b_ys = Buf()
            gsel = sb5("gsel", [128, 4], F32)
            b_gs = Buf()
            stmp = sb5("stmp", [128, 512], F32)
            b_stmp = Buf()
            dtmp = sb5("dtmp", [128, 512], F32)
            b_dtmp = Buf()
            yring = Ring(cx, "yst", 2, [128, 512], F32, es5)
            nexp = 16 if (debug is None or stage >= 7) else 2
            wblocks = []
            for e in range(nexp):
                for blk in range(8):
                    wblocks.append(("g", e, blk))
                    wblocks.append(("u", e, blk))
                for cb in range(8):
                    wblocks.append(("d", e, cb))
            wloaded = []
            wstate = {"issued": 0, "used": 0}

            def w_issue_to(k):
                while wstate["issued"] < min(k, len(wblocks)):
                    kind, e_, i_ = wblocks[wstate["issued"]]
                    j_ = wstate["issued"]
                    if j_ >= 2:
                        cx._wait("pool", wloaded[j_ - 2][2], wloaded[j_ - 2][3])
                    wt, wb, wsm = wring.next()
                    if kind == "d":
                        cx.dma("pool", wt[:].rearrange("p (f c) -> p f c", f=16), wed[e_][:, i_ * 512:(i_ + 1) * 512].rearrange("(f p) c -> p f c", p=128), [], [wb], wsm)
                    else:
                        Wm = weg if kind == "g" else weu
                        cx.dma("pool", wt[:].rearrange("p (k c) -> p k c", k=KC), Wm[e_][:, i_ * 256:(i_ + 1) * 256].rearrange("(kc p) c -> p kc c", p=128), [], [wb], wsm)
                    wloaded.append((wt, wb, wsm.sem, wsm.n))
                    wstate["issued"] += 1

            def w_take(n):
                i = wstate["used"]
                wstate["used"] += n
                w_issue_to(i + 4)
                return wloaded[i:i + n]

            for e in range(nexp):
                for t in range(NT):
                    cx.op("dve", lambda: V.tensor_scalar(Pv[:, t, :], iota_r[:], rktm[:, t, e:e + 1], None, ALU.is_equal), [b_c5], [b_P])
                ps, pb = next_ps()
                for jc in range(4):
                    cx.mm(ps[:, jc:jc + 1], [(Pv[:, t, jc * 128:(jc + 1) * 128], gtm[:, t, e:e + 1]) for t in range(NT)], [b_P, b_c5], pb)
                cx.op("act", lambda: A.copy(out=gsel[:], in_=ps[:, 0:4]), [pb], [b_gs])
                for q8 in range(8):
                    xn2h, b_xh, s_xh = xring.next()
                    cx.dma("sp", xn2h[:], xn2tm[:, q8 * 512:(q8 + 1) * 512].rearrange("(t p) c -> p t c", p=128), [b_xn2tm], [b_xh], s_xh)
                    for dc in range(4):
                        ps, pb = next_ps()
                        cx.mm(ps[:, 0:CAP], [(xn2h[:, t, dc * 128:(dc + 1) * 128], Pv[:, t, :]) for t in range(NT)], [b_xh, b_P], pb)
                        if dc % 2 == 0:
                            cx.op("act", lambda: A.copy(out=xsT[:, q8 * 4 + dc, :], in_=ps[:, 0:CAP]), [pb], [b_xs])
                        else:
                            cx.op("dve", lambda: V.tensor_copy(out=xsT[:, q8 * 4 + dc, :], in_=ps[:, 0:CAP]), [pb], [b_xs])
                for blk in range(8):
                    ws_ = [(wt[:].rearrange("p (k c) -> p k c", k=KC), wb) for (wt, wb, _s, _n) in w_take(2)]
                    for fs in range(2):
                        pss = []
                        for (wv, wb) in ws_:
                            ps, pb = next_ps()
                            cx.mm(ps[:, 0:CAP], [(wv[:, k, fs * 128:(fs + 1) * 128], xsT[:, k, :]) for k in range(KC)], [wb, b_xs], pb)
                            pss.append((ps, pb))
                        cx.op("act", lambda: A.activation(out=stmp[:], in_=pss[0][0][:, 0:CAP], func=AF.Silu), [pss[0][1]], [b_stmp])
                        cx.op("dve", lambda: V.tensor_tensor(out=hidT[:, blk * 2 + fs, :], in0=stmp[:], in1=pss[1][0][:, 0:CAP], op=ALU.mult), [b_stmp, pss[1][1]], [b_hid])
                for tb in range(4):
                    tsl = slice(tb * 512, (tb + 1) * 512)
                    ps, pb = next_ps()
                    cx.mm(ps[:, 0:512], [(selc[:, e, :], rkT[:, tsl])], [b_c5], pb)
                    for jc in range(4):
                        cx.op("dve", lambda: V.tensor_scalar(dtmp[:], ps[:, 0:512], jv[:, jc:jc + 1], None, ALU.subtract), [pb, b_c5], [b_dtmp])
                        cx.op("act", lambda: A.activation(out=dtmp[:], in_=dtmp[:], func=AF.Square), [b_dtmp], [b_dtmp])
                        cx.op("dve", lambda: V.tensor_single_scalar(PTv[:, jc, tsl], dtmp[:], 0.25, ALU.is_lt), [b_dtmp], [b_P])
                for cb in range(8):
                    csl = slice(cb * 512, (cb + 1) * 512)
                    (wt, wb, _s, _n), = w_take(1)
                    wv = wt[:].rearrange("p (f c) -> p f c", f=16)
                    for jc in range(4):
                        ps, pb = next_ps()
                        cx.mm(ps[:, 0:512], [(hidT[:, f, jc * 128:(jc + 1) * 128], wv[:, f, :]) for f in range(16)], [b_hid, wb], pb)
                        cx.op("act", lambda: A.activation(out=ysel[:, jc, :], in_=ps[:, 0:512], func=AF.Copy, scale=gsel[:, jc:jc + 1]), [pb, b_gs], [b_ys])
                    for t in range(NT):
                        ps, pb = next_ps()
                        cx.mm(ps[:, 0:512], [(PTv[:, jc, t * 128:(t + 1) * 128], ysel[:, jc, :]) for jc in range(4)], [b_P, b_ys], pb)
                        yt, yb, ysm = yring.next()
                        if t % 2 == 0:
                            cx.op("act", lambda: A.copy(out=yt[:], in_=ps[:, 0:512]), [pb], [yb])
                        else:
                            cx.op("dve", lambda: V.tensor_copy(out=yt[:], in_=ps[:, 0:512]), [pb], [yb])
                        cx.dma("pool", h1[t * 128:(t + 1) * 128, csl], yt[:], [yb, b_h1], [b_h2[t][cb]], ysm, accum_op=ALU.add)
            cx.barrier()
        if stage < 7:
            fin = cx.semctr("fin")
            cx.dma("sp", out[0:128, 0:128], ident_in, [], [], fin)
            nc.sync.wait_ge(fin.sem, fin.n)
            return nc
        norm_ple = din("norm_ple", [1, D])
        w_pg = din("w_ple_gate", [D, D])
        w_pp = din("w_ple_proj", [256, D])
        p_in = din("p", [T, 256])
        norm_final = din("norm_final", [1, D])
        h3 = scr("h3", [T, D], F32)
        b_h3 = Buf()
        esX = es.enter_context(ExitStack())
        XT = esX.enter_context(nc.sbuf_tensor("AT6", [128, KC, T], BF16))
        b_XT = Buf("AT6")
        norm_T("n3", h1, [], norm_ple[0], XT, b_XT)
        with ExitStack() as es6:
            sb6 = lambda name, shape, d: es6.enter_context(nc.sbuf_tensor(name, list(shape), d))
            b_c6 = Buf()
            wpp = sb6("wpp", [128, 2, D], BF16)
            cx.dma("pool", wpp[:], w_pp.rearrange("(k p) c -> p k c", p=128), [], [b_c6], setup)
            pT = sb6("pT", [128, 2, T], BF16)
            b_pT = Buf()
            h2b = sb6("h2b", [128, NT, 256], F32)
            b_h2b = Buf()
            s_h2b = cx.semctr("s_h2b")
            with ExitStack() as es6p:
                ptm = es6p.enter_context(nc.sbuf_tensor("ptm", [128, NT, 256], F32))
                pbf = es6p.enter_context(nc.sbuf_tensor("pbf", [128, NT, 256], BF16))
                b_pp = Buf()
                cx.dma("sp", ptm[:], p_in.rearrange("(t p) c -> p t c", p=128), [], [b_pp], setup)
                cx.op("dve", lambda: V.tensor_copy(out=pbf[:], in_=ptm[:]), [b_pp], [b_pp])
                for t in range(NT):
                    ps, pb = next_ps()
                    pv = ps[:].bitcast(BF16)
                    cx.mm_multi([(lambda k2=k2: nc.tensor.transpose(pv[:, k2 * 128:(k2 + 1) * 128], pbf[:, t, k2 * 128:(k2 + 1) * 128], ident[:])) for k2 in range(2)], [b_pp, b_const], pb)
                    cx.op("act", lambda: A.copy(out=pT[:, :, t * 128:(t + 1) * 128], in_=pv[:, 0:256].rearrange("p (a b) -> p a b", a=2)), [pb], [b_pT])
                cx.barrier()
            h3s = h2b
            b_h3s = b_h2b
            s_h3s = s_h2b
            load_w = w_loader(Ring(cx, "w6", 2, [128, KC, 256], BF16, es6))
            sgt = sb6("sgt", [128, 256], F32)
            b_sgt = Buf()
            t1t = sb6("t1t", [128, 256], F32)
            b_t1 = Buf()
            for blk in range(16):
                csl = slice(blk * 256, (blk + 1) * 256)
                wt, wb = load_w(w_pg, blk * 256)
                cx.dma("sp", h2b[:], h1[:, csl].rearrange("(t p) c -> p t c", p=128), [], [b_h2b], s_h2b)
                for t in range(NT):
                    tsl = slice(t * 128, (t + 1) * 128)
                    ps, pb = next_ps()
                    cx.mm(ps[:, 0:256], [(XT[:, k, tsl], wt[:, k, :]) for k in range(KC)], [wb, b_XT], pb)
                    ps2, pb2 = next_ps()
                    cx.mm(ps2[:, 0:256], [(pT[:, k2, tsl], wpp[:, k2, csl]) for k2 in range(2)], [b_pT, b_c6], pb2)
                    cx.op("act", lambda: A.activation(out=sgt[:], in_=ps[:, 0:256], func=AF.Sigmoid), [pb], [b_sgt])
                    cx.op("dve", lambda: V.tensor_tensor(out=t1t[:], in0=sgt[:], in1=ps2[:, 0:256], op=ALU.mult), [b_sgt, pb2], [b_t1])
                    cx.op("pool", lambda: G.tensor_tensor(out=h3s[:, t, :], in0=t1t[:], in1=h2b[:, t, :], op=ALU.add), [b_t1, b_h2b], [b_h3s])
                cx.dma("sp", h3[:, csl].rearrange("(t p) c -> p t c", p=128), h3s[:], [b_h3s], [b_h3], s_h3s, partial=True)
            cx.barrier()
        esX.close()
        fin = cx.semctr("fin")
        with ExitStack() as es7:
            sb7 = lambda name, shape, d: es7.enter_context(nc.sbuf_tensor(name, list(shape), d))
            gain = sb7("gainF", [128, D], F32)
            b_g = Buf()
            cx.dma("sp", gain[:], norm_final[0].partition_broadcast(128), [], [b_g], setup)
            xr_t = [sb7(f"fr{i}", [128, D], F32) for i in range(2)]
            xr_b = [Buf() for _ in range(2)]
            xr_s = [cx.semctr(f"s_fr{i}") for i in range(2)]
            yo_t = [sb7(f"fo{i}", [128, D], F32) for i in range(2)]
            yo_b = [Buf() for _ in range(2)]
            junk = sb7("junkF", [128, D], BF16)
            b_junk = Buf()
            st = sb7("stF", [128, 4 * NT], F32)
            b_st = Buf()
            for t in range(NT):
                xt, xb, xs = xr_t[t % 2], xr_b[t % 2], xr_s[t % 2]
                yo, yb = yo_t[t % 2], yo_b[t % 2]
                cx.dma("sp", xt[:], h3[t * 128:(t + 1) * 128, :], [b_h3], [xb], xs)
                c0 = st[:, 4 * t:4 * t + 1]
                c1 = st[:, 4 * t + 1:4 * t + 2]
                c2 = st[:, 4 * t + 2:4 * t + 3]
                cx.op("act", lambda: A.activation(out=junk[:], in_=xt[:], func=AF.Square, accum_out=c0), [xb], [b_junk, b_st])
                cx.op("dve", lambda: V.tensor_scalar(c1, c0, 1.0 / D, EPS, ALU.mult, ALU.add), [b_st], [b_st])
                cx.op("act", lambda: A.activation(out=c2, in_=c1, func=AF.Sqrt), [b_st], [b_st])
                cx.op("dve", lambda: V.reciprocal(c1, c2), [b_st], [b_st])
                cx.op("dve", lambda: V.scalar_tensor_tensor(out=yo[:], in0=xt[:], scalar=c1, in1=gain[:], op0=ALU.mult, op1=ALU.mult), [xb, b_st, b_g], [yb])
                cx.dma("sp", out[t * 128:(t + 1) * 128, :], yo[:], [yb], [], fin)
            nc.sync.wait_ge(fin.sem, fin.n)
            cx.barrier()
    return nc


def make_consts():
    ident = np.eye(128, dtype=np.float32)
    invf = (10000.0 ** (-np.arange(128, dtype=np.float32) / np.float32(128))).astype(np.float32).reshape(128, 1)
    jj = np.arange(128)
    maskF = (jj[None, :] >= jj[:, None]).astype(np.float32)
    maskB = (jj[:, None] > jj[None, :]).astype(np.float32)
    rmask = (np.arange(T) % 128 != 0).astype(np.float32).reshape(1, T)
    iota_row = np.arange(512, dtype=np.float32).reshape(1, 512)
    jvals = (np.arange(128)[:, None] + 128 * np.arange(4)[None, :]).astype(np.float32)
    selc = np.zeros((16, 16, 128), np.float32)
    for e in range(16):
        selc[e, e, :] = 1.0
    return {"ident": ident, "invf": invf, "maskF": maskF, "maskB": maskB, "rmask": rmask, "iota_row": iota_row, "jvals": jvals, "selc": selc}


def make_in_maps(inputs, cores):
    f = lambda k: np.asarray(inputs[k], dtype=np.float32)
    x = f("x")
    positions = np.asarray(inputs["positions"], dtype=np.int32)
    p = f("p")[0]
    consts = make_consts()
    shared = {
        "norm_mix": f("norm_mix").reshape(1, D),
        "w_in": np.ascontiguousarray(f("w_in")[0]),
        "ret_decay_logit": f("ret_decay_logit").reshape(1, 16),
        "gla_gate_w": np.ascontiguousarray(f("gla_gate_w")[0]),
        "gla_gate_b": np.ascontiguousarray(f("gla_gate_b")[0]),
        "ret_norm": f("ret_norm").reshape(1, D),
        "gla_norm": f("gla_norm").reshape(1, D),
        "w_branch": np.ascontiguousarray(f("w_branch")[0]),
        "w_out": np.ascontiguousarray(f("w_out")[0]),
        "norm_ffn": f("norm_ffn").reshape(1, D),
        "w_router": np.ascontiguousarray(f("w_router")[0]),
        "w_expert_gate": np.ascontiguousarray(f("w_expert_gate")[0]),
        "w_expert_up": np.ascontiguousarray(f("w_expert_up")[0]),
        "w_expert_down": np.ascontiguousarray(f("w_expert_down")[0]),
        "norm_ple": f("norm_ple").reshape(1, D),
        "w_ple_gate": np.ascontiguousarray(f("w_ple_gate")[0]),
        "w_ple_proj": np.ascontiguousarray(f("w_ple_proj")[0]),
        "norm_final": f("norm_final").reshape(1, D),
    }
    in_maps = []
    for c in cores:
        b, hf = c // 2, c % 2
        sl = slice(hf * T, (hf + 1) * T)
        m = dict(consts)
        m.update(shared)
        m["x"] = np.ascontiguousarray(x[b, sl])
        m["pos"] = np.ascontiguousarray(positions[b, sl]).reshape(1, T)
        m["p"] = np.ascontiguousarray(p[b, sl])
        m["flags"] = np.array([[float(hf), float(1 - hf)]], np.float32)
        in_maps.append(m)
    return in_maps


def kernel(**inputs):
    n = NCORES
    in_maps = make_in_maps(inputs, list(range(n)))
    nc = build(stage=99)
    res = run_bass_kernel_spmd(nc, in_maps, core_ids=list(range(n)))
    outs = [np.asarray(r["out"], dtype=np.float32) for r in res.results]
    return np.stack(outs, 0).reshape(n // 2, 2 * T, D)
```

```python
import numpy as np
import ml_dtypes
from contextlib import ExitStack
import concourse.bass as bass
import concourse.mybir as mybir
from concourse.bass_utils import run_bass_kernel_spmd

F32 = mybir.dt.float32
BF16 = mybir.dt.bfloat16
I32 = mybir.dt.int32
AF = mybir.ActivationFunctionType
ALU = mybir.AluOpType
AX = mybir.AxisListType

T = 2048
NT = 16
D = 4096
KC = 32
INW = 32800
EPS = 1e-6
NH = 8
NCORES = 8


class Buf:
    __slots__ = ("w", "r", "name")

    def __init__(self, name=""):
        self.w = {}
        self.r = {}
        self.name = name


class SemCtr:
    def __init__(self, sem):
        self.sem = sem
        self.n = 0


class Ctx:
    def __init__(self, nc, es):
        self.nc = nc
        self.es = es
        self.eng = {"pe": nc.tensor, "act": nc.scalar, "dve": nc.vector, "pool": nc.gpsimd, "sp": nc.sync}
        self.esem = {k: self.sem("E_" + k) for k in ("pe", "act", "dve", "pool")}
        self.ecnt = {k: 0 for k in self.esem}
        self.waited = {k: {} for k in self.eng}
        self.nsem = 0
        self.all_sc = []

    def sem(self, name):
        return self.es.enter_context(self.nc.semaphore(name))

    def semctr(self, name):
        sc = SemCtr(self.sem(name))
        self.all_sc.append(sc)
        return sc

    def barrier(self):
        for e in self.eng:
            for k, sem in self.esem.items():
                if self.ecnt[k] > 0 and k != e:
                    self._wait(e, sem, self.ecnt[k])
            for sc in self.all_sc:
                if sc.n > 0:
                    self._wait(e, sc.sem, sc.n)

    def _wait(self, e, sem, val):
        if e == "pe" and sem is self.esem["pe"]:
            return
        w = self.waited[e]
        if w.get(sem, 0) >= val:
            return
        w[sem] = val
        self.eng[e].wait_ge(sem, val)

    def deps(self, e, reads, writes):
        for b in reads:
            for sem, v in b.w.items():
                self._wait(e, sem, v)
        for b in writes:
            for sem, v in b.w.items():
                self._wait(e, sem, v)
            for sem, v in b.r.items():
                self._wait(e, sem, v)

    def _record(self, sem, val, reads, writes, partial=False):
        for b in reads:
            b.r[sem] = val
        for b in writes:
            if partial:
                b.w[sem] = val
            else:
                b.w = {sem: val}
                b.r = {}

    def op(self, e, fn, reads=(), writes=()):
        self.deps(e, reads, writes)
        ins = fn()
        self.ecnt[e] += 1
        ins.then_inc(self.esem[e], 1)
        self._record(self.esem[e], self.ecnt[e], reads, writes)
        return ins

    def mm(self, out_ap, pairs, reads, out_buf, transpose=False):
        self.deps("pe", reads, [out_buf])
        n = len(pairs)
        ins = None
        for i, (l, r) in enumerate(pairs):
            ins = self.nc.tensor.matmul(out_ap, l, r, start=(i == 0), stop=(i == n - 1))
        self.ecnt["pe"] += 1
        ins.then_inc(self.esem["pe"], 1)
        self._record(self.esem["pe"], self.ecnt["pe"], reads, [out_buf])

    def mm_multi(self, fns, reads, out_buf):
        self.deps("pe", reads, [out_buf])
        ins = None
        for f in fns:
            ins = f()
        self.ecnt["pe"] += 1
        ins.then_inc(self.esem["pe"], 1)
        self._record(self.esem["pe"], self.ecnt["pe"], reads, [out_buf])

    def dma(self, q, out_ap, in_ap, reads, writes, sc, partial=False, **kw):
        self.deps(q, reads, [] if partial else writes)
        ins = self.eng[q].dma_start(out=out_ap, in_=in_ap, **kw)
        sc.n += 16
        ins.then_inc(sc.sem, 16)
        self._record(sc.sem, sc.n, reads, writes, partial)
        return ins

    def wait_all(self, e, bufs):
        self.deps(e, bufs, [])


class Ring:
    def __init__(self, cx, name, n, shape, dt, es=None):
        es = es or cx.es
        self.t = [es.enter_context(cx.nc.sbuf_tensor(f"{name}{i}", shape, dt)) for i in range(n)]
        self.b = [Buf(f"{name}{i}") for i in range(n)]
        self.s = [cx.semctr(f"s_{name}{i}") for i in range(n)]
        self.n = n
        self.i = 0

    def next(self):
        i = self.i % self.n
        self.i += 1
        return self.t[i], self.b[i], self.s[i]


def build(stage=99, debug=None):
    nc = bass.Bass("TRN2", target_bir_lowering=False)
    dt = nc.dram_tensor

    def din(name, shape, d=F32):
        return dt(name, list(shape), d, kind="ExternalInput").ap()

    x = din("x", [T, D])
    norm_mix = din("norm_mix", [1, D])
    ident_in = din("ident", [128, 128])
    invf_in = din("invf", [128, 1])
    if stage >= 1:
        pos = din("pos", [1, T], I32)
        w_in = din("w_in", [D, INW])
    out = dt("out", [T, D], F32, kind="ExternalOutput").ap()

    def scr(name, shape, d=BF16):
        kind = "ExternalOutput" if (debug and name in debug) else "Internal"
        return dt(name, list(shape), d, kind=kind).ap()

    qkT = [scr(f"qkT{b}", [NH, 2, 2, 128, T]) for b in range(2)]
    vtm = [scr(f"v{b}", [T, D]) for b in range(2)]
    sgtm = [scr(f"sg{b}", [T, D]) for b in range(2)]
    sbgT = scr("sbgT", [2 * KC, 128, T])
    glrT_d = scr("glrT", [2, 16, T], F32)
    b_qkT = [Buf() for _ in range(2)]
    b_v = [Buf() for _ in range(2)]
    b_sg = [Buf() for _ in range(2)]
    b_sbgT = Buf()
    b_glr = Buf()

    with ExitStack() as es:
        cx = Ctx(nc, es)
        sb = lambda name, shape, d: es.enter_context(nc.sbuf_tensor(name, list(shape), d))
        setup = cx.semctr("setup")
        b_const = Buf("const")
        ident = sb("identb", [128, 128], BF16)
        cx.dma("pool", ident[:], ident_in, [], [b_const], setup, partial=True)
        invf = sb("invf_s", [128, 1], F32)
        cx.dma("sp", invf[:], invf_in, [], [b_const], setup, partial=True)
        psb = [es.enter_context(nc.psum_tensor(f"ps{i}", [128, 512], F32)) for i in range(8)]
        b_ps = [Buf(f"ps{i}") for i in range(8)]
        psi = [0]

        def next_ps():
            i = psi[0] % 8
            psi[0] += 1
            return psb[i], b_ps[i]

        esX = es.enter_context(ExitStack())
        XT = esX.enter_context(nc.sbuf_tensor("AT", [128, KC, T], BF16))
        b_XT = Buf("AT")

        def norm_T(pfx, src, src_bufs, gain_row, XT_, b_XT_, tm_dst=None, b_tm=None):
            with ExitStack() as es0:
                sb0 = lambda name, shape, d: es0.enter_context(nc.sbuf_tensor(pfx + name, list(shape), d))
                gain = sb0("gain", [128, D], F32)
                b_g = Buf()
                cx.dma("sp", gain[:], gain_row.partition_broadcast(128), [], [b_g], setup)
                xr_t = [sb0(f"xr{i}", [128, D], F32) for i in range(2)]
                xr_b = [Buf() for _ in range(2)]
                xr_s = [cx.semctr(f"s_{pfx}xr{i}") for i in range(2)]
                junk = sb0("junk", [128, D], BF16)
                b_junk = Buf()
                xnb_t = [sb0(f"xnb{i}", [128, D], BF16) for i in range(2)]
                xnb_b = [Buf() for _ in range(2)]
                xnb_s = [cx.semctr(f"s_{pfx}xnb{i}") for i in range(2)]
                st = sb0("st", [128, 4 * NT], F32)
                b_st = Buf()
                for t in range(NT):
                    xt, xb, xs = xr_t[t % 2], xr_b[t % 2], xr_s[t % 2]
                    xnb, b_xnb, s_xnb = xnb_t[t % 2], xnb_b[t % 2], xnb_s[t % 2]
                    cx.dma("sp", xt[:], src[t * 128:(t + 1) * 128, :], src_bufs, [xb], xs)
                    c0 = st[:, 4 * t:4 * t + 1]
                    c1 = st[:, 4 * t + 1:4 * t + 2]
                    c2 = st[:, 4 * t + 2:4 * t + 3]
                    cx.op("act", lambda: nc.scalar.activation(out=junk[:], in_=xt[:], func=AF.Square, accum_out=c0), [xb], [b_junk, b_st])
                    cx.op("dve", lambda: nc.vector.tensor_scalar(c1, c0, 1.0 / D, EPS, ALU.mult, ALU.add), [b_st], [b_st])
                    cx.op("act", lambda: nc.scalar.activation(out=c2, in_=c1, func=AF.Sqrt), [b_st], [b_st])
                    cx.op("dve", lambda: nc.vector.reciprocal(c1, c2), [b_st], [b_st])
                    cx.op("dve", lambda: nc.vector.scalar_tensor_tensor(out=xnb[:], in0=xt[:], scalar=c1, in1=gain[:], op0=ALU.mult, op1=ALU.mult),
                          [xb, b_st, b_g], [b_xnb])
                    if tm_dst is not None:
                        cx.dma("sp", tm_dst[t * 128:(t + 1) * 128, :], xnb[:], [b_xnb], [b_tm], s_xnb, partial=True)
                    for g in range(4):
                        ps, pb = next_ps()
                        psv = ps[:].bitcast(BF16)
                        pst = psv[:, 0:1024].rearrange("p (a b) -> p a b", a=8)
                        cx.mm_multi([(lambda j=j: nc.tensor.transpose(pst[:, j, :], xnb[:, (g * 8 + j) * 128:(g * 8 + j + 1) * 128], ident[:])) for j in range(8)],
                                    [b_xnb, b_const], pb)
                        dst = XT_[:, g * 8:(g + 1) * 8, t * 128:(t + 1) * 128]
                        if g % 2 == 0:
                            cx.op("act", lambda: nc.scalar.copy(out=dst, in_=pst), [pb], [b_XT_])
                        else:
                            cx.op("dve", lambda: nc.vector.tensor_copy(out=dst, in_=pst), [pb], [b_XT_])
                cx.barrier()

        norm_T("n0", x, [], norm_mix[0], XT, b_XT)
        if debug == "XT":
            dbg = dt("dbg", [128, KC, T], BF16, kind="ExternalOutput").ap()
            fin = cx.semctr("fin")
            cx.dma("sp", dbg, XT[:], [b_XT], [], fin)
            nc.sync.wait_ge(fin.sem, fin.n)
            return nc
        cx.barrier()
        if stage < 1:
            return nc
        TWO_PI = float(2 * np.pi)
        PI = float(np.pi)
        with ExitStack() as es1:
            sb1 = lambda name, shape, d: es1.enter_context(nc.sbuf_tensor(name, list(shape), d))
            cosb = sb1("cosb", [128, T], BF16)
            sinb = sb1("sinb", [128, T], BF16)
            b_tab = Buf("tab")
            with ExitStack() as est:
                sbt = lambda name, shape, d: est.enter_context(nc.sbuf_tensor(name, list(shape), d))
                posi = sbt("posi", [128, T], I32)
                ang = sbt("ang", [128, T], F32)
                a2 = sbt("a2", [128, T], F32)
                ki = sbt("ki", [128, T], I32)
                kf = sbt("kf", [128, T], F32)
                msk = sbt("msk", [128, T], F32)
                b_t = Buf("tmp_tab")
                cx.dma("sp", posi[:], pos[0].partition_broadcast(128), [], [b_t], setup)
                V = nc.vector
                cx.op("dve", lambda: V.tensor_copy(out=ang[:], in_=posi[:]), [b_t], [b_t])
                cx.op("dve", lambda: V.tensor_scalar(ang[:], ang[:], invf[:, 0:1], None, ALU.mult), [b_t, b_const], [b_t])
                for which, dst in ((0, sinb), (1, cosb)):
                    cx.op("dve", lambda: V.tensor_scalar(a2[:], ang[:], (PI / 2 if which else 0.0), None, ALU.add), [b_t], [b_t])
                    cx.op("dve", lambda: V.tensor_scalar(kf[:], a2[:], 1.0 / TWO_PI, None, ALU.mult), [b_t], [b_t])
                    cx.op("dve", lambda: V.tensor_copy(out=ki[:], in_=kf[:]), [b_t], [b_t])
                    cx.op("dve", lambda: V.tensor_copy(out=kf[:], in_=ki[:]), [b_t], [b_t])
                    cx.op("dve", lambda: V.scalar_tensor_tensor(out=a2[:], in0=kf[:], scalar=-TWO_PI, in1=a2[:], op0=ALU.mult, op1=ALU.add), [b_t], [b_t])
                    cx.op("dve", lambda: V.tensor_single_scalar(msk[:], a2[:], PI, ALU.is_gt), [b_t], [b_t])
                    cx.op("dve", lambda: V.scalar_tensor_tensor(out=a2[:], in0=msk[:], scalar=-TWO_PI, in1=a2[:], op0=ALU.mult, op1=ALU.add), [b_t], [b_t])
                    cx.op("dve", lambda: V.tensor_single_scalar(msk[:], a2[:], -PI, ALU.is_lt), [b_t], [b_t])
                    cx.op("dve", lambda: V.scalar_tensor_tensor(out=a2[:], in0=msk[:], scalar=TWO_PI, in1=a2[:], op0=ALU.mult, op1=ALU.add), [b_t], [b_t])
                    cx.op("dve", lambda: V.tensor_scalar(a2[:], a2[:], PI, -PI, ALU.min, ALU.max), [b_t], [b_t])
                    cx.op("act", lambda: nc.scalar.activation(out=dst[:], in_=a2[:], func=AF.Sin), [b_t], [b_tab])
            cx.barrier()
            wring = Ring(cx, "w", 2, [128, KC, 256], BF16, es1)
            sring = Ring(cx, "stg", 2, [128, 4096], BF16, es1)
            ta = sb1("rot_a", [128, 512], F32)
            tb_ = sb1("rot_b", [128, 512], F32)
            b_rt = Buf("rot_tmp")
            glr_s = sb1("glr_s", [16, 2, T], F32)
            b_glrs = Buf()

            def load_w(c0, ncols):
                wt, wb, ws = wring.next()
                src = w_in[:, c0:c0 + ncols].rearrange("(kc p) c -> p kc c", p=128)
                cx.dma("pool", wt[:, :, 0:ncols], src, [], [wb], ws)
                return wt, wb

            def fm_block(c0, kind, scale, dst_ap, dst_buf):
                wt, wb = load_w(c0, 256)
                stt, stb, sts = sring.next()
                stv = stt[:].rearrange("p (a b) -> p a b", a=2)
                for tb in range(4):
                    tsl = slice(tb * 512, (tb + 1) * 512)
                    pss = []
                    for dch in range(2):
                        ps, pb = next_ps()
                        cx.mm(ps[:, 0:512], [(wt[:, k, dch * 128:(dch + 1) * 128], XT[:, k, tsl]) for k in range(KC)], [wb, b_XT], pb)
                        pss.append((ps, pb))
                    if kind == "rot":
                        (p1, b1), (p2, b2) = pss
                        V = nc.vector
                        cx.op("dve", lambda: V.tensor_tensor(out=ta[:], in0=p1[:, 0:512], in1=cosb[:, tsl], op=ALU.mult), [b1, b_tab], [b_rt])
                        cx.op("dve", lambda: V.tensor_tensor(out=tb_[:], in0=p2[:, 0:512], in1=sinb[:, tsl], op=ALU.mult), [b2, b_tab], [b_rt])
                        cx.op("dve", lambda: V.scalar_tensor_tensor(out=stv[:, 0, tsl], in0=ta[:], scalar=scale, in1=tb_[:], op0=ALU.mult, op1=ALU.subtract) if False else
                              V.tensor_tensor(out=ta[:], in0=ta[:], in1=tb_[:], op=ALU.subtract), [b_rt], [b_rt])
                        cx.op("act", lambda: nc.scalar.activation(out=stv[:, 0, tsl], in_=ta[:], func=AF.Copy, scale=scale), [b_rt], [stb])
                        cx.op("dve", lambda: V.tensor_tensor(out=tb_[:], in0=p1[:, 0:512], in1=sinb[:, tsl], op=ALU.mult), [b1, b_tab], [b_rt])
                        cx.op("dve", lambda: V.tensor_tensor(out=ta[:], in0=p2[:, 0:512], in1=cosb[:, tsl], op=ALU.mult), [b2, b_tab, stb], [b_rt])
                        cx.op("dve", lambda: V.tensor_tensor(out=ta[:], in0=ta[:], in1=tb_[:], op=ALU.add), [b_rt], [b_rt])
                        cx.op("act", lambda: nc.scalar.activation(out=stv[:, 1, tsl], in_=ta[:], func=AF.Copy, scale=scale), [b_rt], [stb])
                    else:
                        fn = AF.Sigmoid if kind == "sig" else AF.Copy
                        for dch, (ps, pb) in enumerate(pss):
                            cx.op("act", lambda: nc.scalar.activation(out=stv[:, dch, tsl], in_=ps[:, 0:512], func=fn, scale=scale), [pb], [stb])
                cx.dma("sp", dst_ap.rearrange("a p t -> p a t"), stv, [stb], [dst_buf], sts, partial=True)

            def tm_block(c0, kind, dst_ap, dst_buf):
                wt, wb = load_w(c0, 256)
                stt, stb, sts = sring.next()
                stv = stt[:].rearrange("p (a b) -> p a b", a=NT)
                for t in range(NT):
                    ps, pb = next_ps()
                    cx.mm(ps[:, 0:256], [(XT[:, k, t * 128:(t + 1) * 128], wt[:, k, :]) for k in range(KC)], [wb, b_XT], pb)
                    if kind == "silu":
                        cx.op("act", lambda: nc.scalar.activation(out=stv[:, t, :], in_=ps[:, 0:256], func=AF.Silu), [pb], [stb])
                    elif t % 2 == 0:
                        cx.op("act", lambda: nc.scalar.copy(out=stv[:, t, :], in_=ps[:, 0:256]), [pb], [stb])
                    else:
                        cx.op("dve", lambda: nc.vector.tensor_copy(out=stv[:, t, :], in_=ps[:, 0:256]), [pb], [stb])
                cx.dma("sp", dst_ap.rearrange("(t p) c -> p t c", p=128), stv, [stb], [dst_buf], sts, partial=True)

            OFF = {"rq": 0, "rk": 2048, "rv": 4096, "rg": 8192, "gq": 12288, "gk": 14336, "gv": 16384, "gg": 20480, "glr": 24576, "bg": 24608}
            nblk = NH if stage >= 2 else 1
            for h in range(nblk):
                fm_block(OFF["rq"] + 256 * h, "rot", 1.0, qkT[0][h, 0], b_qkT[0])
            for h in range(nblk):
                fm_block(OFF["rk"] + 256 * h, "rot", 1.0 / 16, qkT[0][h, 1], b_qkT[0])
            for h in range(nblk):
                fm_block(OFF["gq"] + 256 * h, "copy", 1.0 / 16, qkT[1][h, 0], b_qkT[1])
            for h in range(nblk):
                fm_block(OFF["gk"] + 256 * h, "copy", 1.0, qkT[1][h, 1], b_qkT[1])
            for j in range(2 * nblk):
                tm_block(OFF["rv"] + 256 * j, "copy", vtm[0][:, 256 * j:256 * (j + 1)], b_v[0])
            for j in range(2 * nblk):
                tm_block(OFF["gv"] + 256 * j, "copy", vtm[1][:, 256 * j:256 * (j + 1)], b_v[1])
            for j in range(2 * nblk):
                tm_block(OFF["rg"] + 256 * j, "silu", sgtm[0][:, 256 * j:256 * (j + 1)], b_sg[0])
            for j in range(2 * nblk):
                tm_block(OFF["gg"] + 256 * j, "silu", sgtm[1][:, 256 * j:256 * (j + 1)], b_sg[1])
            for j in range(4 * nblk):
                fm_block(OFF["bg"] + 256 * j, "sig", 1.0, sbgT[2 * j:2 * j + 2], b_sbgT)
            wt, wb = load_w(OFF["glr"], 32)
            glr_sc = cx.semctr("s_glr")
            for z in range(2):
                for tb in range(4):
                    tsl = slice(tb * 512, (tb + 1) * 512)
                    ps, pb = next_ps()
                    cx.mm(ps[0:16, 0:512], [(wt[:, k, z * 16:(z + 1) * 16], XT[:, k, tsl]) for k in range(KC)], [wb, b_XT], pb)
                    cx.op("act", lambda: nc.scalar.copy(out=glr_s[:, z, tsl], in_=ps[0:16, 0:512]), [pb], [b_glrs])
            cx.dma("sp", glrT_d.rearrange("z r t -> r z t"), glr_s[:], [b_glrs], [b_glr], glr_sc, partial=True)
            allb = b_qkT + b_v + b_sg + [b_sbgT, b_glr]
        cx.barrier()
        esX.close()
        if stage < 3:
            fin = cx.semctr("fin")
            cx.dma("sp", out[0:128, 0:128], ident_in, allb, [], fin)
            nc.sync.wait_ge(fin.sem, fin.n)
            return nc
        nhead = NH if stage >= 4 or debug is None else 1
        XROWS = 2 * NH * 2 * 2 * 128
        XCH = 1024
        NXC = XROWS // XCH
        xs_src = [dt(f"xs_src{i}", [XCH, 512], BF16).ap() for i in range(NXC)]
        xs_dst = [dt(f"xs_dst{i}", [2 * XCH, 512], BF16).ap() for i in range(NXC)]
        b_xsrc = Buf("xs_src")
        b_xdst = Buf("xs_dst")
        branchT = [scr(f"branchT{b}", [KC, 128, T]) for b in range(2)]
        b_brT = [Buf() for _ in range(2)]
        with ExitStack() as es2:
            sb2 = lambda name, shape, d: es2.enter_context(nc.sbuf_tensor(name, list(shape), d))
            V = nc.vector
            A = nc.scalar
            G = nc.gpsimd
            identf = sb2("identf", [128, 128], F32)
            maskF = sb2("maskF_s", [128, 128], F32)
            maskB = sb2("maskB_s", [128, 128], F32)
            rmask = sb2("rmask_s", [128, T], F32)
            flags = sb2("flags_s", [128, 2], F32)
            dl = sb2("dl", [128, 16], F32)
            negb = sb2("negb", [128, 32], F32)
            gbr = sb2("gbr", [32, 128], F32)
            b_c2 = Buf("c2")
            cx.dma("sp", identf[:], ident_in, [], [b_c2], setup, partial=True)
            cx.dma("sp", maskF[:], din("maskF", [128, 128]), [], [b_c2], setup, partial=True)
            cx.dma("sp", maskB[:], din("maskB", [128, 128]), [], [b_c2], setup, partial=True)
            cx.dma("sp", rmask[:], din("rmask", [1, T])[0].partition_broadcast(128), [], [b_c2], setup, partial=True)
            cx.dma("sp", flags[:], din("flags", [1, 2])[0].partition_broadcast(128), [], [b_c2], setup, partial=True)
            cx.dma("sp", dl[:], din("ret_decay_logit", [1, 16])[0].partition_broadcast(128), [], [b_c2], setup, partial=True)
            gate_b = din("gla_gate_b", [2, 2048])
            gate_w = din("gla_gate_w", [2, 16, 2048])
            ret_norm = din("ret_norm", [1, 4096])
            gla_norm = din("gla_norm", [1, 4096])
            cx.dma("sp", gbr[:], gate_b.rearrange("z (c p) -> (z c) p", p=128), [], [b_c2], setup, partial=True)
            cx.op("act", lambda: A.activation(out=dl[:], in_=dl[:], func=AF.Exp, scale=-1.0), [b_c2], [b_c2])
            cx.op("act", lambda: A.activation(out=dl[:], in_=dl[:], func=AF.Ln, bias=1.0), [b_c2], [b_c2])
            ps, pb = next_ps()
            cx.mm_multi([lambda: nc.tensor.transpose(ps[:, 0:32], gbr[:], identf[0:32, 0:32])], [b_c2], pb)
            cx.op("act", lambda: A.activation(out=negb[:], in_=ps[:, 0:32], func=AF.Copy, scale=-1.0), [pb], [b_c2])

            glr = sb2("glr2", [16, 2, T], F32)
            b_glr2 = Buf()
            ld = cx.semctr("s_ld2")
            cx.dma("sp", glr[:], glrT_d.rearrange("z r t -> r z t"), [b_glr], [b_glr2], ld)
            gw = sb2("gw", [16, 2, 256], F32)
            b_gw = Buf()
            qk = sb2("qk", [128, 2, 2, T], BF16)
            b_qk = Buf()
            vv = sb2("vv", [128, NT, 512], BF16)
            b_vv = Buf()
            spt = sb2("spt", [128, T], F32)
            cum = sb2("cum", [128, T], F32)
            Et = sb2("Et", [128, T], F32)
            b_dec = Buf("dec")
            qh = [sb2(f"qh{z}", [128, 2, T], BF16) for z in range(2)]
            kh = [sb2(f"kh{z}", [128, 2, T], BF16) for z in range(2)]
            b_qh = [Buf() for _ in range(2)]
            b_kh = [Buf() for _ in range(2)]
            ktm = [sb2(f"ktm{z}", [128, NT, 256], BF16) for z in range(2)]
            b_ktm = [Buf() for _ in range(2)]
            sdec = [sb2(f"sdec{z}", [128, 2, NT], F32) for z in range(2)]
            b_sdec = [Buf() for _ in range(2)]
            R = [sb2(f"R{z}", [128, 2, 512], F32) for z in range(2)]
            Rb = [sb2(f"Rb{z}", [128, 2, 512], BF16) for z in range(2)]
            b_R = [Buf() for _ in range(2)]
            b_Rb = [Buf() for _ in range(2)]
            Rtmp = sb2("Rtmp", [128, 512], F32)
            b_Rtmp = Buf()
            cum3 = cum[:].rearrange("p (n c) -> p n c", c=128)
            spt3 = spt[:].rearrange("p (n c) -> p n c", c=128)
            SC = (1.0, 1.0 / 16)

            def prep(b, h, need_q):
                cx.dma("sp", qk[:], qkT[b][h].rearrange("a c p t -> p a c t"), [b_qkT[b]], [b_qk], ld)
                cx.dma("sp", vv[:], vtm[b][:, h * 512:(h + 1) * 512].rearrange("(n p) c -> p n c", p=128), [b_v[b]], [b_vv], ld)
                if b == 1:
                    cx.dma("sp", gw[:], gate_w[:, :, h * 256:(h + 1) * 256].rearrange("z r c -> r z c"), [], [b_gw], ld)
                s = SC[b]
                for z in range(2):
                    for dch in range(2):
                        if b == 0:
                            cx.op("act", lambda: A.activation(out=spt[:], in_=rmask[:], func=AF.Identity, scale=0.0, bias=dl[:, z * 8 + h:z * 8 + h + 1]),
                                  [b_c2], [b_dec])
                        else:
                            col = z * 16 + h * 2 + dch
                            for tb in range(4):
                                tsl = slice(tb * 512, (tb + 1) * 512)
                                ps, pb = next_ps()
                                cx.mm(ps[:, 0:512], [(gw[:, z, dch * 128:(dch + 1) * 128], glr[:, z, tsl])], [b_gw, b_glr2], pb)
                                cx.op("act", lambda: A.activation(out=spt[:, tsl], in_=ps[:, 0:512], func=AF.Exp, scale=-1.0, bias=negb[:, col:col + 1]),
                                      [pb, b_c2], [b_dec])
                            cx.op("act", lambda: A.activation(out=spt[:], in_=spt[:], func=AF.Ln, bias=1.0), [b_dec], [b_dec])
                        cx.op("dve", lambda: V.tensor_tensor_scan(out=cum[:], data0=rmask[:], data1=spt[:], initial=0.0, op0=ALU.mult, op1=ALU.add),
                              [b_dec, b_c2], [b_dec])
                        cx.op("act", lambda: A.activation(out=sdec[z][:, dch, :], in_=cum3[:, :, 127], func=AF.Exp, scale=-s), [b_dec], [b_sdec[z]])
                        if z == 1:
                            cx.op("dve", lambda: V.tensor_tensor(out=cum[:], in0=cum[:], in1=spt[:], op=ALU.subtract), [b_dec], [b_dec])
                        sq = -s if z == 0 else s
                        if need_q:
                            cx.op("act", lambda: A.activation(out=Et[:], in_=cum[:], func=AF.Exp, scale=sq), [b_dec], [b_dec])
                            cx.op("dve", lambda: V.tensor_tensor(out=qh[z][:, dch, :], in0=qk[:, 0, dch, :], in1=Et[:], op=ALU.mult), [b_dec, b_qk], [b_qh[z]])
                        cx.op("act", lambda: A.activation(out=Et[:], in_=cum[:], func=AF.Exp, scale=-sq), [b_dec, b_qh[z]], [b_dec])
                        cx.op("dve", lambda: V.tensor_tensor(out=kh[z][:, dch, :], in0=qk[:, 1, dch, :], in1=Et[:], op=ALU.mult), [b_dec, b_qk], [b_kh[z]])
                    for n in range(NT):
                        ps, pb = next_ps()
                        pv = ps[:].bitcast(BF16)
                        cx.mm_multi([(lambda d_=d_: nc.tensor.transpose(pv[:, d_ * 128:(d_ + 1) * 128], kh[z][:, d_, n * 128:(n + 1) * 128], ident[:])) for d_ in range(2)],
                                    [b_kh[z], b_const], pb)
                        if n % 2 == 0:
                            cx.op("act", lambda: A.copy(out=ktm[z][:, n, :], in_=pv[:, 0:256]), [pb], [b_ktm[z]])
                        else:
                            cx.op("dve", lambda: V.tensor_copy(out=ktm[z][:, n, :], in_=pv[:, 0:256]), [pb], [b_ktm[z]])

            def kv_update(z, n, form_f):
                for dch in range(2):
                    ps, pb = next_ps()
                    cx.mm(ps[:, 0:512], [(ktm[z][:, n, dch * 128:(dch + 1) * 128], vv[:, n, :])], [b_ktm[z], b_vv], pb)
                    if form_f:
                        cx.op("dve", lambda: V.tensor_tensor(out=Rtmp[:], in0=ps[:, 0:512], in1=R[z][:, dch, :], op=ALU.add), [pb, b_R[z]], [b_Rtmp])
                        cx.op("act", lambda: A.activation(out=R[z][:, dch, :], in_=Rtmp[:], func=AF.Copy, scale=sdec[z][:, dch, n:n + 1]), [b_Rtmp, b_sdec[z]], [b_R[z]])
                    else:
                        cx.op("dve", lambda: V.tensor_tensor(out=R[z][:, dch, :], in0=ps[:, 0:512], in1=R[z][:, dch, :], op=ALU.add), [pb, b_R[z]], [b_R[z]])

            def scale_state(z, n):
                for dch in range(2):
                    cx.op("act", lambda: A.activation(out=R[z][:, dch, :], in_=R[z][:, dch, :], func=AF.Copy, scale=sdec[z][:, dch, n:n + 1]), [b_R[z], b_sdec[z]], [b_R[z]])

            def xloc(b, h, z):
                base = ((b * NH + h) * 2 + z) * 256
                return base // XCH, base % XCH

            stA = cx.semctr("s_stA")
            for b in range(2):
                for h in range(nhead):
                    prep(b, h, False)
                    for z in range(2):
                        cx.op("pool", lambda: G.memset(R[z][:], 0.0), [], [b_R[z]])
                    for n in range(NT):
                        kv_update(0, n, True)
                        scale_state(1, NT - 1 - n)
                        kv_update(1, NT - 1 - n, False)
                    for z in range(2):
                        cx.op("dve", lambda: V.tensor_copy(out=Rb[z][:], in_=R[z][:]), [b_R[z]], [b_Rb[z]])
                        ci, r0 = xloc(b, h, z)
                        cx.dma("sp", xs_src[ci][r0:r0 + 256, :].rearrange("(c p) f -> p c f", p=128), Rb[z][:], [b_Rb[z]], [b_xsrc], stA, partial=True)
            cx.deps("pool", [b_xsrc], [])
            ccs = cx.sem("ccsem")
            for i in range(NXC):
                nc.gpsimd.collective_compute("AllGather", ALU.bypass, replica_groups=[[2 * r, 2 * r + 1] for r in range(NCORES // 2)],
                                             ins=[xs_src[i].opt()], outs=[xs_dst[i].opt()]).then_inc(ccs)
                nc.gpsimd.wait_ge(ccs, i + 1)
            for e in ("pool", "sp"):
                cx.eng[e].wait_ge(ccs, NXC)
            o_acc = sb2("o_acc", [128, NT, 512], F32)
            b_o = Buf()
            b_oc = [Buf() for _ in range(NT)]
            brs = sb2("brs", [128, 4, T], BF16)
            b_brs = Buf()
            sgc = [sb2(f"sgc{i}", [128, 512], BF16) for i in range(2)]
            b_sgc = [Buf() for _ in range(2)]
            s_sgc = [cx.semctr(f"s_sgc{i}") for i in range(2)]
            gn = sb2("gn", [128, 512], F32)
            b_gn = Buf()
            PT = sb2("PT", [128, 128], BF16)
            b_PT = Buf()
            pt1 = sb2("pt1", [128, 128], F32)
            pt2 = sb2("pt2", [128, 128], F32)
            b_pt = Buf()
            stt = sb2("stt", [128, 8], F32)
            b_stt = Buf()
            yn = sb2("yn", [128, 512], F32)
            ynb = sb2("ynb", [128, 512], BF16)
            b_yn = Buf()
            junk2 = sb2("junk2", [128, 512], BF16)
            b_j2 = Buf()
            stB = cx.semctr("s_stB")
            sgi = [0]
            for b in range(2):
                for h in range(nhead):
                    prep(b, h, True)
                    nrm = ret_norm if b == 0 else gla_norm
                    cx.dma("sp", gn[:], nrm[0, h * 512:(h + 1) * 512].partition_broadcast(128), [], [b_gn], ld)
                    for z in range(2):
                        ci, r0 = xloc(b, h, z)
                        r0 += z * XCH
                        cx.dma("sp", Rb[z][:], xs_dst[ci][r0:r0 + 256, :].rearrange("(c p) f -> p c f", p=128), [], [b_Rb[z]], ld)
                        cx.op("dve", lambda: V.tensor_scalar(R[z][:], Rb[z][:], flags[:, z:z + 1], None, ALU.mult), [b_Rb[z], b_c2], [b_R[z]])

                    def finalize(n):
                        csl = slice(n * 128, (n + 1) * 128)
                        c = lambda i: stt[:, i:i + 1]
                        cx.op("act", lambda: A.activation(out=junk2[:], in_=yn[:], func=AF.Identity, accum_out=c(0)), [b_yn], [b_j2, b_stt])
                        cx.op("act", lambda: A.activation(out=junk2[:], in_=yn[:], func=AF.Square, accum_out=c(1)), [b_yn], [b_j2, b_stt])
                        cx.op("dve", lambda: V.tensor_scalar(c(2), c(0), 1.0 / 512, None, ALU.mult), [b_stt], [b_stt])
                        cx.op("dve", lambda: V.tensor_scalar(c(3), c(1), 1.0 / 512, EPS, ALU.mult, ALU.add), [b_stt], [b_stt])
                        if b == 0:
                            cx.op("dve", lambda: V.tensor_tensor(out=c(4), in0=c(2), in1=c(2), op=ALU.mult), [b_stt], [b_stt])
                            cx.op("dve", lambda: V.tensor_tensor(out=c(3), in0=c(3), in1=c(4), op=ALU.subtract), [b_stt], [b_stt])
                        cx.op("act", lambda: A.activation(out=c(5), in_=c(3), func=AF.Sqrt), [b_stt], [b_stt])
                        cx.op("dve", lambda: V.reciprocal(c(6), c(5)), [b_stt], [b_stt])
                        if b == 0:
                            cx.op("dve", lambda: V.tensor_scalar(yn[:], yn[:], c(2), c(6), ALU.subtract, ALU.mult), [b_stt, b_yn], [b_yn])
                        else:
                            cx.op("dve", lambda: V.tensor_scalar(yn[:], yn[:], c(6), None, ALU.mult), [b_stt, b_yn], [b_yn])
                        i = sgi[0] % 2
                        sgi[0] += 1
                        cx.dma("sp", sgc[i][:], sgtm[b][n * 128:(n + 1) * 128, h * 512:(h + 1) * 512], [b_sg[b]], [b_sgc[i]], s_sgc[i])
                        cx.op("dve", lambda: V.tensor_tensor(out=yn[:], in0=yn[:], in1=gn[:], op=ALU.mult), [b_yn, b_gn], [b_yn])
                        cx.op("dve", lambda: V.tensor_tensor(out=ynb[:], in0=yn[:], in1=sgc[i][:], op=ALU.mult), [b_yn, b_sgc[i]], [b_yn])
                        ps3, pb3 = next_ps()
                        pv = ps3[:].bitcast(BF16)
                        cx.mm_multi([(lambda cc=cc: nc.tensor.transpose(pv[:, cc * 128:(cc + 1) * 128], ynb[:, cc * 128:(cc + 1) * 128], ident[:])) for cc in range(4)],
                                    [b_yn, b_const], pb3)
                        cx.op("act", lambda: A.copy(out=brs[:, :, csl], in_=pv[:, 0:512].rearrange("p (a b) -> p a b", a=4)), [pb3], [b_brs])

                    def land(n, ps2, pb2, first):
                        if first:
                            cx.op("act", lambda: A.copy(out=o_acc[:, n, :], in_=ps2[:, 0:512]), [pb2], [b_oc[n]])
                        else:
                            cx.op("dve", lambda: V.tensor_tensor(out=yn[:], in0=ps2[:, 0:512], in1=o_acc[:, n, :], op=ALU.add), [pb2, b_oc[n]], [b_yn])

                    def fwd_step(n, first):
                        csl = slice(n * 128, (n + 1) * 128)
                        cx.op("dve", lambda: V.tensor_copy(out=Rb[0][:], in_=R[0][:]), [b_R[0]], [b_Rb[0]])
                        ps, pb = next_ps()
                        cx.mm(ps[:, 0:128], [(kh[0][:, d_, csl], qh[0][:, d_, csl]) for d_ in range(2)], [b_kh[0], b_qh[0]], pb)
                        cx.mm(ps[:, 128:256], [(kh[1][:, d_, csl], qh[1][:, d_, csl]) for d_ in range(2)], [b_kh[1], b_qh[1]], pb)
                        cx.op("dve", lambda: V.tensor_tensor(out=pt1[:], in0=ps[:, 0:128], in1=maskF[:], op=ALU.mult), [pb, b_c2], [b_pt])
                        cx.op("dve", lambda: V.tensor_tensor(out=pt2[:], in0=ps[:, 128:256], in1=maskB[:], op=ALU.mult), [pb, b_c2], [b_pt])
                        cx.op("dve", lambda: V.tensor_tensor(out=PT[:], in0=pt1[:], in1=pt2[:], op=ALU.add), [b_pt], [b_PT])
                        ps2, pb2 = next_ps()
                        cx.mm(ps2[:, 0:512], [(PT[:], vv[:, n, :])] + [(qh[0][:, d_, csl], Rb[0][:, d_, :]) for d_ in range(2)],
                              [b_PT, b_vv, b_qh[0], b_Rb[0]], pb2)
                        land(n, ps2, pb2, first)
                        kv_update(0, n, True)
                        if not first:
                            finalize(n)

                    def bwd_step(n, first):
                        csl = slice(n * 128, (n + 1) * 128)
                        scale_state(1, n)
                        cx.op("dve", lambda: V.tensor_copy(out=Rb[1][:], in_=R[1][:]), [b_R[1]], [b_Rb[1]])
                        ps2, pb2 = next_ps()
                        cx.mm(ps2[:, 0:512], [(qh[1][:, d_, csl], Rb[1][:, d_, :]) for d_ in range(2)], [b_qh[1], b_Rb[1]], pb2)
                        land(n, ps2, pb2, first)
                        kv_update(1, n, False)
                        if not first:
                            finalize(n)

                    for i_ in range(NT):
                        fwd_step(i_, i_ < NT // 2)
                        bwd_step(NT - 1 - i_, i_ < NT // 2)
                    cx.dma("sp", branchT[b][h * 4:(h + 1) * 4].rearrange("a p t -> p a t"), brs[:], [b_brs], [b_brT[b]], stB, partial=True)
        cx.barrier()
        if stage < 4:
            fin = cx.semctr("fin")
            cx.dma("sp", out[0:128, 0:128], ident_in, b_brT, [], fin)
            nc.sync.wait_ge(fin.sem, fin.n)
            return nc
        w_branch = din("w_branch", [2, D, D])
        w_out = din("w_out", [D, D])
        norm_ffn = din("norm_ffn", [1, D])
        m0T = scr("m0T", [KC, 128, T])
        mergedT = scr("mergedT", [KC, 128, T])
        h1 = scr("h1", [T, D], F32)
        xn2tm = scr("xn2tm", [T, D])
        b_m0 = Buf()
        b_mT = Buf()
        b_h1 = Buf()
        b_xn2tm = Buf()
        V = nc.vector
        A = nc.scalar
        G = nc.gpsimd
        esX = es.enter_context(ExitStack())
        XT = esX.enter_context(nc.sbuf_tensor("AT3", [128, KC, T], BF16))
        b_XT = Buf("AT3")
        ldx = cx.semctr("s_ldx")

        def load_XT(src, src_buf):
            for g in range(4):
                cx.dma("sp", XT[:, g * 8:(g + 1) * 8, :], src[g * 8:(g + 1) * 8].rearrange("k p t -> p k t"), [src_buf], [b_XT], ldx, partial=(g > 0))

        def w_loader(ring):
            def load_w(W, c0):
                wt, wb, ws = ring.next()
                cx.dma("pool", wt[:], W[:, c0:c0 + 256].rearrange("(kc p) c -> p kc c", p=128), [], [wb], ws)
                return wt, wb
            return load_w

        with ExitStack() as es3:
            sb3 = lambda name, shape, d: es3.enter_context(nc.sbuf_tensor(name, list(shape), d))
            load_w = w_loader(Ring(cx, "w3", 2, [128, KC, 256], BF16, es3))
            with ExitStack() as es3a:
                sb3a = lambda name, shape, d: es3a.enter_context(nc.sbuf_tensor(name, list(shape), d))
                sring = Ring(cx, "stg3", 2, [128, 2, T], BF16, es3a)
                sbg1 = sb3a("sbg1", [128, 2, T], BF16)
                b_sbg1 = Buf()
                s_sbg1 = cx.semctr("s_sbg1")
                m0b = sb3a("m0b", [128, 2, T], BF16)
                b_m0b = Buf()
                s_m0b = cx.semctr("s_m0b")
                gtmp = sb3a("gtmp", [128, 512], F32)
                b_gtmp = Buf()
                for b in range(2):
                    load_XT(branchT[b], b_brT[b])
                    for blk in range(16):
                        wt, wb = load_w(w_branch[b], blk * 256)
                        cx.dma("sp", sbg1[:], sbgT[b * KC + 2 * blk:b * KC + 2 * blk + 2].rearrange("a p t -> p a t"), [b_sbgT], [b_sbg1], s_sbg1)
                        if b == 1:
                            cx.dma("sp", m0b[:], m0T[2 * blk:2 * blk + 2].rearrange("a p t -> p a t"), [b_m0], [b_m0b], s_m0b)
                        stt_, stb, sts = sring.next()
                        for tb in range(4):
                            tsl = slice(tb * 512, (tb + 1) * 512)
                            for dch in range(2):
                                ps, pb = next_ps()
                                cx.mm(ps[:, 0:512], [(wt[:, k, dch * 128:(dch + 1) * 128], XT[:, k, tsl]) for k in range(KC)], [wb, b_XT], pb)
                                if b == 0:
                                    cx.op("dve", lambda: V.tensor_tensor(out=stt_[:, dch, tsl], in0=ps[:, 0:512], in1=sbg1[:, dch, tsl], op=ALU.mult), [pb, b_sbg1], [stb])
                                else:
                                    cx.op("dve", lambda: V.tensor_tensor(out=gtmp[:], in0=ps[:, 0:512], in1=sbg1[:, dch, tsl], op=ALU.mult), [pb, b_sbg1], [b_gtmp])
                                    cx.op("pool", lambda: G.tensor_tensor(out=stt_[:, dch, tsl], in0=gtmp[:], in1=m0b[:, dch, tsl], op=ALU.add), [b_gtmp, b_m0b], [stb])
                        dstT, dstB = (m0T, b_m0) if b == 0 else (mergedT, b_mT)
                        cx.dma("sp", dstT[2 * blk:2 * blk + 2].rearrange("a p t -> p a t"), stt_[:], [stb], [dstB], sts, partial=True)
                cx.barrier()
            with ExitStack() as es3b:
                sb3b = lambda name, shape, d: es3b.enter_context(nc.sbuf_tensor(name, list(shape), d))
                xblk = sb3b("xblk", [128, NT, 256], F32)
                b_xblk = Buf()
                s_xblk = cx.semctr("s_xblk")
                h1s = sb3b("h1s", [128, NT, 256], F32)
                b_h1s = Buf()
                s_h1s = cx.semctr("s_h1s")
                load_XT(mergedT, b_mT)
                for blk in range(16):
                    csl = slice(blk * 256, (blk + 1) * 256)
                    wt, wb = load_w(w_out, blk * 256)
                    cx.dma("sp", xblk[:], x[:, csl].rearrange("(t p) c -> p t c", p=128), [], [b_xblk], s_xblk)
                    for t in range(NT):
                        ps, pb = next_ps()
                        cx.mm(ps[:, 0:256], [(XT[:, k, t * 128:(t + 1) * 128], wt[:, k, :]) for k in range(KC)], [wb, b_XT], pb)
                        cx.op("dve", lambda: V.tensor_tensor(out=h1s[:, t, :], in0=ps[:, 0:256], in1=xblk[:, t, :], op=ALU.add), [pb, b_xblk], [b_h1s])
                    cx.dma("sp", h1[:, csl].rearrange("(t p) c -> p t c", p=128), h1s[:], [b_h1s], [b_h1], s_h1s, partial=True)
                cx.barrier()
        norm_T("n2", h1, [b_h1], norm_ffn[0], XT, b_XT, tm_dst=xn2tm, b_tm=b_xn2tm)
        if stage < 5:
            fin = cx.semctr("fin")
            cx.dma("sp", out[0:128, 0:128], ident_in, [b_xn2tm, b_h1], [], fin)
            nc.sync.wait_ge(fin.sem, fin.n)
            return nc
        w_router = din("w_router", [D, 16])
        CAP = 512
        xa_src = dt("xa_src", [16, T], F32).ap()
        xa_dst = dt("xa_dst", [32, T], F32).ap()
        b_xa = Buf()
        es4 = es.enter_context(ExitStack())
        sb4 = lambda name, shape, d: es4.enter_context(nc.sbuf_tensor(name, list(shape), d))
        rkT_d = scr("rkT_d", [16, T], F32)
        rktm_d = scr("rktm_d", [128, NT * 16], F32)
        gtm_d = scr("gtm_d", [128, NT * 16], BF16)
        b_rt = Buf()
        with ExitStack() as es4a:
            sb4a = lambda name, shape, d: es4a.enter_context(nc.sbuf_tensor(name, list(shape), d))
            identf = sb4a("identf4", [128, 128], F32)
            b_c4 = Buf()
            cx.dma("sp", identf[:], ident_in, [], [b_c4], setup)
            wr = sb4a("wr", [128, KC, 16], BF16)
            cx.dma("pool", wr[:], w_router.rearrange("(kc p) e -> p kc e", p=128), [], [b_c4], setup, partial=True)
            aff = sb4a("aff", [128, NT, 16], F32)
            b_aff = Buf()
            sm4 = sb4a("sm4", [128, 4 * NT], F32)
            b_sm4 = Buf()
            affT = sb4a("affT", [16, T], F32)
            b_affT = Buf()
            for t in range(NT):
                ps, pb = next_ps()
                cx.mm(ps[:, 0:16], [(XT[:, k, t * 128:(t + 1) * 128], wr[:, k, :]) for k in range(KC)], [b_XT, b_c4], pb)
                c = lambda i: sm4[:, 4 * t + i:4 * t + i + 1]
                cx.op("dve", lambda: V.reduce_max(out=c(0), in_=ps[:, 0:16], axis=AX.X), [pb], [b_sm4])
                cx.op("dve", lambda: V.tensor_scalar(c(1), c(0), -1.0, None, ALU.mult), [b_sm4], [b_sm4])
                cx.op("act", lambda: A.activation(out=aff[:, t, :], in_=ps[:, 0:16], func=AF.Exp, bias=c(1), accum_out=c(2)), [pb, b_sm4], [b_aff, b_sm4])
                cx.op("dve", lambda: V.reciprocal(c(3), c(2)), [b_sm4], [b_sm4])
                cx.op("dve", lambda: V.tensor_scalar(aff[:, t, :], aff[:, t, :], c(3), None, ALU.mult), [b_sm4, b_aff], [b_aff])
            for g in range(4):
                ps, pb = next_ps()
                cx.mm_multi([(lambda j=j: nc.tensor.transpose(ps[0:16, j * 128:(j + 1) * 128], aff[:, g * 4 + j, :], identf[:])) for j in range(4)], [b_aff, b_c4], pb)
                cx.op("act", lambda: A.copy(out=affT[:, g * 512:(g + 1) * 512], in_=ps[0:16, 0:512]), [pb], [b_affT])
            s_xa = cx.semctr("s_xa")
            cx.dma("sp", xa_src, affT[:], [b_affT], [b_xa], s_xa)
            cx.deps("pool", [b_xa], [])
            ccs2 = cx.sem("ccsem2")
            nc.gpsimd.collective_compute("AllGather", ALU.bypass, replica_groups=[[2 * r, 2 * r + 1] for r in range(NCORES // 2)],
                                         ins=[xa_src.opt()], outs=[xa_dst.opt()]).then_inc(ccs2)
            for e_ in ("pool", "sp"):
                cx.eng[e_].wait_ge(ccs2, 1)
            work = sb4a("work", [16, 2, T], F32)
            b_work = Buf()
            cx.dma("sp", work[:], xa_dst.rearrange("(r e) t -> e r t", e=16), [], [b_work], s_xa)
            m8 = sb4a("m8", [16, 8], F32)
            b_m8 = Buf()
            workf = work[:].rearrange("e r t -> e (r t)")
            for it in range(CAP // 8):
                cx.op("dve", lambda: V.max(out=m8[:], in_=workf), [b_work], [b_m8])
                if it < CAP // 8 - 1:
                    cx.op("dve", lambda: V.match_replace(out=workf, in_to_replace=m8[:], in_values=workf, imm_value=-1.0), [b_m8, b_work], [b_work])
            maskT = sb4a("maskT", [16, T], F32)
            cntT = sb4a("cntT", [16, T], F32)
            onesT = sb4a("onesT", [16, T], F32)
            rkT = sb4a("rkT", [16, T], F32)
            b_mk = Buf()
            cx.op("pool", lambda: G.memset(onesT[:], 1.0), [], [b_mk])
            cx.op("dve", lambda: V.tensor_scalar(maskT[:], affT[:], m8[:, 7:8], None, ALU.is_ge), [b_affT, b_m8], [b_mk])
            cx.op("dve", lambda: V.tensor_tensor_scan(out=cntT[:], data0=onesT[:], data1=maskT[:], initial=0.0, op0=ALU.mult, op1=ALU.add), [b_mk], [b_mk])
            cx.op("dve", lambda: V.tensor_tensor(out=cntT[:], in0=cntT[:], in1=maskT[:], op=ALU.mult), [b_mk], [b_mk])
            cx.op("dve", lambda: V.tensor_scalar(rkT[:], cntT[:], -1.0, None, ALU.add), [b_mk], [b_mk])
            rktm = sb4a("rktm", [128, NT, 16], F32)
            mtm = sb4a("mtm", [128, NT, 16], F32)
            gtm = sb4a("gtm", [128, NT, 16], BF16)
            b_rk = Buf()
            ps, pb = next_ps()
            cx.mm_multi([(lambda t=t: nc.tensor.transpose(ps[:, t * 16:(t + 1) * 16], rkT[:, t * 128:(t + 1) * 128], identf[0:16, 0:16])) for t in range(NT)], [b_mk, b_c4], pb)
            cx.op("act", lambda: A.copy(out=rktm[:].rearrange("p t e -> p (t e)"), in_=ps[:, 0:256]), [pb], [b_rk])
            cx.op("dve", lambda: V.tensor_single_scalar(mtm[:], rktm[:], 0.0, ALU.is_ge), [b_rk], [b_rk])
            cx.op("dve", lambda: V.tensor_tensor(out=gtm[:], in0=aff[:], in1=mtm[:], op=ALU.mult), [b_rk, b_aff], [b_rk])
            cx.dma("sp", rkT_d, rkT[:], [b_mk], [b_rt], s_xa, partial=True)
            cx.dma("sp", rktm_d, rktm[:].rearrange("p t e -> p (t e)"), [b_rk], [b_rt], s_xa, partial=True)
            cx.dma("sp", gtm_d, gtm[:].rearrange("p t e -> p (t e)"), [b_rk], [b_rt], s_xa, partial=True)
            cx.barrier()
        es4.close()
        esX.close()
        if stage < 6:
            fin = cx.semctr("fin")
            cx.dma("sp", out[0:128, 0:128], ident_in, [b_rt], [], fin)
            nc.sync.wait_ge(fin.sem, fin.n)
            return nc
        weg = din("w_expert_gate", [16, D, 2048])
        weu = din("w_expert_up", [16, D, 2048])
        wed = din("w_expert_down", [16, 2048, D])
        b_h2 = [[Buf() for _ in range(8)] for _ in range(NT)]
        with ExitStack() as es5:
            sb5 = lambda name, shape, d: es5.enter_context(nc.sbuf_tensor(name, list(shape), d))
            b_c5 = Buf()
            rkT = sb5("rkT5", [16, T], F32)
            rktm = sb5("rktm5", [128, NT, 16], F32)
            gtm = sb5("gtm5", [128, NT, 16], BF16)
            cx.dma("sp", rkT[:], rkT_d, [b_rt], [b_c5], setup)
            cx.dma("sp", rktm[:].rearrange("p t e -> p (t e)"), rktm_d, [b_rt], [b_c5], setup, partial=True)
            cx.dma("sp", gtm[:].rearrange("p t e -> p (t e)"), gtm_d, [b_rt], [b_c5], setup, partial=True)
            iota_r = sb5("iota_r", [128, CAP], F32)
            cx.dma("sp", iota_r[:], din("iota_row", [1, CAP])[0].partition_broadcast(128), [], [b_c5], setup, partial=True)
            jv = sb5("jv", [128, 4], F32)
            cx.dma("sp", jv[:], din("jvals", [128, 4]), [], [b_c5], setup, partial=True)
            selc = sb5("selc_s", [16, 16, 128], F32)
            cx.dma("sp", selc[:], din("selc", [16, 16, 128]), [], [b_c5], setup, partial=True)
            xring = Ring(cx, "xn2h", 2, [128, NT, 512], BF16, es5)
            wring = Ring(cx, "w5", 4, [128, KC * 256], BF16, es5)
            Pm = sb5("Pm", [128, NT * CAP], BF16)
            b_P = Buf()
            Pv = Pm[:].rearrange("p (t j) -> p t j", t=NT)
            PTv = Pm[:].rearrange("p (c t) -> p c t", c=4)
            xsT = sb5("xsT", [128, KC, CAP], BF16)
            b_xs = Buf()
            hidT = sb5("hidT", [128, 16, CAP], BF16)
            b_hid = Buf()
            ysel = sb5("ysel", [128, 4, 512], BF16)
            b_ys = Buf()
            gsel = sb5("gsel", [128, 4], F32)
            b_gs = Buf()
            stmp = sb5("stmp", [128, 512], F32)
            b_stmp = Buf()
            dtmp = sb5("dtmp", [128, 512], F32)
            b_dtmp = Buf()
            yring = Ring(cx, "yst", 2, [128, 512], F32, es5)
            nexp = 16 if (debug is None or stage >= 7) else 2
            wblocks = []
            for e in range(nexp):
                for blk in range(8):
                    wblocks.append(("g", e, blk))
                    wblocks.append(("u", e, blk))
                for cb in range(8):
                    wblocks.append(("d", e, cb))
            wloaded = []
            wstate = {"issued": 0, "used": 0}

            def w_issue_to(k):
                while wstate["issued"] < min(k, len(wblocks)):
                    kind, e_, i_ = wblocks[wstate["issued"]]
                    j_ = wstate["issued"]
                    if j_ >= 2:
                        cx._wait("pool", wloaded[j_ - 2][2], wloaded[j_ - 2][3])
                    wt, wb, wsm = wring.next()
                    if kind == "d":
                        cx.dma("pool", wt[:].rearrange("p (f c) -> p f c", f=16), wed[e_][:, i_ * 512:(i_ + 1) * 512].rearrange("(f p) c -> p f c", p=128), [], [wb], wsm)
                    else:
                        Wm = weg if kind == "g" else weu
                        cx.dma("pool", wt[:].rearrange("p (k c) -> p k c", k=KC), Wm[e_][:, i_ * 256:(i_ + 1) * 256].rearrange("(kc p) c -> p kc c", p=128), [], [wb], wsm)
                    wloaded.append((wt, wb, wsm.sem, wsm.n))
                    wstate["issued"] += 1

            def w_take(n):
                i = wstate["used"]
                wstate["used"] += n
                w_issue_to(i + 4)
                return wloaded[i:i + n]

            for e in range(nexp):
                for t in range(NT):
                    cx.op("dve", lambda: V.tensor_scalar(Pv[:, t, :], iota_r[:], rktm[:, t, e:e + 1], None, ALU.is_equal), [b_c5], [b_P])
                ps, pb = next_ps()
                for jc in range(4):
                    cx.mm(ps[:, jc:jc + 1], [(Pv[:, t, jc * 128:(jc + 1) * 128], gtm[:, t, e:e + 1]) for t in range(NT)], [b_P, b_c5], pb)
                cx.op("act", lambda: A.copy(out=gsel[:], in_=ps[:, 0:4]), [pb], [b_gs])
                for q8 in range(8):
                    xn2h, b_xh, s_xh = xring.next()
                    cx.dma("sp", xn2h[:], xn2tm[:, q8 * 512:(q8 + 1) * 512].rearrange("(t p) c -> p t c", p=128), [b_xn2tm], [b_xh], s_xh)
                    for dc in range(4):
                        ps, pb = next_ps()
                        cx.mm(ps[:, 0:CAP], [(xn2h[:, t, dc * 128:(dc + 1) * 128], Pv[:, t, :]) for t in range(NT)], [b_xh, b_P], pb)
                        if dc % 2 == 0:
                            cx.op("act", lambda: A.copy(out=xsT[:, q8 * 4 + dc, :], in_=ps[:, 0:CAP]), [pb], [b_xs])
                        else:
                            cx.op("dve", lambda: V.tensor_copy(out=xsT[:, q8 * 4 + dc, :], in_=ps[:, 0:CAP]), [pb], [b_xs])
                for blk in range(8):
                    ws_ = [(wt[:].rearrange("p (k c) -> p k c", k=KC), wb) for (wt, wb, _s, _n) in w_take(2)]
                    for fs in range(2):
                        pss = []
                        for (wv, wb) in ws_:
                            ps, pb = next_ps()
                            cx.mm(ps[:, 0:CAP], [(wv[:, k, fs * 128:(fs + 1) * 128], xsT[:, k, :]) for k in range(KC)], [wb, b_xs], pb)
                            pss.append((ps, pb))
                        cx.op("act", lambda: A.activation(out=stmp[:], in_=pss[0][0][:, 0:CAP], func=AF.Silu), [pss[0][1]], [b_stmp])
                        cx.op("dve", lambda: V.tensor_tensor(out=hidT[:, blk * 2 + fs, :], in0=stmp[:], in1=pss[1][0][:, 0:CAP], op=ALU.mult), [b_stmp, pss[1][1]], [b_hid])
                for tb in range(4):
                    tsl = slice(tb * 512, (tb + 1) * 512)
                    ps, pb = next_ps()
                    cx.mm(ps[:, 0:512], [(selc[:, e, :], rkT[:, tsl])], [b_c5], pb)
                    for jc in range(4):
                        cx.op("dve", lambda: V.tensor_scalar(dtmp[:], ps[:, 0:512], jv[:, jc:jc + 1], None, ALU.subtract), [pb, b_c5], [b_dtmp])
                        cx.op("act", lambda: A.activation(out=dtmp[:], in_=dtmp[:], func=AF.Square), [b_dtmp], [b_dtmp])
                        cx.op("dve", lambda: V.tensor_single_scalar(PTv[:, jc, tsl], dtmp[:], 0.25, ALU.is_lt), [b_dtmp], [b_P])
                for cb in range(8):
                    csl = slice(cb * 512, (cb + 1) * 512)
                    (wt, wb, _s, _n), = w_take(1)
                    wv = wt[:].rearrange("p (f c) -> p f c", f=16)
                    for jc in range(4):
                        ps, pb = next_ps()
                        cx.mm(ps[:, 0:512], [(hidT[:, f, jc * 128:(jc + 1) * 128], wv[:, f, :]) for f in range(16)], [b_hid, wb], pb)
                        cx.op("act", lambda: A.activation(out=ysel[:, jc, :], in_=ps[:, 0:512], func=AF.Copy, scale=gsel[:, jc:jc + 1]), [pb, b_gs], [b_ys])
                    for t in range(NT):
                        ps, pb = next_ps()
                        cx.mm(ps[:, 0:512], [(PTv[:, jc, t * 128:(t + 1) * 128], ysel[:, jc, :]) for jc in range(4)], [b_P, b_ys], pb)
                        yt, yb, ysm = yring.next()
                        if t % 2 == 0:
                            cx.op("act", lambda: A.copy(out=yt[:], in_=ps[:, 0:512]), [pb], [yb])
                        else:
                            cx.op("dve", lambda: V.tensor_copy(out=yt[:], in_=ps[:, 0:512]), [pb], [yb])
                        cx.dma("pool", h1[t * 128:(t + 1) * 128, csl], yt[:], [yb, b_h1], [b_h2[t][cb]], ysm, accum_op=ALU.add)
            cx.barrier()
        if stage < 7:
            fin = cx.semctr("fin")
            cx.dma("sp", out[0:128, 0:128], ident_in, [], [], fin)
            nc.sync.wait_ge(fin.sem, fin.n)
            return nc
        norm_ple = din("norm_ple", [1, D])
        w_pg = din("w_ple_gate", [D, D])
        w_pp = din("w_ple_proj", [256, D])
        p_in = din("p", [T, 256])
        norm_final = din("norm_final", [1, D])
        h3 = scr("h3", [T, D], F32)
        b_h3 = Buf()
        esX = es.enter_context(ExitStack())
        XT = esX.enter_context(nc.sbuf_tensor("AT6", [128, KC, T], BF16))
        b_XT = Buf("AT6")
        norm_T("n3", h1, [], norm_ple[0], XT, b_XT)
        with ExitStack() as es6:
            sb6 = lambda name, shape, d: es6.enter_context(nc.sbuf_tensor(name, list(shape), d))
            b_c6 = Buf()
            wpp = sb6("wpp", [128, 2, D], BF16)
            cx.dma("pool", wpp[:], w_pp.rearrange("(k p) c -> p k c", p=128), [], [b_c6], setup)
            pT = sb6("pT", [128, 2, T], BF16)
            b_pT = Buf()
            h2b = sb6("h2b", [128, NT, 256], F32)
            b_h2b = Buf()
            s_h2b = cx.semctr("s_h2b")
            with ExitStack() as es6p:
                ptm = es6p.enter_context(nc.sbuf_tensor("ptm", [128, NT, 256], F32))
                pbf = es6p.enter_context(nc.sbuf_tensor("pbf", [128, NT, 256], BF16))
                b_pp = Buf()
                cx.dma("sp", ptm[:], p_in.rearrange("(t p) c -> p t c", p=128), [], [b_pp], setup)
                cx.op("dve", lambda: V.tensor_copy(out=pbf[:], in_=ptm[:]), [b_pp], [b_pp])
                for t in range(NT):
                    ps, pb = next_ps()
                    pv = ps[:].bitcast(BF16)
                    cx.mm_multi([(lambda k2=k2: nc.tensor.transpose(pv[:, k2 * 128:(k2 + 1) * 128], pbf[:, t, k2 * 128:(k2 + 1) * 128], ident[:])) for k2 in range(2)], [b_pp, b_const], pb)
                    cx.op("act", lambda: A.copy(out=pT[:, :, t * 128:(t + 1) * 128], in_=pv[:, 0:256].rearrange("p (a b) -> p a b", a=2)), [pb], [b_pT])
                cx.barrier()
            h3s = h2b
            b_h3s = b_h2b
            s_h3s = s_h2b
            load_w = w_loader(Ring(cx, "w6", 2, [128, KC, 256], BF16, es6))
            sgt = sb6("sgt", [128, 256], F32)
            b_sgt = Buf()
            t1t = sb6("t1t", [128, 256], F32)
            b_t1 = Buf()
            for blk in range(16):
                csl = slice(blk * 256, (blk + 1) * 256)
                wt, wb = load_w(w_pg, blk * 256)
                cx.dma("sp", h2b[:], h1[:, csl].rearrange("(t p) c -> p t c", p=128), [], [b_h2b], s_h2b)
                for t in range(NT):
                    tsl = slice(t * 128, (t + 1) * 128)
                    ps, pb = next_ps()
                    cx.mm(ps[:, 0:256], [(XT[:, k, tsl], wt[:, k, :]) for k in range(KC)], [wb, b_XT], pb)
                    ps2, pb2 = next_ps()
                    cx.mm(ps2[:, 0:256], [(pT[:, k2, tsl], wpp[:, k2, csl]) for k2 in range(2)], [b_pT, b_c6], pb2)
                    cx.op("act", lambda: A.activation(out=sgt[:], in_=ps[:, 0:256], func=AF.Sigmoid), [pb], [b_sgt])
                    cx.op("dve", lambda: V.tensor_tensor(out=t1t[:], in0=sgt[:], in1=ps2[:, 0:256], op=ALU.mult), [b_sgt, pb2], [b_t1])
                    cx.op("pool", lambda: G.tensor_tensor(out=h3s[:, t, :], in0=t1t[:], in1=h2b[:, t, :], op=ALU.add), [b_t1, b_h2b], [b_h3s])
                cx.dma("sp", h3[:, csl].rearrange("(t p) c -> p t c", p=128), h3s[:], [b_h3s], [b_h3], s_h3s, partial=True)
            cx.barrier()
        esX.close()
        fin = cx.semctr("fin")
        with ExitStack() as es7:
            sb7 = lambda name, shape, d: es7.enter_context(nc.sbuf_tensor(name, list(shape), d))
            gain = sb7("gainF", [128, D], F32)
            b_g = Buf()
            cx.dma("sp", gain[:], norm_final[0].partition_broadcast(128), [], [b_g], setup)
            xr_t = [sb7(f"fr{i}", [128, D], F32) for i in range(2)]
            xr_b = [Buf() for _ in range(2)]
            xr_s = [cx.semctr(f"s_fr{i}") for i in range(2)]
            yo_t = [sb7(f"fo{i}", [128, D], F32) for i in range(2)]
            yo_b = [Buf() for _ in range(2)]
            junk = sb7("junkF", [128, D], BF16)
            b_junk = Buf()
            st = sb7("stF", [128, 4 * NT], F32)
            b_st = Buf()
            for t in range(NT):
                xt, xb, xs = xr_t[t % 2], xr_b[t % 2], xr_s[t % 2]
                yo, yb = yo_t[t % 2], yo_b[t % 2]
                cx.dma("sp", xt[:], h3[t * 128:(t + 1) * 128, :], [b_h3], [xb], xs)
                c0 = st[:, 4 * t:4 * t + 1]
                c1 = st[:, 4 * t + 1:4 * t + 2]
                c2 = st[:, 4 * t + 2:4 * t + 3]
                cx.op("act", lambda: A.activation(out=junk[:], in_=xt[:], func=AF.Square, accum_out=c0), [xb], [b_junk, b_st])
                cx.op("dve", lambda: V.tensor_scalar(c1, c0, 1.0 / D, EPS, ALU.mult, ALU.add), [b_st], [b_st])
                cx.op("act", lambda: A.activation(out=c2, in_=c1, func=AF.Sqrt), [b_st], [b_st])
                cx.op("dve", lambda: V.reciprocal(c1, c2), [b_st], [b_st])
                cx.op("dve", lambda: V.scalar_tensor_tensor(out=yo[:], in0=xt[:], scalar=c1, in1=gain[:], op0=ALU.mult, op1=ALU.mult), [xb, b_st, b_g], [yb])
                cx.dma("sp", out[t * 128:(t + 1) * 128, :], yo[:], [yb], [], fin)
            nc.sync.wait_ge(fin.sem, fin.n)
            cx.barrier()
    return nc


def make_consts():
    ident = np.eye(128, dtype=np.float32)
    invf = (10000.0 ** (-np.arange(128, dtype=np.float32) / np.float32(128))).astype(np.float32).reshape(128, 1)
    jj = np.arange(128)
    maskF = (jj[None, :] >= jj[:, None]).astype(np.float32)
    maskB = (jj[:, None] > jj[None, :]).astype(np.float32)
    rmask = (np.arange(T) % 128 != 0).astype(np.float32).reshape(1, T)
    iota_row = np.arange(512, dtype=np.float32).reshape(1, 512)
    jvals = (np.arange(128)[:, None] + 128 * np.arange(4)[None, :]).astype(np.float32)
    selc = np.zeros((16, 16, 128), np.float32)
    for e in range(16):
        selc[e, e, :] = 1.0
    return {"ident": ident, "invf": invf, "maskF": maskF, "maskB": maskB, "rmask": rmask, "iota_row": iota_row, "jvals": jvals, "selc": selc}


def make_in_maps(inputs, cores):
    f = lambda k: np.asarray(inputs[k], dtype=np.float32)
    x = f("x")
    positions = np.asarray(inputs["positions"], dtype=np.int32)
    p = f("p")[0]
    consts = make_consts()
    shared = {
        "norm_mix": f("norm_mix").reshape(1, D),
        "w_in": np.ascontiguousarray(f("w_in")[0]),
        "ret_decay_logit": f("ret_decay_logit").reshape(1, 16),
        "gla_gate_w": np.ascontiguousarray(f("gla_gate_w")[0]),
        "gla_gate_b": np.ascontiguousarray(f("gla_gate_b")[0]),
        "ret_norm": f("ret_norm").reshape(1, D),
        "gla_norm": f("gla_norm").reshape(1, D),
        "w_branch": np.ascontiguousarray(f("w_branch")[0]),
        "w_out": np.ascontiguousarray(f("w_out")[0]),
        "norm_ffn": f("norm_ffn").reshape(1, D),
        "w_router": np.ascontiguousarray(f("w_router")[0]),
        "w_expert_gate": np.ascontiguousarray(f("w_expert_gate")[0]),
        "w_expert_up": np.ascontiguousarray(f("w_expert_up")[0]),
        "w_expert_down": np.ascontiguousarray(f("w_expert_down")[0]),
        "norm_ple": f("norm_ple").reshape(1, D),
        "w_ple_gate": np.ascontiguousarray(f("w_ple_gate")[0]),
        "w_ple_proj": np.ascontiguousarray(f("w_ple_proj")[0]),
        "norm_final": f("norm_final").reshape(1, D),
    }
    in_maps = []
    for c in cores:
        b, hf = c // 2, c % 2
        sl = slice(hf * T, (hf + 1) * T)
        m = dict(consts)
        m.update(shared)
        m["x"] = np.ascontiguousarray(x[b, sl])
        m["pos"] = np.ascontiguousarray(positions[b, sl]).reshape(1, T)
        m["p"] = np.ascontiguousarray(p[b, sl])
        m["flags"] = np.array([[float(hf), float(1 - hf)]], np.float32)
        in_maps.append(m)
    return in_maps


def kernel(**inputs):
    n = NCORES
    in_maps = make_in_maps(inputs, list(range(n)))
    nc = build(stage=99)
    res = run_bass_kernel_spmd(nc, in_maps, core_ids=list(range(n)))
    outs = [np.asarray(r["out"], dtype=np.float32) for r in res.results]
    return np.stack(outs, 0).reshape(n // 2, 2 * T, D)
```

```python
import numpy as np
import ml_dtypes
from contextlib import ExitStack
import concourse.bass as bass
import concourse.mybir as mybir
from concourse.bass_utils import run_bass_kernel_spmd

F32 = mybir.dt.float32
BF16 = mybir.dt.bfloat16
I32 = mybir.dt.int32
AF = mybir.ActivationFunctionType
ALU = mybir.AluOpType
AX = mybir.AxisListType

T = 2048
NT = 16
D = 4096
KC = 32
INW = 32800
EPS = 1e-6
NH = 8
NCORES = 8


class Buf:
    __slots__ = ("w", "r", "name")

    def __init__(self, name=""):
        self.w = {}
        self.r = {}
        self.name = name


class SemCtr:
    def __init__(self, sem):
        self.sem = sem
        self.n = 0


class Ctx:
    def __init__(self, nc, es):
        self.nc = nc
        self.es = es
        self.eng = {"pe": nc.tensor, "act": nc.scalar, "dve": nc.vector, "pool": nc.gpsimd, "sp": nc.sync}
        self.esem = {k: self.sem("E_" + k) for k in ("pe", "act", "dve", "pool")}
        self.ecnt = {k: 0 for k in self.esem}
        self.waited = {k: {} for k in self.eng}
        self.nsem = 0
        self.all_sc = []

    def sem(self, name):
        return self.es.enter_context(self.nc.semaphore(name))

    def semctr(self, name):
        sc = SemCtr(self.sem(name))
        self.all_sc.append(sc)
        return sc

    def barrier(self):
        for e in self.eng:
            for k, sem in self.esem.items():
                if self.ecnt[k] > 0 and k != e:
                    self._wait(e, sem, self.ecnt[k])
            for sc in self.all_sc:
                if sc.n > 0:
                    self._wait(e, sc.sem, sc.n)

    def _wait(self, e, sem, val):
        if e == "pe" and sem is self.esem["pe"]:
            return
        w = self.waited[e]
        if w.get(sem, 0) >= val:
            return
        w[sem] = val
        self.eng[e].wait_ge(sem, val)

    def deps(self, e, reads, writes):
        for b in reads:
            for sem, v in b.w.items():
                self._wait(e, sem, v)
        for b in writes:
            for sem, v in b.w.items():
                self._wait(e, sem, v)
            for sem, v in b.r.items():
                self._wait(e, sem, v)

    def _record(self, sem, val, reads, writes, partial=False):
        for b in reads:
            b.r[sem] = val
        for b in writes:
            if partial:
                b.w[sem] = val
            else:
                b.w = {sem: val}
                b.r = {}

    def op(self, e, fn, reads=(), writes=()):
        self.deps(e, reads, writes)
        ins = fn()
        self.ecnt[e] += 1
        ins.then_inc(self.esem[e], 1)
        self._record(self.esem[e], self.ecnt[e], reads, writes)
        return ins

    def mm(self, out_ap, pairs, reads, out_buf, transpose=False):
        self.deps("pe", reads, [out_buf])
        n = len(pairs)
        ins = None
        for i, (l, r) in enumerate(pairs):
            ins = self.nc.tensor.matmul(out_ap, l, r, start=(i == 0), stop=(i == n - 1))
        self.ecnt["pe"] += 1
        ins.then_inc(self.esem["pe"], 1)
        self._record(self.esem["pe"], self.ecnt["pe"], reads, [out_buf])

    def mm_multi(self, fns, reads, out_buf):
        self.deps("pe", reads, [out_buf])
        ins = None
        for f in fns:
            ins = f()
        self.ecnt["pe"] += 1
        ins.then_inc(self.esem["pe"], 1)
        self._record(self.esem["pe"], self.ecnt["pe"], reads, [out_buf])

    def dma(self, q, out_ap, in_ap, reads, writes, sc, partial=False, **kw):
        self.deps(q, reads, [] if partial else writes)
        ins = self.eng[q].dma_start(out=out_ap, in_=in_ap, **kw)
        sc.n += 16
        ins.then_inc(sc.sem, 16)
        self._record(sc.sem, sc.n, reads, writes, partial)
        return ins

    def wait_all(self, e, bufs):
        self.deps(e, bufs, [])


class Ring:
    def __init__(self, cx, name, n, shape, dt, es=None):
        es = es or cx.es
        self.t = [es.enter_context(cx.nc.sbuf_tensor(f"{name}{i}", shape, dt)) for i in range(n)]
        self.b = [Buf(f"{name}{i}") for i in range(n)]
        self.s = [cx.semctr(f"s_{name}{i}") for i in range(n)]
        self.n = n
        self.i = 0

    def next(self):
        i = self.i % self.n
        self.i += 1
        return self.t[i], self.b[i], self.s[i]


def build(stage=99, debug=None):
    nc = bass.Bass("TRN2", target_bir_lowering=False)
    dt = nc.dram_tensor

    def din(name, shape, d=F32):
        return dt(name, list(shape), d, kind="ExternalInput").ap()

    x = din("x", [T, D])
    norm_mix = din("norm_mix", [1, D])
    ident_in = din("ident", [128, 128])
    invf_in = din("invf", [128, 1])
    if stage >= 1:
        pos = din("pos", [1, T], I32)
        w_in = din("w_in", [D, INW])
    out = dt("out", [T, D], F32, kind="ExternalOutput").ap()

    def scr(name, shape, d=BF16):
        kind = "ExternalOutput" if (debug and name in debug) else "Internal"
        return dt(name, list(shape), d, kind=kind).ap()

    qkT = [scr(f"qkT{b}", [NH, 2, 2, 128, T]) for b in range(2)]
    vtm = [scr(f"v{b}", [T, D]) for b in range(2)]
    sgtm = [scr(f"sg{b}", [T, D]) for b in range(2)]
    sbgT = scr("sbgT", [2 * KC, 128, T])
    glrT_d = scr("glrT", [2, 16, T], F32)
    b_qkT = [Buf() for _ in range(2)]
    b_v = [Buf() for _ in range(2)]
    b_sg = [Buf() for _ in range(2)]
    b_sbgT = Buf()
    b_glr = Buf()

    with ExitStack() as es:
        cx = Ctx(nc, es)
        sb = lambda name, shape, d: es.enter_context(nc.sbuf_tensor(name, list(shape), d))
        setup = cx.semctr("setup")
        b_const = Buf("const")
        ident = sb("identb", [128, 128], BF16)
        cx.dma("pool", ident[:], ident_in, [], [b_const], setup, partial=True)
        invf = sb("invf_s", [128, 1], F32)
        cx.dma("sp", invf[:], invf_in, [], [b_const], setup, partial=True)
        psb = [es.enter_context(nc.psum_tensor(f"ps{i}", [128, 512], F32)) for i in range(8)]
        b_ps = [Buf(f"ps{i}") for i in range(8)]
        psi = [0]

        def next_ps():
            i = psi[0] % 8
            psi[0] += 1
            return psb[i], b_ps[i]

        esX = es.enter_context(ExitStack())
        XT = esX.enter_context(nc.sbuf_tensor("AT", [128, KC, T], BF16))
        b_XT = Buf("AT")

        def norm_T(pfx, src, src_bufs, gain_row, XT_, b_XT_, tm_dst=None, b_tm=None):
            with ExitStack() as es0:
                sb0 = lambda name, shape, d: es0.enter_context(nc.sbuf_tensor(pfx + name, list(shape), d))
                gain = sb0("gain", [128, D], F32)
                b_g = Buf()
                cx.dma("sp", gain[:], gain_row.partition_broadcast(128), [], [b_g], setup)
                xr_t = [sb0(f"xr{i}", [128, D], F32) for i in range(2)]
                xr_b = [Buf() for _ in range(2)]
                xr_s = [cx.semctr(f"s_{pfx}xr{i}") for i in range(2)]
                junk = sb0("junk", [128, D], BF16)
                b_junk = Buf()
                xnb_t = [sb0(f"xnb{i}", [128, D], BF16) for i in range(2)]
                xnb_b = [Buf() for _ in range(2)]
                xnb_s = [cx.semctr(f"s_{pfx}xnb{i}") for i in range(2)]
                st = sb0("st", [128, 4 * NT], F32)
                b_st = Buf()
                for t in range(NT):
                    xt, xb, xs = xr_t[t % 2], xr_b[t % 2], xr_s[t % 2]
                    xnb, b_xnb, s_xnb = xnb_t[t % 2], xnb_b[t % 2], xnb_s[t % 2]
                    cx.dma("sp", xt[:], src[t * 128:(t + 1) * 128, :], src_bufs, [xb], xs)
                    c0 = st[:, 4 * t:4 * t + 1]
                    c1 = st[:, 4 * t + 1:4 * t + 2]
                    c2 = st[:, 4 * t + 2:4 * t + 3]
                    cx.op("act", lambda: nc.scalar.activation(out=junk[:], in_=xt[:], func=AF.Square, accum_out=c0), [xb], [b_junk, b_st])
                    cx.op("dve", lambda: nc.vector.tensor_scalar(c1, c0, 1.0 / D, EPS, ALU.mult, ALU.add), [b_st], [b_st])
                    cx.op("act", lambda: nc.scalar.activation(out=c2, in_=c1, func=AF.Sqrt), [b_st], [b_st])
                    cx.op("dve", lambda: nc.vector.reciprocal(c1, c2), [b_st], [b_st])
                    cx.op("dve", lambda: nc.vector.scalar_tensor_tensor(out=xnb[:], in0=xt[:], scalar=c1, in1=gain[:], op0=ALU.mult, op1=ALU.mult),
                          [xb, b_st, b_g], [b_xnb])
                    if tm_dst is not None:
                        cx.dma("sp", tm_dst[t * 128:(t + 1) * 128, :], xnb[:], [b_xnb], [b_tm], s_xnb, partial=True)
                    for g in range(4):
                        ps, pb = next_ps()
                        psv = ps[:].bitcast(BF16)
                        pst = psv[:, 0:1024].rearrange("p (a b) -> p a b", a=8)
                        cx.mm_multi([(lambda j=j: nc.tensor.transpose(pst[:, j, :], xnb[:, (g * 8 + j) * 128:(g * 8 + j + 1) * 128], ident[:])) for j in range(8)],
                                    [b_xnb, b_const], pb)
                        dst = XT_[:, g * 8:(g + 1) * 8, t * 128:(t + 1) * 128]
                        if g % 2 == 0:
                            cx.op("act", lambda: nc.scalar.copy(out=dst, in_=pst), [pb], [b_XT_])
                        else:
                            cx.op("dve", lambda: nc.vector.tensor_copy(out=dst, in_=pst), [pb], [b_XT_])
                cx.barrier()

        norm_T("n0", x, [], norm_mix[0], XT, b_XT)
        if debug == "XT":
            dbg = dt("dbg", [128, KC, T], BF16, kind="ExternalOutput").ap()
            fin = cx.semctr("fin")
            cx.dma("sp", dbg, XT[:], [b_XT], [], fin)
            nc.sync.wait_ge(fin.sem, fin.n)
            return nc
        cx.barrier()
        if stage < 1:
            return nc
        TWO_PI = float(2 * np.pi)
        PI = float(np.pi)
        with ExitStack() as es1:
            sb1 = lambda name, shape, d: es1.enter_context(nc.sbuf_tensor(name, list(shape), d))
            cosb = sb1("cosb", [128, T], BF16)
            sinb = sb1("sinb", [128, T], BF16)
            b_tab = Buf("tab")
            with ExitStack() as est:
                sbt = lambda name, shape, d: est.enter_context(nc.sbuf_tensor(name, list(shape), d))
                posi = sbt("posi", [128, T], I32)
                ang = sbt("ang", [128, T], F32)
                a2 = sbt("a2", [128, T], F32)
                ki = sbt("ki", [128, T], I32)
                kf = sbt("kf", [128, T], F32)
                msk = sbt("msk", [128, T], F32)
                b_t = Buf("tmp_tab")
                cx.dma("sp", posi[:], pos[0].partition_broadcast(128), [], [b_t], setup)
                V = nc.vector
                cx.op("dve", lambda: V.tensor_copy(out=ang[:], in_=posi[:]), [b_t], [b_t])
                cx.op("dve", lambda: V.tensor_scalar(ang[:], ang[:], invf[:, 0:1], None, ALU.mult), [b_t, b_const], [b_t])
                for which, dst in ((0, sinb), (1, cosb)):
                    cx.op("dve", lambda: V.tensor_scalar(a2[:], ang[:], (PI / 2 if which else 0.0), None, ALU.add), [b_t], [b_t])
                    cx.op("dve", lambda: V.tensor_scalar(kf[:], a2[:], 1.0 / TWO_PI, None, ALU.mult), [b_t], [b_t])
                    cx.op("dve", lambda: V.tensor_copy(out=ki[:], in_=kf[:]), [b_t], [b_t])
                    cx.op("dve", lambda: V.tensor_copy(out=kf[:], in_=ki[:]), [b_t], [b_t])
                    cx.op("dve", lambda: V.scalar_tensor_tensor(out=a2[:], in0=kf[:], scalar=-TWO_PI, in1=a2[:], op0=ALU.mult, op1=ALU.add), [b_t], [b_t])
                    cx.op("dve", lambda: V.tensor_single_scalar(msk[:], a2[:], PI, ALU.is_gt), [b_t], [b_t])
                    cx.op("dve", lambda: V.scalar_tensor_tensor(out=a2[:], in0=msk[:], scalar=-TWO_PI, in1=a2[:], op0=ALU.mult, op1=ALU.add), [b_t], [b_t])
                    cx.op("dve", lambda: V.tensor_single_scalar(msk[:], a2[:], -PI, ALU.is_lt), [b_t], [b_t])
                    cx.op("dve", lambda: V.scalar_tensor_tensor(out=a2[:], in0=msk[:], scalar=TWO_PI, in1=a2[:], op0=ALU.mult, op1=ALU.add), [b_t], [b_t])
                    cx.op("dve", lambda: V.tensor_scalar(a2[:], a2[:], PI, -PI, ALU.min, ALU.max), [b_t], [b_t])
                    cx.op("act", lambda: nc.scalar.activation(out=dst[:], in_=a2[:], func=AF.Sin), [b_t], [b_tab])
            cx.barrier()
            wring = Ring(cx, "w", 2, [128, KC, 256], BF16, es1)
            sring = Ring(cx, "stg", 2, [128, 4096], BF16, es1)
            ta = sb1("rot_a", [128, 512], F32)
            tb_ = sb1("rot_b", [128, 512], F32)
            b_rt = Buf("rot_tmp")
            glr_s = sb1("glr_s", [16, 2, T], F32)
            b_glrs = Buf()

            def load_w(c0, ncols):
                wt, wb, ws = wring.next()
                src = w_in[:, c0:c0 + ncols].rearrange("(kc p) c -> p kc c", p=128)
                cx.dma("pool", wt[:, :, 0:ncols], src, [], [wb], ws)
                return wt, wb

            def fm_block(c0, kind, scale, dst_ap, dst_buf):
                wt, wb = load_w(c0, 256)
                stt, stb, sts = sring.next()
                stv = stt[:].rearrange("p (a b) -> p a b", a=2)
                for tb in range(4):
                    tsl = slice(tb * 512, (tb + 1) * 512)
                    pss = []
                    for dch in range(2):
                        ps, pb = next_ps()
                        cx.mm(ps[:, 0:512], [(wt[:, k, dch * 128:(dch + 1) * 128], XT[:, k, tsl]) for k in range(KC)], [wb, b_XT], pb)
                        pss.append((ps, pb))
                    if kind == "rot":
                        (p1, b1), (p2, b2) = pss
                        V = nc.vector
                        cx.op("dve", lambda: V.tensor_tensor(out=ta[:], in0=p1[:, 0:512], in1=cosb[:, tsl], op=ALU.mult), [b1, b_tab], [b_rt])
                        cx.op("dve", lambda: V.tensor_tensor(out=tb_[:], in0=p2[:, 0:512], in1=sinb[:, tsl], op=ALU.mult), [b2, b_tab], [b_rt])
                        cx.op("dve", lambda: V.scalar_tensor_tensor(out=stv[:, 0, tsl], in0=ta[:], scalar=scale, in1=tb_[:], op0=ALU.mult, op1=ALU.subtract) if False else
                              V.tensor_tensor(out=ta[:], in0=ta[:], in1=tb_[:], op=ALU.subtract), [b_rt], [b_rt])
                        cx.op("act", lambda: nc.scalar.activation(out=stv[:, 0, tsl], in_=ta[:], func=AF.Copy, scale=scale), [b_rt], [stb])
                        cx.op("dve", lambda: V.tensor_tensor(out=tb_[:], in0=p1[:, 0:512], in1=sinb[:, tsl], op=ALU.mult), [b1, b_tab], [b_rt])
                        cx.op("dve", lambda: V.tensor_tensor(out=ta[:], in0=p2[:, 0:512], in1=cosb[:, tsl], op=ALU.mult), [b2, b_tab, stb], [b_rt])
                        cx.op("dve", lambda: V.tensor_tensor(out=ta[:], in0=ta[:], in1=tb_[:], op=ALU.add), [b_rt], [b_rt])
                        cx.op("act", lambda: nc.scalar.activation(out=stv[:, 1, tsl], in_=ta[:], func=AF.Copy, scale=scale), [b_rt], [stb])
                    else:
                        fn = AF.Sigmoid if kind == "sig" else AF.Copy
                        for dch, (ps, pb) in enumerate(pss):
                            cx.op("act", lambda: nc.scalar.activation(out=stv[:, dch, tsl], in_=ps[:, 0:512], func=fn, scale=scale), [pb], [stb])
                cx.dma("sp", dst_ap.rearrange("a p t -> p a t"), stv, [stb], [dst_buf], sts, partial=True)

            def tm_block(c0, kind, dst_ap, dst_buf):
                wt, wb = load_w(c0, 256)
                stt, stb, sts = sring.next()
                stv = stt[:].rearrange("p (a b) -> p a b", a=NT)
                for t in range(NT):
                    ps, pb = next_ps()
                    cx.mm(ps[:, 0:256], [(XT[:, k, t * 128:(t + 1) * 128], wt[:, k, :]) for k in range(KC)], [wb, b_XT], pb)
                    if kind == "silu":
                        cx.op("act", lambda: nc.scalar.activation(out=stv[:, t, :], in_=ps[:, 0:256], func=AF.Silu), [pb], [stb])
                    elif t % 2 == 0:
                        cx.op("act", lambda: nc.scalar.copy(out=stv[:, t, :], in_=ps[:, 0:256]), [pb], [stb])
                    else:
                        cx.op("dve", lambda: nc.vector.tensor_copy(out=stv[:, t, :], in_=ps[:, 0:256]), [pb], [stb])
                cx.dma("sp", dst_ap.rearrange("(t p) c -> p t c", p=128), stv, [stb], [dst_buf], sts, partial=True)

            OFF = {"rq": 0, "rk": 2048, "rv": 4096, "rg": 8192, "gq": 12288, "gk": 14336, "gv": 16384, "gg": 20480, "glr": 24576, "bg": 24608}
            nblk = NH if stage >= 2 else 1
            for h in range(nblk):
                fm_block(OFF["rq"] + 256 * h, "rot", 1.0, qkT[0][h, 0], b_qkT[0])
            for h in range(nblk):
                fm_block(OFF["rk"] + 256 * h, "rot", 1.0 / 16, qkT[0][h, 1], b_qkT[0])
            for h in range(nblk):
                fm_block(OFF["gq"] + 256 * h, "copy", 1.0 / 16, qkT[1][h, 0], b_qkT[1])
            for h in range(nblk):
                fm_block(OFF["gk"] + 256 * h, "copy", 1.0, qkT[1][h, 1], b_qkT[1])
            for j in range(2 * nblk):
                tm_block(OFF["rv"] + 256 * j, "copy", vtm[0][:, 256 * j:256 * (j + 1)], b_v[0])
            for j in range(2 * nblk):
                tm_block(OFF["gv"] + 256 * j, "copy", vtm[1][:, 256 * j:256 * (j + 1)], b_v[1])
            for j in range(2 * nblk):
                tm_block(OFF["rg"] + 256 * j, "silu", sgtm[0][:, 256 * j:256 * (j + 1)], b_sg[0])
            for j in range(2 * nblk):
                tm_block(OFF["gg"] + 256 * j, "silu", sgtm[1][:, 256 * j:256 * (j + 1)], b_sg[1])
            for j in range(4 * nblk):
                fm_block(OFF["bg"] + 256 * j, "sig", 1.0, sbgT[2 * j:2 * j + 2], b_sbgT)
            wt, wb = load_w(OFF["glr"], 32)
            glr_sc = cx.semctr("s_glr")
            for z in range(2):
                for tb in range(4):
                    tsl = slice(tb * 512, (tb + 1) * 512)
                    ps, pb = next_ps()
                    cx.mm(ps[0:16, 0:512], [(wt[:, k, z * 16:(z + 1) * 16], XT[:, k, tsl]) for k in range(KC)], [wb, b_XT], pb)
                    cx.op("act", lambda: nc.scalar.copy(out=glr_s[:, z, tsl], in_=ps[0:16, 0:512]), [pb], [b_glrs])
            cx.dma("sp", glrT_d.rearrange("z r t -> r z t"), glr_s[:], [b_glrs], [b_glr], glr_sc, partial=True)
            allb = b_qkT + b_v + b_sg + [b_sbgT, b_glr]
        cx.barrier()
        esX.close()
        if stage < 3:
            fin = cx.semctr("fin")
            cx.dma("sp", out[0:128, 0:128], ident_in, allb, [], fin)
            nc.sync.wait_ge(fin.sem, fin.n)
            return nc
        nhead = NH if stage >= 4 or debug is None else 1
        XROWS = 2 * NH * 2 * 2 * 128
        XCH = 1024
        NXC = XROWS // XCH
        xs_src = [dt(f"xs_src{i}", [XCH, 512], BF16).ap() for i in range(NXC)]
        xs_dst = [dt(f"xs_dst{i}", [2 * XCH, 512], BF16).ap() for i in range(NXC)]
        b_xsrc = Buf("xs_src")
        b_xdst = Buf("xs_dst")
        branchT = [scr(f"branchT{b}", [KC, 128, T]) for b in range(2)]
        b_brT = [Buf() for _ in range(2)]
        with ExitStack() as es2:
            sb2 = lambda name, shape, d: es2.enter_context(nc.sbuf_tensor(name, list(shape), d))
            V = nc.vector
            A = nc.scalar
            G = nc.gpsimd
            identf = sb2("identf", [128, 128], F32)
            maskF = sb2("maskF_s", [128, 128], F32)
            maskB = sb2("maskB_s", [128, 128], F32)
            rmask = sb2("rmask_s", [128, T], F32)
            flags = sb2("flags_s", [128, 2], F32)
            dl = sb2("dl", [128, 16], F32)
            negb = sb2("negb", [128, 32], F32)
            gbr = sb2("gbr", [32, 128], F32)
            b_c2 = Buf("c2")
            cx.dma("sp", identf[:], ident_in, [], [b_c2], setup, partial=True)
            cx.dma("sp", maskF[:], din("maskF", [128, 128]), [], [b_c2], setup, partial=True)
            cx.dma("sp", maskB[:], din("maskB", [128, 128]), [], [b_c2], setup, partial=True)
            cx.dma("sp", rmask[:], din("rmask", [1, T])[0].partition_broadcast(128), [], [b_c2], setup, partial=True)
            cx.dma("sp", flags[:], din("flags", [1, 2])[0].partition_broadcast(128), [], [b_c2], setup, partial=True)
            cx.dma("sp", dl[:], din("ret_decay_logit", [1, 16])[0].partition_broadcast(128), [], [b_c2], setup, partial=True)
            gate_b = din("gla_gate_b", [2, 2048])
            gate_w = din("gla_gate_w", [2, 16, 2048])
            ret_norm = din("ret_norm", [1, 4096])
            gla_norm = din("gla_norm", [1, 4096])
            cx.dma("sp", gbr[:], gate_b.rearrange("z (c p) -> (z c) p", p=128), [], [b_c2], setup, partial=True)
            cx.op("act", lambda: A.activation(out=dl[:], in_=dl[:], func=AF.Exp, scale=-1.0), [b_c2], [b_c2])
            cx.op("act", lambda: A.activation(out=dl[:], in_=dl[:], func=AF.Ln, bias=1.0), [b_c2], [b_c2])
            ps, pb = next_ps()
            cx.mm_multi([lambda: nc.tensor.transpose(ps[:, 0:32], gbr[:], identf[0:32, 0:32])], [b_c2], pb)
            cx.op("act", lambda: A.activation(out=negb[:], in_=ps[:, 0:32], func=AF.Copy, scale=-1.0), [pb], [b_c2])

            glr = sb2("glr2", [16, 2, T], F32)
            b_glr2 = Buf()
            ld = cx.semctr("s_ld2")
            cx.dma("sp", glr[:], glrT_d.rearrange("z r t -> r z t"), [b_glr], [b_glr2], ld)
            gw = sb2("gw", [16, 2, 256], F32)
            b_gw = Buf()
            qk = sb2("qk", [128, 2, 2, T], BF16)
            b_qk = Buf()
            vv = sb2("vv", [128, NT, 512], BF16)
            b_vv = Buf()
            spt = sb2("spt", [128, T], F32)
            cum = sb2("cum", [128, T], F32)
            Et = sb2("Et", [128, T], F32)
            b_dec = Buf("dec")
            qh = [sb2(f"qh{z}", [128, 2, T], BF16) for z in range(2)]
            kh = [sb2(f"kh{z}", [128, 2, T], BF16) for z in range(2)]
            b_qh = [Buf() for _ in range(2)]
            b_kh = [Buf() for _ in range(2)]
            ktm = [sb2(f"ktm{z}", [128, NT, 256], BF16) for z in range(2)]
            b_ktm = [Buf() for _ in range(2)]
            sdec = [sb2(f"sdec{z}", [128, 2, NT], F32) for z in range(2)]
            b_sdec = [Buf() for _ in range(2)]
            R = [sb2(f"R{z}", [128, 2, 512], F32) for z in range(2)]
            Rb = [sb2(f"Rb{z}", [128, 2, 512], BF16) for z in range(2)]
            b_R = [Buf() for _ in range(2)]
            b_Rb = [Buf() for _ in range(2)]
            Rtmp = sb2("Rtmp", [128, 512], F32)
            b_Rtmp = Buf()
            cum3 = cum[:].rearrange("p (n c) -> p n c", c=128)
            spt3 = spt[:].rearrange("p (n c) -> p n c", c=128)
            SC = (1.0, 1.0 / 16)

            def prep(b, h, need_q):
                cx.dma("sp", qk[:], qkT[b][h].rearrange("a c p t -> p a c t"), [b_qkT[b]], [b_qk], ld)
                cx.dma("sp", vv[:], vtm[b][:, h * 512:(h + 1) * 512].rearrange("(n p) c -> p n c", p=128), [b_v[b]], [b_vv], ld)
                if b == 1:
                    cx.dma("sp", gw[:], gate_w[:, :, h * 256:(h + 1) * 256].rearrange("z r c -> r z c"), [], [b_gw], ld)
                s = SC[b]
                for z in range(2):
                    for dch in range(2):
                        if b == 0:
                            cx.op("act", lambda: A.activation(out=spt[:], in_=rmask[:], func=AF.Identity, scale=0.0, bias=dl[:, z * 8 + h:z * 8 + h + 1]),
                                  [b_c2], [b_dec])
                        else:
                            col = z * 16 + h * 2 + dch
                            for tb in range(4):
                                tsl = slice(tb * 512, (tb + 1) * 512)
                                ps, pb = next_ps()
                                cx.mm(ps[:, 0:512], [(gw[:, z, dch * 128:(dch + 1) * 128], glr[:, z, tsl])], [b_gw, b_glr2], pb)
                                cx.op("act", lambda: A.activation(out=spt[:, tsl], in_=ps[:, 0:512], func=AF.Exp, scale=-1.0, bias=negb[:, col:col + 1]),
                                      [pb, b_c2], [b_dec])
                            cx.op("act", lambda: A.activation(out=spt[:], in_=spt[:], func=AF.Ln, bias=1.0), [b_dec], [b_dec])
                        cx.op("dve", lambda: V.tensor_tensor_scan(out=cum[:], data0=rmask[:], data1=spt[:], initial=0.0, op0=ALU.mult, op1=ALU.add),
                              [b_dec, b_c2], [b_dec])
                        cx.op("act", lambda: A.activation(out=sdec[z][:, dch, :], in_=cum3[:, :, 127], func=AF.Exp, scale=-s), [b_dec], [b_sdec[z]])
                        if z == 1:
                            cx.op("dve", lambda: V.tensor_tensor(out=cum[:], in0=cum[:], in1=spt[:], op=ALU.subtract), [b_dec], [b_dec])
                        sq = -s if z == 0 else s
                        if need_q:
                            cx.op("act", lambda: A.activation(out=Et[:], in_=cum[:], func=AF.Exp, scale=sq), [b_dec], [b_dec])
                            cx.op("dve", lambda: V.tensor_tensor(out=qh[z][:, dch, :], in0=qk[:, 0, dch, :], in1=Et[:], op=ALU.mult), [b_dec, b_qk], [b_qh[z]])
                        cx.op("act", lambda: A.activation(out=Et[:], in_=cum[:], func=AF.Exp, scale=-sq), [b_dec, b_qh[z]], [b_dec])
                        cx.op("dve", lambda: V.tensor_tensor(out=kh[z][:, dch, :], in0=qk[:, 1, dch, :], in1=Et[:], op=ALU.mult), [b_dec, b_qk], [b_kh[z]])
                    for n in range(NT):
                        ps, pb = next_ps()
                        pv = ps[:].bitcast(BF16)
                        cx.mm_multi([(lambda d_=d_: nc.tensor.transpose(pv[:, d_ * 128:(d_ + 1) * 128], kh[z][:, d_, n * 128:(n + 1) * 128], ident[:])) for d_ in range(2)],
                                    [b_kh[z], b_const], pb)
                        if n % 2 == 0:
                            cx.op("act", lambda: A.copy(out=ktm[z][:, n, :], in_=pv[:, 0:256]), [pb], [b_ktm[z]])
                        else:
                            cx.op("dve", lambda: V.tensor_copy(out=ktm[z][:, n, :], in_=pv[:, 0:256]), [pb], [b_ktm[z]])

            def kv_update(z, n, form_f):
                for dch in range(2):
                    ps, pb = next_ps()
                    cx.mm(ps[:, 0:512], [(ktm[z][:, n, dch * 128:(dch + 1) * 128], vv[:, n, :])], [b_ktm[z], b_vv], pb)
                    if form_f:
                        cx.op("dve", lambda: V.tensor_tensor(out=Rtmp[:], in0=ps[:, 0:512], in1=R[z][:, dch, :], op=ALU.add), [pb, b_R[z]], [b_Rtmp])
                        cx.op("act", lambda: A.activation(out=R[z][:, dch, :], in_=Rtmp[:], func=AF.Copy, scale=sdec[z][:, dch, n:n + 1]), [b_Rtmp, b_sdec[z]], [b_R[z]])
                    else:
                        cx.op("dve", lambda: V.tensor_tensor(out=R[z][:, dch, :], in0=ps[:, 0:512], in1=R[z][:, dch, :], op=ALU.add), [pb, b_R[z]], [b_R[z]])

            def scale_state(z, n):
                for dch in range(2):
                    cx.op("act", lambda: A.activation(out=R[z][:, dch, :], in_=R[z][:, dch, :], func=AF.Copy, scale=sdec[z][:, dch, n:n + 1]), [b_R[z], b_sdec[z]], [b_R[z]])

            def xloc(b, h, z):
                base = ((b * NH + h) * 2 + z) * 256
                return base // XCH, base % XCH

            stA = cx.semctr("s_stA")
            for b in range(2):
                for h in range(nhead):
                    prep(b, h, False)
                    for z in range(2):
                        cx.op("pool", lambda: G.memset(R[z][:], 0.0), [], [b_R[z]])
                    for n in range(NT):
                        kv_update(0, n, True)
                        scale_state(1, NT - 1 - n)
                        kv_update(1, NT - 1 - n, False)
                    for z in range(2):
                        cx.op("dve", lambda: V.tensor_copy(out=Rb[z][:], in_=R[z][:]), [b_R[z]], [b_Rb[z]])
                        ci, r0 = xloc(b, h, z)
                        cx.dma("sp", xs_src[ci][r0:r0 + 256, :].rearrange("(c p) f -> p c f", p=128), Rb[z][:], [b_Rb[z]], [b_xsrc], stA, partial=True)
            cx.deps("pool", [b_xsrc], [])
            ccs = cx.sem("ccsem")
            for i in range(NXC):
                nc.gpsimd.collective_compute("AllGather", ALU.bypass, replica_groups=[[2 * r, 2 * r + 1] for r in range(NCORES // 2)],
                                             ins=[xs_src[i].opt()], outs=[xs_dst[i].opt()]).then_inc(ccs)
                nc.gpsimd.wait_ge(ccs, i + 1)
            for e in ("pool", "sp"):
                cx.eng[e].wait_ge(ccs, NXC)
            o_acc = sb2("o_acc", [128, NT, 512], F32)
            b_o = Buf()
            b_oc = [Buf() for _ in range(NT)]
            brs = sb2("brs", [128, 4, T], BF16)
            b_brs = Buf()
            sgc = [sb2(f"sgc{i}", [128, 512], BF16) for i in range(2)]
            b_sgc = [Buf() for _ in range(2)]
            s_sgc = [cx.semctr(f"s_sgc{i}") for i in range(2)]
            gn = sb2("gn", [128, 512], F32)
            b_gn = Buf()
            PT = sb2("PT", [128, 128], BF16)
            b_PT = Buf()
            pt1 = sb2("pt1", [128, 128], F32)
            pt2 = sb2("pt2", [128, 128], F32)
            b_pt = Buf()
            stt = sb2("stt", [128, 8], F32)
            b_stt = Buf()
            yn = sb2("yn", [128, 512], F32)
            ynb = sb2("ynb", [128, 512], BF16)
            b_yn = Buf()
            junk2 = sb2("junk2", [128, 512], BF16)
            b_j2 = Buf()
            stB = cx.semctr("s_stB")
            sgi = [0]
            for b in range(2):
                for h in range(nhead):
                    prep(b, h, True)
                    nrm = ret_norm if b == 0 else gla_norm
                    cx.dma("sp", gn[:], nrm[0, h * 512:(h + 1) * 512].partition_broadcast(128), [], [b_gn], ld)
                    for z in range(2):
                        ci, r0 = xloc(b, h, z)
                        r0 += z * XCH
                        cx.dma("sp", Rb[z][:], xs_dst[ci][r0:r0 + 256, :].rearrange("(c p) f -> p c f", p=128), [], [b_Rb[z]], ld)
                        cx.op("dve", lambda: V.tensor_scalar(R[z][:], Rb[z][:], flags[:, z:z + 1], None, ALU.mult), [b_Rb[z], b_c2], [b_R[z]])

                    def finalize(n):
                        csl = slice(n * 128, (n + 1) * 128)
                        c = lambda i: stt[:, i:i + 1]
                        cx.op("act", lambda: A.activation(out=junk2[:], in_=yn[:], func=AF.Identity, accum_out=c(0)), [b_yn], [b_j2, b_stt])
                        cx.op("act", lambda: A.activation(out=junk2[:], in_=yn[:], func=AF.Square, accum_out=c(1)), [b_yn], [b_j2, b_stt])
                        cx.op("dve", lambda: V.tensor_scalar(c(2), c(0), 1.0 / 512, None, ALU.mult), [b_stt], [b_stt])
                        cx.op("dve", lambda: V.tensor_scalar(c(3), c(1), 1.0 / 512, EPS, ALU.mult, ALU.add), [b_stt], [b_stt])
                        if b == 0:
                            cx.op("dve", lambda: V.tensor_tensor(out=c(4), in0=c(2), in1=c(2), op=ALU.mult), [b_stt], [b_stt])
                            cx.op("dve", lambda: V.tensor_tensor(out=c(3), in0=c(3), in1=c(4), op=ALU.subtract), [b_stt], [b_stt])
                        cx.op("act", lambda: A.activation(out=c(5), in_=c(3), func=AF.Sqrt), [b_stt], [b_stt])
                        cx.op("dve", lambda: V.reciprocal(c(6), c(5)), [b_stt], [b_stt])
                        if b == 0:
                            cx.op("dve", lambda: V.tensor_scalar(yn[:], yn[:], c(2), c(6), ALU.subtract, ALU.mult), [b_stt, b_yn], [b_yn])
                        else:
                            cx.op("dve", lambda: V.tensor_scalar(yn[:], yn[:], c(6), None, ALU.mult), [b_stt, b_yn], [b_yn])
                        i = sgi[0] % 2
                        sgi[0] += 1
                        cx.dma("sp", sgc[i][:], sgtm[b][n * 128:(n + 1) * 128, h * 512:(h + 1) * 512], [b_sg[b]], [b_sgc[i]], s_sgc[i])
                        cx.op("dve", lambda: V.tensor_tensor(out=yn[:], in0=yn[:], in1=gn[:], op=ALU.mult), [b_yn, b_gn], [b_yn])
                        cx.op("dve", lambda: V.tensor_tensor(out=ynb[:], in0=yn[:], in1=sgc[i][:], op=ALU.mult), [b_yn, b_sgc[i]], [b_yn])
                        ps3, pb3 = next_ps()
                        pv = ps3[:].bitcast(BF16)
                        cx.mm_multi([(lambda cc=cc: nc.tensor.transpose(pv[:, cc * 128:(cc + 1) * 128], ynb[:, cc * 128:(cc + 1) * 128], ident[:])) for cc in range(4)],
                                    [b_yn, b_const], pb3)
                        cx.op("act", lambda: A.copy(out=brs[:, :, csl], in_=pv[:, 0:512].rearrange("p (a b) -> p a b", a=4)), [pb3], [b_brs])

                    def land(n, ps2, pb2, first):
                        if first:
                            cx.op("act", lambda: A.copy(out=o_acc[:, n, :], in_=ps2[:, 0:512]), [pb2], [b_oc[n]])
                        else:
                            cx.op("dve", lambda: V.tensor_tensor(out=yn[:], in0=ps2[:, 0:512], in1=o_acc[:, n, :], op=ALU.add), [pb2, b_oc[n]], [b_yn])

                    def fwd_step(n, first):
                        csl = slice(n * 128, (n + 1) * 128)
                        cx.op("dve", lambda: V.tensor_copy(out=Rb[0][:], in_=R[0][:]), [b_R[0]], [b_Rb[0]])
                        ps, pb = next_ps()
                        cx.mm(ps[:, 0:128], [(kh[0][:, d_, csl], qh[0][:, d_, csl]) for d_ in range(2)], [b_kh[0], b_qh[0]], pb)
                        cx.mm(ps[:, 128:256], [(kh[1][:, d_, csl], qh[1][:, d_, csl]) for d_ in range(2)], [b_kh[1], b_qh[1]], pb)
                        cx.op("dve", lambda: V.tensor_tensor(out=pt1[:], in0=ps[:, 0:128], in1=maskF[:], op=ALU.mult), [pb, b_c2], [b_pt])
                        cx.op("dve", lambda: V.tensor_tensor(out=pt2[:], in0=ps[:, 128:256], in1=maskB[:], op=ALU.mult), [pb, b_c2], [b_pt])
                        cx.op("dve", lambda: V.tensor_tensor(out=PT[:], in0=pt1[:], in1=pt2[:], op=ALU.add), [b_pt], [b_PT])
                        ps2, pb2 = next_ps()
                        cx.mm(ps2[:, 0:512], [(PT[:], vv[:, n, :])] + [(qh[0][:, d_, csl], Rb[0][:, d_, :]) for d_ in range(2)],
                              [b_PT, b_vv, b_qh[0], b_Rb[0]], pb2)
                        land(n, ps2, pb2, first)
                        kv_update(0, n, True)
                        if not first:
                            finalize(n)

                    def bwd_step(n, first):
                        csl = slice(n * 128, (n + 1) * 128)
                        scale_state(1, n)
                        cx.op("dve", lambda: V.tensor_copy(out=Rb[1][:], in_=R[1][:]), [b_R[1]], [b_Rb[1]])
                        ps2, pb2 = next_ps()
                        cx.mm(ps2[:, 0:512], [(qh[1][:, d_, csl], Rb[1][:, d_, :]) for d_ in range(2)], [b_qh[1], b_Rb[1]], pb2)
                        land(n, ps2, pb2, first)
                        kv_update(1, n, False)
                        if not first:
                            finalize(n)

                    for i_ in range(NT):
                        fwd_step(i_, i_ < NT // 2)
                        bwd_step(NT - 1 - i_, i_ < NT // 2)
                    cx.dma("sp", branchT[b][h * 4:(h + 1) * 4].rearrange("a p t -> p a t"), brs[:], [b_brs], [b_brT[b]], stB, partial=True)
        cx.barrier()
        if stage < 4:
            fin = cx.semctr("fin")
            cx.dma("sp", out[0:128, 0:128], ident_in, b_brT, [], fin)
            nc.sync.wait_ge(fin.sem, fin.n)
            return nc
        w_branch = din("w_branch", [2, D, D])
        w_out = din("w_out", [D, D])
        norm_ffn = din("norm_ffn", [1, D])
        m0T = scr("m0T", [KC, 128, T])
        mergedT = scr("mergedT", [KC, 128, T])
        h1 = scr("h1", [T, D], F32)
        xn2tm = scr("xn2tm", [T, D])
        b_m0 = Buf()
        b_mT = Buf()
        b_h1 = Buf()
        b_xn2tm = Buf()
        V = nc.vector
        A = nc.scalar
        G = nc.gpsimd
        esX = es.enter_context(ExitStack())
        XT = esX.enter_context(nc.sbuf_tensor("AT3", [128, KC, T], BF16))
        b_XT = Buf("AT3")
        ldx = cx.semctr("s_ldx")

        def load_XT(src, src_buf):
            for g in range(4):
                cx.dma("sp", XT[:, g * 8:(g + 1) * 8, :], src[g * 8:(g + 1) * 8].rearrange("k p t -> p k t"), [src_buf], [b_XT], ldx, partial=(g > 0))

        def w_loader(ring):
            def load_w(W, c0):
                wt, wb, ws = ring.next()
                cx.dma("pool", wt[:], W[:, c0:c0 + 256].rearrange("(kc p) c -> p kc c", p=128), [], [wb], ws)
                return wt, wb
            return load_w

        with ExitStack() as es3:
            sb3 = lambda name, shape, d: es3.enter_context(nc.sbuf_tensor(name, list(shape), d))
            load_w = w_loader(Ring(cx, "w3", 2, [128, KC, 256], BF16, es3))
            with ExitStack() as es3a:
                sb3a = lambda name, shape, d: es3a.enter_context(nc.sbuf_tensor(name, list(shape), d))
                sring = Ring(cx, "stg3", 2, [128, 2, T], BF16, es3a)
                sbg1 = sb3a("sbg1", [128, 2, T], BF16)
                b_sbg1 = Buf()
                s_sbg1 = cx.semctr("s_sbg1")
                m0b = sb3a("m0b", [128, 2, T], BF16)
                b_m0b = Buf()
                s_m0b = cx.semctr("s_m0b")
                gtmp = sb3a("gtmp", [128, 512], F32)
                b_gtmp = Buf()
                for b in range(2):
                    load_XT(branchT[b], b_brT[b])
                    for blk in range(16):
                        wt, wb = load_w(w_branch[b], blk * 256)
                        cx.dma("sp", sbg1[:], sbgT[b * KC + 2 * blk:b * KC + 2 * blk + 2].rearrange("a p t -> p a t"), [b_sbgT], [b_sbg1], s_sbg1)
                        if b == 1:
                            cx.dma("sp", m0b[:], m0T[2 * blk:2 * blk + 2].rearrange("a p t -> p a t"), [b_m0], [b_m0b], s_m0b)
                        stt_, stb, sts = sring.next()
                        for tb in range(4):
                            tsl = slice(tb * 512, (tb + 1) * 512)
                            for dch in range(2):
                                ps, pb = next_ps()
                                cx.mm(ps[:, 0:512], [(wt[:, k, dch * 128:(dch + 1) * 128], XT[:, k, tsl]) for k in range(KC)], [wb, b_XT], pb)
                                if b == 0:
                                    cx.op("dve", lambda: V.tensor_tensor(out=stt_[:, dch, tsl], in0=ps[:, 0:512], in1=sbg1[:, dch, tsl], op=ALU.mult), [pb, b_sbg1], [stb])
                                else:
                                    cx.op("dve", lambda: V.tensor_tensor(out=gtmp[:], in0=ps[:, 0:512], in1=sbg1[:, dch, tsl], op=ALU.mult), [pb, b_sbg1], [b_gtmp])
                                    cx.op("pool", lambda: G.tensor_tensor(out=stt_[:, dch, tsl], in0=gtmp[:], in1=m0b[:, dch, tsl], op=ALU.add), [b_gtmp, b_m0b], [stb])
                        dstT, dstB = (m0T, b_m0) if b == 0 else (mergedT, b_mT)
                        cx.dma("sp", dstT[2 * blk:2 * blk + 2].rearrange("a p t -> p a t"), stt_[:], [stb], [dstB], sts, partial=True)
                cx.barrier()
            with ExitStack() as es3b:
                sb3b = lambda name, shape, d: es3b.enter_context(nc.sbuf_tensor(name, list(shape), d))
                xblk = sb3b("xblk", [128, NT, 256], F32)
                b_xblk = Buf()
                s_xblk = cx.semctr("s_xblk")
                h1s = sb3b("h1s", [128, NT, 256], F32)
                b_h1s = Buf()
                s_h1s = cx.semctr("s_h1s")
                load_XT(mergedT, b_mT)
                for blk in range(16):
                    csl = slice(blk * 256, (blk + 1) * 256)
                    wt, wb = load_w(w_out, blk * 256)
                    cx.dma("sp", xblk[:], x[:, csl].rearrange("(t p) c -> p t c", p=128), [], [b_xblk], s_xblk)
                    for t in range(NT):
                        ps, pb = next_ps()
                        cx.mm(ps[:, 0:256], [(XT[:, k, t * 128:(t + 1) * 128], wt[:, k, :]) for k in range(KC)], [wb, b_XT], pb)
                        cx.op("dve", lambda: V.tensor_tensor(out=h1s[:, t, :], in0=ps[:, 0:256], in1=xblk[:, t, :], op=ALU.add), [pb, b_xblk], [b_h1s])
                    cx.dma("sp", h1[:, csl].rearrange("(t p) c -> p t c", p=128), h1s[:], [b_h1s], [b_h1], s_h1s, partial=True)
                cx.barrier()
        norm_T("n2", h1, [b_h1], norm_ffn[0], XT, b_XT, tm_dst=xn2tm, b_tm=b_xn2tm)
        if stage < 5:
            fin = cx.semctr("fin")
            cx.dma("sp", out[0:128, 0:128], ident_in, [b_xn2tm, b_h1], [], fin)
            nc.sync.wait_ge(fin.sem, fin.n)
            return nc
        w_router = din("w_router", [D, 16])
        CAP = 512
        xa_src = dt("xa_src", [16, T], F32).ap()
        xa_dst = dt("xa_dst", [32, T], F32).ap()
        b_xa = Buf()
        es4 = es.enter_context(ExitStack())
        sb4 = lambda name, shape, d: es4.enter_context(nc.sbuf_tensor(name, list(shape), d))
        rkT_d = scr("rkT_d", [16, T], F32)
        rktm_d = scr("rktm_d", [128, NT * 16], F32)
        gtm_d = scr("gtm_d", [128, NT * 16], BF16)
        b_rt = Buf()
        with ExitStack() as es4a:
            sb4a = lambda name, shape, d: es4a.enter_context(nc.sbuf_tensor(name, list(shape), d))
            identf = sb4a("identf4", [128, 128], F32)
            b_c4 = Buf()
            cx.dma("sp", identf[:], ident_in, [], [b_c4], setup)
            wr = sb4a("wr", [128, KC, 16], BF16)
            cx.dma("pool", wr[:], w_router.rearrange("(kc p) e -> p kc e", p=128), [], [b_c4], setup, partial=True)
            aff = sb4a("aff", [128, NT, 16], F32)
            b_aff = Buf()
            sm4 = sb4a("sm4", [128, 4 * NT], F32)
            b_sm4 = Buf()
            affT = sb4a("affT", [16, T], F32)
            b_affT = Buf()
            for t in range(NT):
                ps, pb = next_ps()
                cx.mm(ps[:, 0:16], [(XT[:, k, t * 128:(t + 1) * 128], wr[:, k, :]) for k in range(KC)], [b_XT, b_c4], pb)
                c = lambda i: sm4[:, 4 * t + i:4 * t + i + 1]
                cx.op("dve", lambda: V.reduce_max(out=c(0), in_=ps[:, 0:16], axis=AX.X), [pb], [b_sm4])
                cx.op("dve", lambda: V.tensor_scalar(c(1), c(0), -1.0, None, ALU.mult), [b_sm4], [b_sm4])
                cx.op("act", lambda: A.activation(out=aff[:, t, :], in_=ps[:, 0:16], func=AF.Exp, bias=c(1), accum_out=c(2)), [pb, b_sm4], [b_aff, b_sm4])
                cx.op("dve", lambda: V.reciprocal(c(3), c(2)), [b_sm4], [b_sm4])
                cx.op("dve", lambda: V.tensor_scalar(aff[:, t, :], aff[:, t, :], c(3), None, ALU.mult), [b_sm4, b_aff], [b_aff])
            for g in range(4):
                ps, pb = next_ps()
                cx.mm_multi([(lambda j=j: nc.tensor.transpose(ps[0:16, j * 128:(j + 1) * 128], aff[:, g * 4 + j, :], identf[:])) for j in range(4)], [b_aff, b_c4], pb)
                cx.op("act", lambda: A.copy(out=affT[:, g * 512:(g + 1) * 512], in_=ps[0:16, 0:512]), [pb], [b_affT])
            s_xa = cx.semctr("s_xa")
            cx.dma("sp", xa_src, affT[:], [b_affT], [b_xa], s_xa)
            cx.deps("pool", [b_xa], [])
            ccs2 = cx.sem("ccsem2")
            nc.gpsimd.collective_compute("AllGather", ALU.bypass, replica_groups=[[2 * r, 2 * r + 1] for r in range(NCORES // 2)],
                                         ins=[xa_src.opt()], outs=[xa_dst.opt()]).then_inc(ccs2)
            for e_ in ("pool", "sp"):
                cx.eng[e_].wait_ge(ccs2, 1)
            work = sb4a("work", [16, 2, T], F32)
            b_work = Buf()
            cx.dma("sp", work[:], xa_dst.rearrange("(r e) t -> e r t", e=16), [], [b_work], s_xa)
            m8 = sb4a("m8", [16, 8], F32)
            b_m8 = Buf()
            workf = work[:].rearrange("e r t -> e (r t)")
            for it in range(CAP // 8):
                cx.op("dve", lambda: V.max(out=m8[:], in_=workf), [b_work], [b_m8])
                if it < CAP // 8 - 1:
                    cx.op("dve", lambda: V.match_replace(out=workf, in_to_replace=m8[:], in_values=workf, imm_value=-1.0), [b_m8, b_work], [b_work])
            maskT = sb4a("maskT", [16, T], F32)
            cntT = sb4a("cntT", [16, T], F32)
            onesT = sb4a("onesT", [16, T], F32)
            rkT = sb4a("rkT", [16, T], F32)
            b_mk = Buf()
            cx.op("pool", lambda: G.memset(onesT[:], 1.0), [], [b_mk])
            cx.op("dve", lambda: V.tensor_scalar(maskT[:], affT[:], m8[:, 7:8], None, ALU.is_ge), [b_affT, b_m8], [b_mk])
            cx.op("dve", lambda: V.tensor_tensor_scan(out=cntT[:], data0=onesT[:], data1=maskT[:], initial=0.0, op0=ALU.mult, op1=ALU.add), [b_mk], [b_mk])
            cx.op("dve", lambda: V.tensor_tensor(out=cntT[:], in0=cntT[:], in1=maskT[:], op=ALU.mult), [b_mk], [b_mk])
            cx.op("dve", lambda: V.tensor_scalar(rkT[:], cntT[:], -1.0, None, ALU.add), [b_mk], [b_mk])
            rktm = sb4a("rktm", [128, NT, 16], F32)
            mtm = sb4a("mtm", [128, NT, 16], F32)
            gtm = sb4a("gtm", [128, NT, 16], BF16)
            b_rk = Buf()
            ps, pb = next_ps()
            cx.mm_multi([(lambda t=t: nc.tensor.transpose(ps[:, t * 16:(t + 1) * 16], rkT[:, t * 128:(t + 1) * 128], identf[0:16, 0:16])) for t in range(NT)], [b_mk, b_c4], pb)
            cx.op("act", lambda: A.copy(out=rktm[:].rearrange("p t e -> p (t e)"), in_=ps[:, 0:256]), [pb], [b_rk])
            cx.op("dve", lambda: V.tensor_single_scalar(mtm[:], rktm[:], 0.0, ALU.is_ge), [b_rk], [b_rk])
            cx.op("dve", lambda: V.tensor_tensor(out=gtm[:], in0=aff[:], in1=mtm[:], op=ALU.mult), [b_rk, b_aff], [b_rk])
            cx.dma("sp", rkT_d, rkT[:], [b_mk], [b_rt], s_xa, partial=True)
            cx.dma("sp", rktm_d, rktm[:].rearrange("p t e -> p (t e)"), [b_rk], [b_rt], s_xa, partial=True)
            cx.dma("sp", gtm_d, gtm[:].rearrange("p t e -> p (t e)"), [b_rk], [b_rt], s_xa, partial=True)
            cx.barrier()
        es4.close()
        esX.close()
        if stage < 6:
            fin = cx.semctr("fin")
            cx.dma("sp", out[0:128, 0:128], ident_in, [b_rt], [], fin)
            nc.sync.wait_ge(fin.sem, fin.n)
            return nc
        weg = din("w_expert_gate", [16, D, 2048])
        weu = din("w_expert_up", [16, D, 2048])
        wed = din("w_expert_down", [16, 2048, D])
        b_h2 = [[Buf() for _ in range(8)] for _ in range(NT)]
        with ExitStack() as es5:
            sb5 = lambda name, shape, d: es5.enter_context(nc.sbuf_tensor(name, list(shape), d))
            b_c5 = Buf()
            rkT = sb5("rkT5", [16, T], F32)
            rktm = sb5("rktm5", [128, NT, 16], F32)
            gtm = sb5("gtm5", [128, NT, 16], BF16)
            cx.dma("sp", rkT[:], rkT_d, [b_rt], [b_c5], setup)
            cx.dma("sp", rktm[:].rearrange("p t e -> p (t e)"), rktm_d, [b_rt], [b_c5], setup, partial=True)
            cx.dma("sp", gtm[:].rearrange("p t e -> p (t e)"), gtm_d, [b_rt], [b_c5], setup, partial=True)
            iota_r = sb5("iota_r", [128, CAP], F32)
            cx.dma("sp", iota_r[:], din("iota_row", [1, CAP])[0].partition_broadcast(128), [], [b_c5], setup, partial=True)
            jv = sb5("jv", [128, 4], F32)
            cx.dma("sp", jv[:], din("jvals", [128, 4]), [], [b_c5], setup, partial=True)
            selc = sb5("selc_s", [16, 16, 128], F32)
            cx.dma("sp", selc[:], din("selc", [16, 16, 128]), [], [b_c5], setup, partial=True)
            xring = Ring(cx, "xn2h", 2, [128, NT, 512], BF16, es5)
            wring = Ring(cx, "w5", 4, [128, KC * 256], BF16, es5)
            Pm = sb5("Pm", [128, NT * CAP], BF16)
            b_P = Buf()
            Pv = Pm[:].rearrange("p (t j) -> p t j", t=NT)
            PTv = Pm[:].rearrange("p (c t) -> p c t", c=4)
            xsT = sb5("xsT", [128, KC, CAP], BF16)
            b_xs = Buf()
            hidT = sb5("hidT", [128, 16, CAP], BF16)
            b_hid = Buf()
            ysel = sb5("ysel", [128, 4, 512], BF16)
            b_ys = Buf()
            gsel = sb5("gsel", [128, 4], F32)
            b_gs = Buf()
            stmp = sb5("stmp", [128, 512], F32)
            b_stmp = Buf()
            dtmp = sb5("dtmp", [128, 512], F32)
            b_dtmp = Buf()
            yring = Ring(cx, "yst", 4, [128, 512], F32, es5)
            nexp = 16 if (debug is None or stage >= 7) else 2
            wblocks = []
            for e in range(nexp):
                for blk in range(8):
                    wblocks.append(("g", e, blk))
                    wblocks.append(("u", e, blk))
                for cb in range(8):
                    wblocks.append(("d", e, cb))
            wloaded = []
            wstate = {"issued": 0, "used": 0}

            def w_issue_to(k):
                while wstate["issued"] < min(k, len(wblocks)):
                    kind, e_, i_ = wblocks[wstate["issued"]]
                    j_ = wstate["issued"]
                    if j_ >= 2:
                        cx._wait("pool", wloaded[j_ - 2][2], wloaded[j_ - 2][3])
                    wt, wb, wsm = wring.next()
                    if kind == "d":
                        cx.dma("pool", wt[:].rearrange("p (f c) -> p f c", f=16), wed[e_][:, i_ * 512:(i_ + 1) * 512].rearrange("(f p) c -> p f c", p=128), [], [wb], wsm)
                    else:
                        Wm = weg if kind == "g" else weu
                        cx.dma("pool", wt[:].rearrange("p (k c) -> p k c", k=KC), Wm[e_][:, i_ * 256:(i_ + 1) * 256].rearrange("(kc p) c -> p kc c", p=128), [], [wb], wsm)
                    wloaded.append((wt, wb, wsm.sem, wsm.n))
                    wstate["issued"] += 1

            def w_take(n):
                i = wstate["used"]
                wstate["used"] += n
                w_issue_to(i + 4)
                return wloaded[i:i + n]

            for e in range(nexp):
                for t in range(NT):
                    cx.op("dve", lambda: V.tensor_scalar(Pv[:, t, :], iota_r[:], rktm[:, t, e:e + 1], None, ALU.is_equal), [b_c5], [b_P])
                ps, pb = next_ps()
                for jc in range(4):
                    cx.mm(ps[:, jc:jc + 1], [(Pv[:, t, jc * 128:(jc + 1) * 128], gtm[:, t, e:e + 1]) for t in range(NT)], [b_P, b_c5], pb)
                cx.op("act", lambda: A.copy(out=gsel[:], in_=ps[:, 0:4]), [pb], [b_gs])
                for q8 in range(8):
                    xn2h, b_xh, s_xh = xring.next()
                    cx.dma("sp", xn2h[:], xn2tm[:, q8 * 512:(q8 + 1) * 512].rearrange("(t p) c -> p t c", p=128), [b_xn2tm], [b_xh], s_xh)
                    for dc in range(4):
                        ps, pb = next_ps()
                        cx.mm(ps[:, 0:CAP], [(xn2h[:, t, dc * 128:(dc + 1) * 128], Pv[:, t, :]) for t in range(NT)], [b_xh, b_P], pb)
                        if dc % 2 == 0:
                            cx.op("act", lambda: A.copy(out=xsT[:, q8 * 4 + dc, :], in_=ps[:, 0:CAP]), [pb], [b_xs])
                        else:
                            cx.op("dve", lambda: V.tensor_copy(out=xsT[:, q8 * 4 + dc, :], in_=ps[:, 0:CAP]), [pb], [b_xs])
                for blk in range(8):
                    ws_ = [(wt[:].rearrange("p (k c) -> p k c", k=KC), wb) for (wt, wb, _s, _n) in w_take(2)]
                    for fs in range(2):
                        pss = []
                        for (wv, wb) in ws_:
                            ps, pb = next_ps()
                            cx.mm(ps[:, 0:CAP], [(wv[:, k, fs * 128:(fs + 1) * 128], xsT[:, k, :]) for k in range(KC)], [wb, b_xs], pb)
                            pss.append((ps, pb))
                        cx.op("act", lambda: A.activation(out=stmp[:], in_=pss[0][0][:, 0:CAP], func=AF.Silu), [pss[0][1]], [b_stmp])
                        cx.op("dve", lambda: V.tensor_tensor(out=hidT[:, blk * 2 + fs, :], in0=stmp[:], in1=pss[1][0][:, 0:CAP], op=ALU.mult), [b_stmp, pss[1][1]], [b_hid])
                for tb in range(4):
                    tsl = slice(tb * 512, (tb + 1) * 512)
                    ps, pb = next_ps()
                    cx.mm(ps[:, 0:512], [(selc[:, e, :], rkT[:, tsl])], [b_c5], pb)
                    for jc in range(4):
                        cx.op("dve", lambda: V.tensor_scalar(dtmp[:], ps[:, 0:512], jv[:, jc:jc + 1], None, ALU.subtract), [pb, b_c5], [b_dtmp])
                        cx.op("act", lambda: A.activation(out=dtmp[:], in_=dtmp[:], func=AF.Square), [b_dtmp], [b_dtmp])
                        cx.op("dve", lambda: V.tensor_single_scalar(PTv[:, jc, tsl], dtmp[:], 0.25, ALU.is_lt), [b_dtmp], [b_P])
                for cb in range(8):
                    csl = slice(cb * 512, (cb + 1) * 512)
                    (wt, wb, _s, _n), = w_take(1)
                    wv = wt[:].rearrange("p (f c) -> p f c", f=16)
                    for jc in range(4):
                        ps, pb = next_ps()
                        cx.mm(ps[:, 0:512], [(hidT[:, f, jc * 128:(jc + 1) * 128], wv[:, f, :]) for f in range(16)], [b_hid, wb], pb)
                        cx.op("act", lambda: A.activation(out=ysel[:, jc, :], in_=ps[:, 0:512], func=AF.Copy, scale=gsel[:, jc:jc + 1]), [pb, b_gs], [b_ys])
                    for t in range(NT):
                        ps, pb = next_ps()
                        cx.mm(ps[:, 0:512], [(PTv[:, jc, t * 128:(t + 1) * 128], ysel[:, jc, :]) for jc in range(4)], [b_P, b_ys], pb)
                        yt, yb, ysm = yring.next()
                        if t % 2 == 0:
                            cx.op("act", lambda: A.copy(out=yt[:], in_=ps[:, 0:512]), [pb], [yb])
                        else:
                            cx.op("dve", lambda: V.tensor_copy(out=yt[:], in_=ps[:, 0:512]), [pb], [yb])
                        cx.dma("pool", h1[t * 128:(t + 1) * 128, csl], yt[:], [yb, b_h1], [b_h2[t][cb]], ysm, accum_op=ALU.add)
            cx.barrier()
        if stage < 7:
            fin = cx.semctr("fin")
            cx.dma("sp", out[0:128, 0:128], ident_in, [], [], fin)
            nc.sync.wait_ge(fin.sem, fin.n)
            return nc
        norm_ple = din("norm_ple", [1, D])
        w_pg = din("w_ple_gate", [D, D])
        w_pp = din("w_ple_proj", [256, D])
        p_in = din("p", [T, 256])
        norm_final = din("norm_final", [1, D])
        h3 = scr("h3", [T, D], F32)
        b_h3 = Buf()
        esX = es.enter_context(ExitStack())
        XT = esX.enter_context(nc.sbuf_tensor("AT6", [128, KC, T], BF16))
        b_XT = Buf("AT6")
        norm_T("n3", h1, [], norm_ple[0], XT, b_XT)
        with ExitStack() as es6:
            sb6 = lambda name, shape, d: es6.enter_context(nc.sbuf_tensor(name, list(shape), d))
            b_c6 = Buf()
            wpp = sb6("wpp", [128, 2, D], BF16)
            cx.dma("pool", wpp[:], w_pp.rearrange("(k p) c -> p k c", p=128), [], [b_c6], setup)
            pT = sb6("pT", [128, 2, T], BF16)
            b_pT = Buf()
            h2b = sb6("h2b", [128, NT, 256], F32)
            b_h2b = Buf()
            s_h2b = cx.semctr("s_h2b")
            with ExitStack() as es6p:
                ptm = es6p.enter_context(nc.sbuf_tensor("ptm", [128, NT, 256], F32))
                pbf = es6p.enter_context(nc.sbuf_tensor("pbf", [128, NT, 256], BF16))
                b_pp = Buf()
                cx.dma("sp", ptm[:], p_in.rearrange("(t p) c -> p t c", p=128), [], [b_pp], setup)
                cx.op("dve", lambda: V.tensor_copy(out=pbf[:], in_=ptm[:]), [b_pp], [b_pp])
                for t in range(NT):
                    ps, pb = next_ps()
                    pv = ps[:].bitcast(BF16)
                    cx.mm_multi([(lambda k2=k2: nc.tensor.transpose(pv[:, k2 * 128:(k2 + 1) * 128], pbf[:, t, k2 * 128:(k2 + 1) * 128], ident[:])) for k2 in range(2)], [b_pp, b_const], pb)
                    cx.op("act", lambda: A.copy(out=pT[:, :, t * 128:(t + 1) * 128], in_=pv[:, 0:256].rearrange("p (a b) -> p a b", a=2)), [pb], [b_pT])
                cx.barrier()
            h3s = h2b
            b_h3s = b_h2b
            s_h3s = s_h2b
            load_w = w_loader(Ring(cx, "w6", 2, [128, KC, 256], BF16, es6))
            sgt = sb6("sgt", [128, 256], F32)
            b_sgt = Buf()
            t1t = sb6("t1t", [128, 256], F32)
            b_t1 = Buf()
            for blk in range(16):
                csl = slice(blk * 256, (blk + 1) * 256)
                wt, wb = load_w(w_pg, blk * 256)
                cx.dma("sp", h2b[:], h1[:, csl].rearrange("(t p) c -> p t c", p=128), [], [b_h2b], s_h2b)
                for t in range(NT):
                    tsl = slice(t * 128, (t + 1) * 128)
                    ps, pb = next_ps()
                    cx.mm(ps[:, 0:256], [(XT[:, k, tsl], wt[:, k, :]) for k in range(KC)], [wb, b_XT], pb)
                    ps2, pb2 = next_ps()
                    cx.mm(ps2[:, 0:256], [(pT[:, k2, tsl], wpp[:, k2, csl]) for k2 in range(2)], [b_pT, b_c6], pb2)
                    cx.op("act", lambda: A.activation(out=sgt[:], in_=ps[:, 0:256], func=AF.Sigmoid), [pb], [b_sgt])
                    cx.op("dve", lambda: V.tensor_tensor(out=t1t[:], in0=sgt[:], in1=ps2[:, 0:256], op=ALU.mult), [b_sgt, pb2], [b_t1])
                    cx.op("pool", lambda: G.tensor_tensor(out=h3s[:, t, :], in0=t1t[:], in1=h2b[:, t, :], op=ALU.add), [b_t1, b_h2b], [b_h3s])
                cx.dma("sp", h3[:, csl].rearrange("(t p) c -> p t c", p=128), h3s[:], [b_h3s], [b_h3], s_h3s, partial=True)
            cx.barrier()
        esX.close()
        fin = cx.semctr("fin")
        with ExitStack() as es7:
            sb7 = lambda name, shape, d: es7.enter_context(nc.sbuf_tensor(name, list(shape), d))
            gain = sb7("gainF", [128, D], F32)
            b_g = Buf()
            cx.dma("sp", gain[:], norm_final[0].partition_broadcast(128), [], [b_g], setup)
            xr_t = [sb7(f"fr{i}", [128, D], F32) for i in range(2)]
            xr_b = [Buf() for _ in range(2)]
            xr_s = [cx.semctr(f"s_fr{i}") for i in range(2)]
            yo_t = [sb7(f"fo{i}", [128, D], F32) for i in range(2)]
            yo_b = [Buf() for _ in range(2)]
            junk = sb7("junkF", [128, D], BF16)
            b_junk = Buf()
            st = sb7("stF", [128, 4 * NT], F32)
            b_st = Buf()
            for t in range(NT):
                xt, xb, xs = xr_t[t % 2], xr_b[t % 2], xr_s[t % 2]
                yo, yb = yo_t[t % 2], yo_b[t % 2]
                cx.dma("sp", xt[:], h3[t * 128:(t + 1) * 128, :], [b_h3], [xb], xs)
                c0 = st[:, 4 * t:4 * t + 1]
                c1 = st[:, 4 * t + 1:4 * t + 2]
                c2 = st[:, 4 * t + 2:4 * t + 3]
                cx.op("act", lambda: A.activation(out=junk[:], in_=xt[:], func=AF.Square, accum_out=c0), [xb], [b_junk, b_st])
                cx.op("dve", lambda: V.tensor_scalar(c1, c0, 1.0 / D, EPS, ALU.mult, ALU.add), [b_st], [b_st])
                cx.op("act", lambda: A.activation(out=c2, in_=c1, func=AF.Sqrt), [b_st], [b_st])
                cx.op("dve", lambda: V.reciprocal(c1, c2), [b_st], [b_st])
                cx.op("dve", lambda: V.scalar_tensor_tensor(out=yo[:], in0=xt[:], scalar=c1, in1=gain[:], op0=ALU.mult, op1=ALU.mult), [xb, b_st, b_g], [yb])
                cx.dma("sp", out[t * 128:(t + 1) * 128, :], yo[:], [yb], [], fin)
            nc.sync.wait_ge(fin.sem, fin.n)
            cx.barrier()
    return nc


def make_consts():
    ident = np.eye(128, dtype=np.float32)
    invf = (10000.0 ** (-np.arange(128, dtype=np.float32) / np.float32(128))).astype(np.float32).reshape(128, 1)
    jj = np.arange(128)
    maskF = (jj[None, :] >= jj[:, None]).astype(np.float32)
    maskB = (jj[:, None] > jj[None, :]).astype(np.float32)
    rmask = (np.arange(T) % 128 != 0).astype(np.float32).reshape(1, T)
    iota_row = np.arange(512, dtype=np.float32).reshape(1, 512)
    jvals = (np.arange(128)[:, None] + 128 * np.arange(4)[None, :]).astype(np.float32)
    selc = np.zeros((16, 16, 128), np.float32)
    for e in range(16):
        selc[e, e, :] = 1.0
    return {"ident": ident, "invf": invf, "maskF": maskF, "maskB": maskB, "rmask": rmask, "iota_row": iota_row, "jvals": jvals, "selc": selc}


def make_in_maps(inputs, cores):
    f = lambda k: np.asarray(inputs[k], dtype=np.float32)
    x = f("x")
    positions = np.asarray(inputs["positions"], dtype=np.int32)
    p = f("p")[0]
    consts = make_consts()
    shared = {
        "norm_mix": f("norm_mix").reshape(1, D),
        "w_in": np.ascontiguousarray(f("w_in")[0]),
        "ret_decay_logit": f("ret_decay_logit").reshape(1, 16),
        "gla_gate_w": np.ascontiguousarray(f("gla_gate_w")[0]),
        "gla_gate_b": np.ascontiguousarray(f("gla_gate_b")[0]),
        "ret_norm": f("ret_norm").reshape(1, D),
        "gla_norm": f("gla_norm").reshape(1, D),
        "w_branch": np.ascontiguousarray(f("w_branch")[0]),
        "w_out": np.ascontiguousarray(f("w_out")[0]),
        "norm_ffn": f("norm_ffn").reshape(1, D),
        "w_router": np.ascontiguousarray(f("w_router")[0]),
        "w_expert_gate": np.ascontiguousarray(f("w_expert_gate")[0]),
        "w_expert_up": np.ascontiguousarray(f("w_expert_up")[0]),
        "w_expert_down": np.ascontiguousarray(f("w_expert_down")[0]),
        "norm_ple": f("norm_ple").reshape(1, D),
        "w_ple_gate": np.ascontiguousarray(f("w_ple_gate")[0]),
        "w_ple_proj": np.ascontiguousarray(f("w_ple_proj")[0]),
        "norm_final": f("norm_final").reshape(1, D),
    }
    in_maps = []
    for c in cores:
        b, hf = c // 2, c % 2
        sl = slice(hf * T, (hf + 1) * T)
        m = dict(consts)
        m.update(shared)
        m["x"] = np.ascontiguousarray(x[b, sl])
        m["pos"] = np.ascontiguousarray(positions[b, sl]).reshape(1, T)
        m["p"] = np.ascontiguousarray(p[b, sl])
        m["flags"] = np.array([[float(hf), float(1 - hf)]], np.float32)
        in_maps.append(m)
    return in_maps


def kernel(**inputs):
    n = NCORES
    in_maps = make_in_maps(inputs, list(range(n)))
    nc = build(stage=99)
    res = run_bass_kernel_spmd(nc, in_maps, core_ids=list(range(n)))
    outs = [np.asarray(r["out"], dtype=np.float32) for r in res.results]
    return np.stack(outs, 0).reshape(n // 2, 2 * T, D)
```
